# Optimizing a Trainium2 kernel written in Bass

```python
import math
import jax, jax.numpy as jnp
from jax import lax
import numpy as np

D_MODEL = 1024
BATCH = 32
SEQ = 2048
DEPTH = 1
DEC_BATCH = 32
DEC_SEQ = 32
PAST_LEN = 1024

CHUNK = 64
EPS = 1e-6
A_HEADS = 4
A_DK = 64
A_DV = 128
A_QBLOCK = 128
R_HEADS = 4
R_DK = 128
R_DV = 128
R_BLOCK = 16
P_HEADS = 8
P_NKEYS = 128
P_NEXPERTS = P_NKEYS * P_NKEYS
P_DQ = 128
P_TOPK = 16
P_TBLOCK = 256

A_QK_W = A_HEADS * 2 * A_DK
A_V_W = A_HEADS * A_DV
R_K_W = R_HEADS * R_DK
R_V_W = R_HEADS * R_DV
IN_SPLITS = (A_QK_W, 2 * A_QK_W, 2 * A_QK_W + A_V_W, 2 * A_QK_W + A_V_W + R_K_W,
             2 * A_QK_W + A_V_W + 2 * R_K_W, 2 * A_QK_W + A_V_W + 2 * R_K_W + R_V_W,
             2 * A_QK_W + A_V_W + 2 * R_K_W + 2 * R_V_W)
IN_COLS = 2 * A_QK_W + A_V_W + 2 * R_K_W + 2 * R_V_W + 2 * D_MODEL

kernel_name = 'diffattn_hgrn2_peer_streaming_step'


def rmsnorm(x, w):
    xf = x.astype(jnp.float32)
    y = xf * lax.rsqrt(jnp.mean(xf * xf, axis=-1, keepdims=True) + EPS)
    return (y * w.astype(jnp.float32)).astype(x.dtype)


def head_rms(o, w):
    return o * lax.rsqrt(jnp.mean(o * o, axis=-1, keepdims=True) + EPS) * w.astype(jnp.float32)


def chunk_mask(q_pos, k_pos):
    return (k_pos[None, :] // CHUNK) <= (q_pos[:, None] // CHUNK)


def diff_softmax_mix(q, k, v, mask, lam):
    s = jnp.einsum('bqhmd,bkhmd->bhmqk', q.astype(jnp.float32), k.astype(jnp.float32)) * (A_DK ** -0.5)
    s = jnp.where(mask, s, -jnp.inf)
    p = jax.nn.softmax(s, axis=-1)
    w = p[:, :, 0] - lam * p[:, :, 1]
    return jnp.einsum('bhqk,bkhv->bqhv', w, v.astype(jnp.float32))


def diff_attn_prompt(q, k, v, lam):
    B, T = q.shape[0], q.shape[1]
    nb = T // A_QBLOCK
    qb = q.reshape(B, nb, A_QBLOCK, A_HEADS, 2, A_DK).transpose(1, 0, 2, 3, 4, 5)
    k_pos = jnp.arange(T)

    def blk(args):
        q_blk, j = args
        q_pos = j * A_QBLOCK + jnp.arange(A_QBLOCK)
        return diff_softmax_mix(q_blk, k, v, chunk_mask(q_pos, k_pos), lam)

    o = lax.map(blk, (qb, jnp.arange(nb)))
    return o.transpose(1, 0, 2, 3, 4).reshape(B, T, A_HEADS, A_DV)


def gla_chunked(q, k, v, log_f, s0):
    B, T, H, DK = q.shape
    DV = v.shape[-1]
    pad = (-T) % R_BLOCK
    if pad:
        pw = ((0, 0), (0, pad), (0, 0), (0, 0))
        q, k, v, log_f = (jnp.pad(a, pw) for a in (q, k, v, log_f))
    n = (T + pad) // R_BLOCK
    q, k, v, log_f = (a.reshape(B, n, R_BLOCK, H, a.shape[-1]) for a in (q, k, v, log_f))
    b = jnp.cumsum(log_f, axis=2)
    b_last = b[:, :, -1:]
    q_in = q * jnp.exp(b)
    k_in = k * jnp.exp(-b)
    k_out = k * jnp.exp(b_last - b)
    causal = jnp.tril(jnp.ones((R_BLOCK, R_BLOCK), dtype=bool))
    a = jnp.where(causal, jnp.einsum('bnthd,bnshd->bnhts', q_in, k_in), 0.0)
    o_intra = jnp.einsum('bnhts,bnshv->bnthv', a, v)

    def step(S, xs):
        qi, ko, vi, dl = xs
        o = jnp.einsum('bthd,bhdv->bthv', qi, S)
        S = dl[..., None] * S + jnp.einsum('bshd,bshv->bhdv', ko, vi)
        return S, o

    xs = (q_in.swapaxes(0, 1), k_out.swapaxes(0, 1), v.swapaxes(0, 1),
          jnp.exp(b_last[:, :, 0]).swapaxes(0, 1))
    S, o_inter = lax.scan(step, s0, xs)
    o = o_intra + o_inter.swapaxes(0, 1)
    return o.reshape(B, n * R_BLOCK, H, DV)[:, :T], S


def token_mixer(xn, past_k, past_v, s0, lam, lam_init, w_in, a_subln, lb, r_gnorm, w_a, w_b, w_out):
    B, T, _ = xn.shape
    f32 = jnp.float32
    z = xn @ w_in
    qa, ka, va, f_pre, q_pre, i_pre, og_pre, gates = jnp.split(z, IN_SPLITS, axis=-1)
    qa = qa.reshape(B, T, A_HEADS, 2, A_DK)
    ka = ka.reshape(B, T, A_HEADS, 2, A_DK)
    va = va.reshape(B, T, A_HEADS, A_DV)
    if past_k is None:
        oa = diff_attn_prompt(qa, ka, va, lam)
    else:
        P = past_k.shape[1]
        k_all = jnp.concatenate([past_k.reshape(B, P, A_HEADS, 2, A_DK).astype(ka.dtype), ka], axis=1)
        v_all = jnp.concatenate([past_v.astype(va.dtype), va], axis=1)
        mask = chunk_mask(P + jnp.arange(T), jnp.arange(P + T))
        oa = diff_softmax_mix(qa, k_all, v_all, mask, lam)
    oa = head_rms(oa, a_subln) * (1.0 - lam_init)
    g = lb + (1.0 - lb) * jax.nn.sigmoid(f_pre.astype(f32))
    log_f = jnp.log(g).reshape(B, T, R_HEADS, R_DK)
    kr = (1.0 - g).reshape(B, T, R_HEADS, R_DK)
    qr = jax.nn.silu(q_pre.astype(f32)).reshape(B, T, R_HEADS, R_DK)
    ir = i_pre.astype(f32).reshape(B, T, R_HEADS, R_DV)
    orr, s_new = gla_chunked(qr, kr, ir, log_f, s0)
    orr = head_rms(orr, r_gnorm) * jax.nn.silu(og_pre.astype(f32)).reshape(B, T, R_HEADS, R_DV)
    dt = xn.dtype
    ga, gb = jnp.split(gates, 2, axis=-1)
    pa = oa.reshape(B, T, A_V_W).astype(dt) @ w_a
    pb = orr.reshape(B, T, R_V_W).astype(dt) @ w_b
    y = (jax.nn.sigmoid(ga) * pa + jax.nn.sigmoid(gb) * pb) @ w_out
    return y, ka.reshape(B, T, A_HEADS, 2 * A_DK), va, s_new


def peer(xn, w_q, sub_keys, down, up):
    B, T, D = xn.shape
    n = B * T
    xt = xn.reshape(n, D)
    pad = (-n) % P_TBLOCK
    if pad:
        xt = jnp.pad(xt, ((0, pad), (0, 0)))
    xb = xt.reshape(-1, P_TBLOCK, D)

    def blk(xs):
        q = (xs @ w_q).reshape(P_TBLOCK, P_HEADS, 2, P_DQ).astype(jnp.float32)
        s = jnp.einsum('thcd,hckd->thck', q, sub_keys.astype(jnp.float32))
        sv, si = lax.top_k(s, P_TOPK)
        comb = (sv[:, :, 0, :, None] + sv[:, :, 1, None, :]).reshape(P_TBLOCK, P_HEADS, P_TOPK * P_TOPK)
        cv, ci = lax.top_k(comb, P_TOPK)
        e = (jnp.take_along_axis(si[:, :, 0], ci // P_TOPK, axis=-1) * P_NKEYS
             + jnp.take_along_axis(si[:, :, 1], ci % P_TOPK, axis=-1))
        gate = jax.nn.softmax(cv, axis=-1)
        hid = jax.nn.gelu(jnp.einsum('thkd,td->thk', down[e], xs).astype(jnp.float32), approximate=False)
        return jnp.einsum('thk,thkd->td', (gate * hid).astype(xs.dtype), up[e])

    out = lax.map(blk, xb).reshape(-1, D)[:n]
    return out.reshape(B, T, D)


def trunk(x, past_k, past_v, past_s, lbs, norm1, w_in, lam_params, a_subln, r_gnorm,
          w_a, w_b, w_out, norm2, p_wq, p_keys, p_down, p_up, final_norm):
    B = x.shape[0]
    h = x
    ks, vs, ss = [], [], []
    for l in range(DEPTH):
        lam_init = 0.8 - 0.6 * math.exp(-0.3 * l)
        lp = lam_params[l].astype(jnp.float32)
        lam = jnp.exp(jnp.sum(lp[0] * lp[1])) - jnp.exp(jnp.sum(lp[2] * lp[3])) + lam_init
        if past_s is None:
            s0 = jnp.zeros((B, R_HEADS, R_DK, R_DV), jnp.float32)
            pk, pv = None, None
        else:
            s0 = past_s[l].astype(jnp.float32)
            pk, pv = past_k[l], past_v[l]
        y, nk, nv, ns = token_mixer(rmsnorm(h, norm1[l]), pk, pv, s0, lam, lam_init, w_in[l], a_subln[l],
                                    lbs[l], r_gnorm[l], w_a[l], w_b[l], w_out[l])
        h = h + y
        h = h + peer(rmsnorm(h, norm2[l]), p_wq[l], p_keys[l], p_down[l], p_up[l])
        ks.append(nk)
        vs.append(nv)
        ss.append(ns.astype(x.dtype))
    return rmsnorm(h, final_norm), jnp.stack(ks), jnp.stack(vs), jnp.stack(ss)


def setup_inputs(seed: int = 0) -> dict:
    key = jax.random.key(seed)
    ks = jax.random.split(key, 24)
    f32 = jnp.float32

    def nrm(k, shape, scale):
        return jax.random.normal(k, shape, f32) * scale

    def gain(k, shape):
        return 1.0 + 0.05 * jax.random.normal(k, shape, f32)

    return {
        'x_prompt': nrm(ks[0], (BATCH, SEQ, D_MODEL), 1.0),
        'x_sample': nrm(ks[1], (DEC_BATCH, DEC_SEQ, D_MODEL), 1.0),
        'cache_k': nrm(ks[2], (DEPTH, DEC_BATCH, PAST_LEN, A_HEADS, 2 * A_DK), 1.0),
        'cache_v': nrm(ks[3], (DEPTH, DEC_BATCH, PAST_LEN, A_HEADS, A_DV), 1.0),
        'state_hgrn': nrm(ks[4], (DEPTH, DEC_BATCH, R_HEADS, R_DK, R_DV), 0.5),
        'norm1': gain(ks[5], (DEPTH, D_MODEL)),
        'w_in': nrm(ks[6], (DEPTH, D_MODEL, IN_COLS), D_MODEL ** -0.5),
        'lam_params': nrm(ks[7], (DEPTH, 4, A_DK), 0.1),
        'a_subln': gain(ks[8], (DEPTH, A_DV)),
        'r_lb_logits': nrm(ks[9], (DEPTH + 1, R_K_W), 0.1),
        'r_gnorm': gain(ks[10], (DEPTH, R_DV)),
        'w_a': nrm(ks[11], (DEPTH, A_V_W, D_MODEL), A_V_W ** -0.5),
        'w_b': nrm(ks[12], (DEPTH, R_V_W, D_MODEL), R_V_W ** -0.5),
        'w_out': nrm(ks[13], (DEPTH, D_MODEL, D_MODEL), D_MODEL ** -0.5),
        'norm2': gain(ks[14], (DEPTH, D_MODEL)),
        'p_wq': nrm(ks[15], (DEPTH, D_MODEL, P_HEADS * 2 * P_DQ), D_MODEL ** -0.5),
        'p_keys': nrm(ks[16], (DEPTH, P_HEADS, 2, P_NKEYS, P_DQ), P_DQ ** -0.5),
        'p_down': nrm(ks[17], (DEPTH, P_NEXPERTS, D_MODEL), D_MODEL ** -0.5),
        'p_up': nrm(ks[18], (DEPTH, P_NEXPERTS, D_MODEL), P_HEADS ** -0.5),
        'final_norm': gain(ks[19], (D_MODEL,)),
    }


def reference(x_prompt, x_sample, cache_k, cache_v, state_hgrn, norm1, w_in, lam_params, a_subln,
              r_lb_logits, r_gnorm, w_a, w_b, w_out, norm2, p_wq, p_keys, p_down, p_up, final_norm):
    lbs = jnp.cumsum(jax.nn.softmax(r_lb_logits.astype(jnp.float32), axis=0), axis=0)[:DEPTH]
    y_prompt, k_prompt, v_prompt, s_prompt = trunk(
        x_prompt, None, None, None, lbs, norm1, w_in, lam_params, a_subln, r_gnorm,
        w_a, w_b, w_out, norm2, p_wq, p_keys, p_down, p_up, final_norm)
    y_sample, k_sample, v_sample, s_sample = trunk(
        x_sample, cache_k, cache_v, state_hgrn, lbs, norm1, w_in, lam_params, a_subln, r_gnorm,
        w_a, w_b, w_out, norm2, p_wq, p_keys, p_down, p_up, final_norm)
    return (y_prompt, y_sample, k_prompt, v_prompt, s_prompt, k_sample, v_sample, s_sample)
```

```python
import math
import numpy as np
import concourse.bass as bass
import concourse.mybir as mybir
from concourse.bass_utils import run_bass_kernel_spmd

F32 = mybir.dt.float32
BF16 = mybir.dt.bfloat16
I32 = mybir.dt.int32
U32 = mybir.dt.uint32
AF = mybir.ActivationFunctionType
OP = mybir.AluOpType
AX = mybir.AxisListType

D = 1024
EPS = 1e-6
NCORES = 8
IN_COLS = 5632


class Cfg:
    def __init__(self, nseq_p=4, t_p=2048, nseq_s=4, t_s=32, past=1024, stage=99, nchunk=128):
        self.nseq_p, self.t_p, self.nseq_s, self.t_s, self.past = nseq_p, t_p, nseq_s, t_s, past
        self.stage = stage
        self.nchunk = nchunk


class Buf:
    __slots__ = ("name", "w", "r", "dsem", "dcnt", "excl")

    def __init__(self, name="", excl=False):
        self.name = name
        self.excl = excl
        self.w = {}
        self.r = {}
        self.dsem = None
        self.dcnt = 0


class Sched:
    ENGS = ("pe", "act", "dve", "pool", "sp")

    def __init__(self, nc):
        self.nc = nc
        self.eng = dict(pe=nc.tensor, act=nc.scalar, dve=nc.vector, pool=nc.gpsimd, sp=nc.sync)
        self.sem = {e: nc.alloc_semaphore("c_" + e) for e in self.ENGS}
        self.cnt = {e: 0 for e in self.ENGS}
        self.seen = {e: {} for e in self.ENGS}
        self.dsems = {}
        self.ninst = 0
        self.out_tokens = {}

    def _semof(self, key):
        if key in self.sem:
            return self.sem[key]
        return self.dsems[key][0]

    def _cur(self, key, val):
        if key in self.dsems:
            return max(val, self.dsems[key][1].dcnt * 16)
        return val

    def _wait(self, eng, deps):
        e = self.eng[eng]
        for key, val in deps.items():
            if key == eng and eng == "pe":
                continue
            val = self._cur(key, val)
            if self.seen[eng].get(key, 0) >= val:
                continue
            e.wait_ge(self._semof(key), val)
            self.seen[eng][key] = val
            self.ninst += 1

    @staticmethod
    def _merge(deps, d):
        for k, v in d.items():
            if deps.get(k, 0) < v:
                deps[k] = v

    def _deps(self, reads, writes, eng=None):
        deps = {}
        for b in reads:
            self._merge(deps, b.w)
            if b.excl:
                self._merge(deps, {k: v for k, v in b.r.items() if k != eng})
        for b in writes:
            self._merge(deps, b.w)
            self._merge(deps, b.r)
        return deps

    def op(self, eng, fn, reads=(), writes=()):
        self._wait(eng, self._deps(reads, writes, eng))
        ins = fn(self.eng[eng])
        self.cnt[eng] += 1
        n = self.cnt[eng]
        ins.then_inc(self.sem[eng], 1)
        self.ninst += 1
        for b in reads:
            if b.r.get(eng, 0) < n:
                b.r[eng] = n
        for b in writes:
            b.w = {eng: n}
            b.r = {}
        return ins

    def dma(self, eng, out, in_, reads=(), writes=(), sb=None, fn=None, is_output=False):
        if sb is None:
            sb = writes[0] if writes else reads[0]
        if sb.dsem is None:
            sb.dsem = self.nc.alloc_semaphore("d_%d" % len(self.dsems))
            self.dsems[id(sb)] = (sb.dsem, sb)
        key = id(sb)
        self._wait(eng, self._deps(reads, writes))
        if fn is None:
            ins = self.eng[eng].dma_start(out=out, in_=in_)
        else:
            ins = fn(self.eng[eng])
        sb.dcnt += 1
        val = sb.dcnt * 16
        ins.then_inc(sb.dsem, 16)
        self.ninst += 1
        for b in reads:
            b.r[key] = val
        for b in writes:
            b.w = {key: val}
            b.r = {}
        if is_output:
            self.out_tokens[key] = val
        return ins

    def barrier(self):
        deps = {key: b.dcnt * 16 for key, (sem, b) in self.dsems.items()}
        for k in self.ENGS:
            if self.cnt[k]:
                deps[k] = self.cnt[k]
        for eng in self.ENGS:
            self._wait(eng, {k: v for k, v in deps.items() if k != eng})

    def finish(self, eng="sp"):
        deps = {key: b.dcnt * 16 for key, (sem, b) in self.dsems.items()}
        for k in ("pe", "act", "dve", "pool"):
            if self.cnt[k]:
                deps[k] = self.cnt[k]
        self._wait(eng, deps)


def build(cfg):
    nc = bass.Bass("TRN2", target_bir_lowering=False)
    NP, TP, NS, TS, PAST = cfg.nseq_p, cfg.t_p, cfg.nseq_s, cfg.t_s, cfg.past
    S = Sched(nc)

    def din(name, shape, dt=F32):
        return nc.dram_tensor(name, list(shape), dt, kind="ExternalInput").ap()

    def dout(name, shape, dt=F32):
        return nc.dram_tensor(name, list(shape), dt, kind="ExternalOutput").ap()

    x_p = din("x_prompt", [NP, TP, D])
    x_s = din("x_sample", [NS, TS, D])
    ck = din("cache_k", [NS, PAST, 512])
    cv = din("cache_v", [NS, PAST, 512])
    st = din("state_hgrn", [NS, 4, 128, 128])
    norm1 = din("norm1", [1, D])
    w_in = din("w_in", [D, IN_COLS])
    lam_params = din("lam_params", [1, 256])
    a_subln = din("a_subln", [1, 128])
    r_lb = din("r_lb_logits", [2, 512])
    r_gnorm = din("r_gnorm", [1, 128])
    w_a = din("w_a", [512, D])
    w_b = din("w_b", [512, D])
    w_out = din("w_out", [D, D])
    norm2 = din("norm2", [1, D])
    p_wq = din("p_wq", [D, 2048])
    p_keys = din("p_keys", [16, 128, 128])
    p_downT = din("p_downT", [128, 128, D])
    p_up = din("p_up", [16384, D])
    final_norm = din("final_norm", [1, D])

    y_p = dout("y_prompt", [NP, TP, D])
    y_s = dout("y_sample", [NS, TS, D])
    k_p = dout("k_prompt", [NP, TP, 512])
    v_p = dout("v_prompt", [NP, TP, 512])
    s_p = dout("s_prompt", [NP, 4, 128, 128])
    k_s = dout("k_sample", [NS, TS, 512])
    v_s = dout("v_sample", [NS, TS, 512])
    s_s = dout("s_sample", [NS, 4, 128, 128])

    import contextlib
    es = contextlib.ExitStack()

    def sb(name, shape, dt=F32):
        return es.enter_context(nc.sbuf_tensor(name, list(shape), dt))

    def ps(name, shape, dt=F32):
        return es.enter_context(nc.psum_tensor(name, list(shape), dt))

    with es:
        NKT = max(TP // 128, PAST // 128 + 1)
        NTOK = NP * TP + NS * TS

        def PE(fn, r=(), w=()):
            return S.op("pe", fn, reads=r, writes=w)

        def ACT(fn, r=(), w=()):
            return S.op("act", fn, reads=r, writes=w)

        def DVE(fn, r=(), w=()):
            return S.op("dve", fn, reads=r, writes=w)

        def POOL(fn, r=(), w=()):
            return S.op("pool", fn, reads=r, writes=w)

        es_a = contextlib.ExitStack()

        def sba(name, shape, dt=F32):
            return es_a.enter_context(nc.sbuf_tensor(name, list(shape), dt))

        ident_f = sb("ident_f", [128, 128], F32)
        ident_b = sb("ident_b", [128, 128], BF16)
        tri_f = sba("tri_f", [128, 128], F32)
        up_f = sba("up_f", [128, 128], F32)
        B_c = Buf("consts")
        POOL(lambda e: e.memset(ident_f[:], 0.0), w=[B_c])
        POOL(lambda e: e.affine_select(out=ident_f[:], in_=ident_f[:], pattern=[[-1, 128]], compare_op=OP.not_equal,
                                       fill=1.0, base=0, channel_multiplier=1), r=[B_c], w=[B_c])
        POOL(lambda e: e.memset(tri_f[:], 1.0), w=[B_c])
        POOL(lambda e: e.affine_select(out=tri_f[:], in_=tri_f[:], pattern=[[1, 128]], compare_op=OP.is_ge,
                                       fill=0.0, base=0, channel_multiplier=-1), r=[B_c], w=[B_c])
        POOL(lambda e: e.memset(up_f[:], 1.0), w=[B_c])
        POOL(lambda e: e.affine_select(out=up_f[:], in_=up_f[:], pattern=[[-1, 128]], compare_op=OP.is_gt,
                                       fill=0.0, base=0, channel_multiplier=1), r=[B_c], w=[B_c])
        DVE(lambda e: e.tensor_copy(out=ident_b[:], in_=ident_f[:]), r=[B_c], w=[B_c])

        def bcast_load(name, src, width, alloc=None):
            t = (alloc or sba)(name, [128, width], F32)
            S.dma("sp", t[:], src.partition_broadcast(128), writes=[B_c])
            return t

        n1b = bcast_load("n1b", norm1, D)
        lpb = bcast_load("lpb", lam_params, 256)
        asub_b = bcast_load("asub_b", a_subln, 128)
        gn_b = bcast_load("gn_b", r_gnorm, 128)
        lb0 = bcast_load("lb0", r_lb[0:1, :], 512)
        lb1 = bcast_load("lb1", r_lb[1:2, :], 512)
        oml_b = sba("oml_b", [128, 512], F32)
        DVE(lambda e: e.tensor_tensor(out=lb0[:], in0=lb0[:], in1=lb1[:], op=OP.subtract), r=[B_c], w=[B_c])
        ACT(lambda e: e.activation(out=lb0[:], in_=lb0[:], func=AF.Sigmoid), r=[B_c], w=[B_c])
        DVE(lambda e: e.tensor_scalar(out=oml_b[:], in0=lb0[:], scalar1=-1.0, scalar2=1.0, op0=OP.mult, op1=OP.add),
            r=[B_c], w=[B_c])
        lb_b = lb0
        lam_s = sba("lam_s", [128, 4], F32)
        lam_j = sba("lam_j", [128, 64], F32)
        neg_lam = sba("neg_lam", [128, 1], F32)
        DVE(lambda e: e.scalar_tensor_tensor(out=lam_j[:], in0=lpb[:, 0:64], scalar=1.0, in1=lpb[:, 64:128],
                                             op0=OP.mult, op1=OP.mult, accum_out=lam_s[:, 0:1]), r=[B_c], w=[B_c])
        DVE(lambda e: e.scalar_tensor_tensor(out=lam_j[:], in0=lpb[:, 128:192], scalar=1.0, in1=lpb[:, 192:256],
                                             op0=OP.mult, op1=OP.mult, accum_out=lam_s[:, 1:2]), r=[B_c], w=[B_c])
        ACT(lambda e: e.activation(out=lam_s[:, 2:4], in_=lam_s[:, 0:2], func=AF.Exp), r=[B_c], w=[B_c])
        DVE(lambda e: e.tensor_tensor(out=neg_lam[:], in0=lam_s[:, 3:4], in1=lam_s[:, 2:3], op=OP.subtract), r=[B_c], w=[B_c])
        DVE(lambda e: e.tensor_scalar(out=neg_lam[:], in0=neg_lam[:], scalar1=-0.2, scalar2=None, op0=OP.add), r=[B_c], w=[B_c])
        DVE(lambda e: e.tensor_scalar(out=asub_b[:], in0=asub_b[:], scalar1=0.8, scalar2=None, op0=OP.mult), r=[B_c], w=[B_c])

        KSTOP = ""
        stage_ref = {}
        B_w = Buf("weights")
        kk = [0]

        def load_w(dst3, src2, nchunk, ncols):
            v = src2.rearrange("(c p) n -> p c n", p=128)
            for c in range(nchunk):
                for c0 in range(0, ncols, 1024):
                    w_ = min(1024, ncols - c0)
                    bi = kk[0] % 2
                    stage_bufs, B_stage = stage_ref["bufs"], stage_ref["B"]
                    S.dma("sp", stage_bufs[bi][:, :w_], v[:, c, c0:c0 + w_], writes=[B_stage[bi]])
                    if kk[0] % 2 == 0:
                        DVE(lambda e, bi=bi, c=c, c0=c0, w_=w_: e.tensor_copy(out=dst3[:, c, c0:c0 + w_], in_=stage_bufs[bi][:, :w_]),
                            r=[B_stage[bi]], w=[B_w])
                    else:
                        ACT(lambda e, bi=bi, c=c, c0=c0, w_=w_: e.copy(out=dst3[:, c, c0:c0 + w_], in_=stage_bufs[bi][:, :w_]),
                            r=[B_stage[bi]], w=[B_w])
                    kk[0] += 1

        pbank = [ps("pb%d" % i, [128, 512], F32) for i in range(8)]
        B_pb = [Buf("pb%d" % i, excl=True) for i in range(8)]

        def pb_bf(bk):
            return pbank[bk].bitcast(BF16)

        rr = [0]

        def next_bank():
            b_ = rr[0]
            rr[0] = (b_ + 1) % 4
            return b_

        NCH = cfg.nchunk
        ex_bf = nc.dram_tensor("ex_bf", [128, 128, 2 * D], BF16, kind="Internal").ap()
        B_pre = [Buf("pre%d" % i) for i in range(4)]
        pre_k = [0]

        def prepass(cnt):
            for _ in range(cnt):
                i_ = pre_k[0]
                if i_ >= 2 * NCH:
                    return
                pre_k[0] += 1
                c = i_ // 2
                if i_ % 2 == 0:
                    S.dma("pool", ex_bf[c][:, 0:D], p_downT[c], writes=[B_pre[i_ % 4]])
                else:
                    S.dma("pool", ex_bf[c][:, D:2 * D], p_up[c * 128:(c + 1) * 128, :], writes=[B_pre[i_ % 4]])

        tiles = []
        tok0 = 0
        for b in range(NP):
            for i in range(TP // 128):
                tiles.append(("p", b, i, 128, tok0))
                tok0 += 128
        for b in range(NS):
            tiles.append(("s", b, 0, TS, tok0))
            tok0 += TS

        def x_src(t):
            kind, b, i, n, _ = t
            if kind == "p":
                return x_p[b, i * 128:(i + 1) * 128, :]
            return x_s[b, :, :]

        with es_a:
            w_in_sb = sba("w_in_sb", [128, 8, IN_COLS], BF16)

            xt = [sba("xt%d" % i, [128, D], F32) for i in range(2)]
            B_xt = [Buf("xt%d" % i) for i in range(2)]
            stage_ref["bufs"], stage_ref["B"] = xt, B_xt
            load_w(w_in_sb, w_in, 8, IN_COLS)
            ss = sba("ss", [128, 1], F32)
            rstd = sba("rstd", [128, 1], F32)
            B_ss, B_rstd = Buf("ss"), Buf("rstd")
            xn = sba("xn", [128, D], BF16)
            B_xn = Buf("xn")
            junk, B_junk = xn, B_xn
            xnT = sba("xnT", [128, 8, 128], BF16)
            B_xnT = Buf("xnT")
            kout = [sba("kout0", [128, 512], F32)] * 2
            vout = [sba("vout0", [128, 512], F32)] * 2
            B_kout = [Buf("kout0")] * 2
            B_vout = [Buf("vout0")] * 2
            qa_bf = sba("qa_bf", [128, 512], BF16)
            ka_bf = sba("ka_bf", [128, 512], BF16)
            B_qa, B_ka = Buf("qa_bf"), Buf("ka_bf")
            QT = sba("QT", [128, 4, 128], BF16)
            B_QT = Buf("QT")
            KT = sba("KT", [128, 4, NKT * 128], BF16)
            B_KT = [Buf("KT%d" % j) for j in range(NKT)]
            Vaug = sba("Vaug", [128, NKT, 4, 130], BF16)
            B_V = [Buf("V%d" % j) for j in range(NKT)]
            PT = [sba("PT%d" % i, [128, 512], BF16) for i in range(2)]
            B_PT = [Buf("PT%d" % i) for i in range(2)]
            rz = sba("rz", [128, 2], F32)
            rz1n = sba("rz1n", [128, 1], F32)
            B_rz = Buf("rz")
            t1 = sba("t1", [128, 128], F32)
            B_t1 = Buf("t1")
            oa = sba("oa", [128, 4, 128], F32)
            B_oa = Buf("oa")
            midb = sba("midb", [128, 3072], BF16)
            hss = sba("hss", [128, 4], F32)
            B_hss = Buf("hss")
            oa_bf = midb[:, 0:512]
            B_oabf = Buf("oa_bf")
            sig = sba("sig", [128, 512], F32)
            logf = sba("logf", [128, 512], F32)
            kr = sba("kr", [128, 512], F32)
            kr_bf = sba("kr_bf", [128, 512], BF16)
            qr_bf = sba("qr_bf", [128, 512], BF16)
            iv_bf = sba("iv_bf", [128, 512], BF16)
            sog = sba("sog", [128, 512], BF16)
            sq = sig[:].rearrange("p (h v) -> p h v", h=4)
            B_sig, B_logf, B_kr, B_krbf, B_qr, B_iv, B_sog = (Buf(x) for x in ("sig", "logf", "kr", "krbf", "qr", "iv", "sog"))
            B_sq = B_sig
            ebmb = sba("ebmb", [128, 512], F32)
            k_out = sba("k_out", [128, 512], BF16)
            B_ebmb, B_kout2 = Buf("ebmb"), Buf("k_out")
            ebT = sba("ebT", [128, 4, 128], F32)
            enbT = sba("enbT", [128, 4, 128], F32)
            B_ebT, B_enbT = Buf("ebT"), Buf("enbT")
            q_inT = sba("q_inT", [128, 4, 128], BF16)
            k_inT = sba("k_inT", [128, 4, 128], BF16)
            B_qinT, B_kinT = Buf("q_inT"), Buf("k_inT")
            ATm = [sba("ATm%d" % i, [128, 128], BF16) for i in range(2)]
            B_ATm = [Buf("ATm%d" % i) for i in range(2)]
            Sst = sba("Sst", [128, 4, 128], F32)
            S_bf = sba("S_bf", [128, 4, 128], BF16)
            B_S = [Buf("S%d" % h) for h in range(4)]
            B_Sbf = [Buf("Sbf%d" % h) for h in range(4)]
            orr = sba("orr", [128, 4, 128], F32)
            B_orr = Buf("orr")
            orr_bf = midb[:, 512:1024]
            B_orrbf = Buf("orr_bf")
            sgA = midb[:, 1024:2048]
            sgB = midb[:, 2048:3072]
            B_sgA, B_sgB = Buf("sgA"), Buf("sgB")
            cst = [midb[:, 1024:2048].bitcast(F32), midb[:, 2048:3072].bitcast(F32)]
            B_cst = [B_sgA, B_sgB]
            dbg = sba("dbg", [128, D], F32) if cfg.stage == 2 else None
            B_dbg = Buf("dbg")
            mid = nc.dram_tensor("mid_scratch", [NTOK, 3072], BF16, kind="Internal").ap()
            B_mid = Buf("mid")

            if KSTOP == "weights":
                S.finish("sp"); S.eng["sp"].wait_ge(S.sem["dve"], S.cnt["dve"]); S.eng["sp"].wait_ge(S.sem["act"], S.cnt["act"]); S.eng["sp"].wait_ge(S.sem["pool"], S.cnt["pool"])
                return nc
            POOL(lambda e: e.memset(Vaug[:, :, :, 128:130], 1.0), w=B_V)

            if KSTOP == "vaug":
                S.finish("sp"); S.eng["sp"].wait_ge(S.sem["dve"], S.cnt["dve"]); S.eng["sp"].wait_ge(S.sem["act"], S.cnt["act"]); S.eng["sp"].wait_ge(S.sem["pool"], S.cnt["pool"])
                return nc
            S.dma("sp", xt[0][:tiles[0][3], :], x_src(tiles[0]), writes=[B_xt[0]])
            cstk = [0]

            def seq_start(kind, b):
                if kind == "p":
                    for h in range(4):
                        POOL(lambda e, h=h: e.memset(Sst[:, h, :], 0.0), w=[B_S[h]])
                        POOL(lambda e, h=h: e.memset(S_bf[:, h, :], 0.0), w=[B_Sbf[h]])
                    return
                S.dma("sp", Sst[:], st[b].rearrange("h d v -> d h v"), writes=B_S)
                for h in range(4):
                    ACT(lambda e, h=h: e.copy(out=S_bf[:, h, :], in_=Sst[:, h, :]), r=[B_S[h]], w=[B_Sbf[h]])
                for j in range(PAST // 128):
                    ci = cstk[0] % 2
                    cstk[0] += 1
                    S.dma("sp", cst[ci][:], ck[b, j * 128:(j + 1) * 128, :], writes=[B_cst[ci]])
                    bk = next_bank()
                    for h in range(4):
                        PE(lambda e, h=h, bk=bk, ci=ci: e.transpose(out=pbank[bk][:, h * 128:(h + 1) * 128],
                                                                    in_=cst[ci][:, h * 128:(h + 1) * 128], identity=ident_f[:]),
                           r=[B_cst[ci], B_c], w=[B_pb[bk]])
                    DVE(lambda e, bk=bk, j=j: e.tensor_copy(out=KT[:, :, j * 128:(j + 1) * 128],
                                                           in_=pbank[bk].rearrange("p (h t) -> p h t", h=4)),
                        r=[B_pb[bk]], w=[B_KT[j]])
                    ci = cstk[0] % 2
                    cstk[0] += 1
                    S.dma("sp", cst[ci][:], cv[b, j * 128:(j + 1) * 128, :], writes=[B_cst[ci]])
                    ACT(lambda e, j=j, ci=ci: e.copy(out=Vaug[:, j, :, 0:128], in_=cst[ci].rearrange("p (h v) -> p h v", h=4)),
                        r=[B_cst[ci]], w=[B_V[j]])

            def mixer_tile(ti, t):
                kind, b, i, n, tk0 = t
                cur = ti % 2
                if i == 0 and KSTOP != "noseq":
                    seq_start(kind, b)
                if ti + 1 < len(tiles):
                    nt = tiles[ti + 1]
                    S.dma("sp", xt[1 - cur][:nt[3], :], x_src(nt), writes=[B_xt[1 - cur]])
                X, BX = xt[cur], B_xt[cur]
                jn = i if kind == "p" else PAST // 128
                ACT(lambda e: e.activation(out=junk[:n, :], in_=X[:n, :], func=AF.Square, accum_out=ss[:n, :]),
                    r=[BX], w=[B_junk, B_ss])
                ACT(lambda e: e.activation(out=rstd[:n, :], in_=ss[:n, :], func=AF.Sqrt, scale=1.0 / D, bias=EPS),
                    r=[B_ss], w=[B_rstd])
                DVE(lambda e: e.reciprocal(out=rstd[:n, :], in_=rstd[:n, :]), r=[B_rstd], w=[B_rstd])
                DVE(lambda e: e.scalar_tensor_tensor(out=xn[:n, :], in0=X[:n, :], scalar=rstd[:n, :], in1=n1b[:n, :],
                                                     op0=OP.mult, op1=OP.mult), r=[BX, B_rstd, B_c], w=[B_xn])
                bk = next_bank()
                for c in range(8):
                    PE(lambda e, c=c, bk=bk: e.transpose(out=pb_bf(bk)[:, c * 128:c * 128 + n], in_=xn[:n, c * 128:(c + 1) * 128],
                                                         identity=ident_b[:n, :n]), r=[B_xn, B_c], w=[B_pb[bk]])
                DVE(lambda e, bk=bk: e.tensor_copy(out=xnT[:, :, :n], in_=pb_bf(bk).rearrange("p (c t) -> p c t", c=8)[:, :, :n]),
                    r=[B_pb[bk]], w=[B_xnT])

                def zgroup(g):
                    bk = next_bank()
                    for c in range(8):
                        PE(lambda e, c=c, bk=bk: e.matmul(pbank[bk][:n, :], lhsT=xnT[:, c, :n], rhs=w_in_sb[:, c, g * 512:(g + 1) * 512],
                                                          start=(c == 0), stop=(c == 7)), r=[B_xnT, B_w], w=[B_pb[bk]])
                    return bk

                def transpose4(src_bf, Bsrc, dst_fn, Bdst_list, evac_eng="dve", mul=None, Bmul=None):
                    bk = next_bank()
                    for h in range(4):
                        PE(lambda e, h=h, bk=bk: e.transpose(out=pb_bf(bk)[:, h * 128:h * 128 + n], in_=src_bf[:n, h * 128:(h + 1) * 128],
                                                             identity=ident_b[:n, :n]), r=[Bsrc, B_c], w=[B_pb[bk]])
                    src = pb_bf(bk)[:, 0:512].rearrange("p (h t) -> p h t", h=4)[:, :, :n]
                    if mul is not None:
                        DVE(lambda e: e.tensor_tensor(out=dst_fn(), in0=src, in1=mul, op=OP.mult), r=[B_pb[bk], Bmul], w=Bdst_list)
                    elif evac_eng == "dve":
                        DVE(lambda e: e.tensor_copy(out=dst_fn(), in_=src), r=[B_pb[bk]], w=Bdst_list)
                    else:
                        ACT(lambda e: e.copy(out=dst_fn(), in_=src), r=[B_pb[bk]], w=Bdst_list)

                if KSTOP == "t_xnT":
                    return
                bk = zgroup(0)
                ACT(lambda e, bk=bk: e.copy(out=qa_bf[:n, :], in_=pbank[bk][:n, :]), r=[B_pb[bk]], w=[B_qa])
                transpose4(qa_bf, B_qa, lambda: QT[:, :, :n], [B_QT])
                if KSTOP == "t_g0":
                    return
                bk = zgroup(1)
                ACT(lambda e, bk=bk: e.copy(out=kout[cur][:n, :], in_=pbank[bk][:n, :]), r=[B_pb[bk]], w=[B_kout[cur]])
                DVE(lambda e, bk=bk: e.tensor_copy(out=ka_bf[:n, :], in_=kout[cur][:n, :]), r=[B_kout[cur]], w=[B_ka])
                dst = k_p[b, i * 128:(i + 1) * 128, :] if kind == "p" else k_s[b, :, :]
                S.dma("sp", dst, kout[cur][:n, :], reads=[B_kout[cur]], sb=B_kout[cur], is_output=True)
                transpose4(ka_bf, B_ka, lambda: KT[:, :, jn * 128:jn * 128 + n], [B_KT[jn]])
                if KSTOP == "t_g1":
                    return
                bk = zgroup(2)
                DVE(lambda e, bk=bk: e.tensor_copy(out=vout[cur][:n, :], in_=pbank[bk][:n, :]), r=[B_pb[bk]], w=[B_vout[cur]])
                ACT(lambda e, bk=bk: e.copy(out=Vaug[:n, jn, :, 0:128], in_=pbank[bk].rearrange("p (h v) -> p h v", h=4)[:n]),
                    r=[B_pb[bk]], w=[B_V[jn]])
                dst = v_p[b, i * 128:(i + 1) * 128, :] if kind == "p" else v_s[b, :, :]
                S.dma("sp", dst, vout[cur][:n, :], reads=[B_vout[cur]], sb=B_vout[cur], is_output=True)
                if cfg.stage <= 1:
                    return
                bk = zgroup(3)
                ACT(lambda e, bk=bk: e.activation(out=sig[:n, :], in_=pbank[bk][:n, :], func=AF.Sigmoid), r=[B_pb[bk]], w=[B_sig])
                DVE(lambda e: e.tensor_tensor(out=sig[:n, :], in0=sig[:n, :], in1=oml_b[:n, :], op=OP.mult), r=[B_sig, B_c], w=[B_sig])
                DVE(lambda e: e.tensor_tensor(out=sig[:n, :], in0=sig[:n, :], in1=lb_b[:n, :], op=OP.add), r=[B_sig, B_c], w=[B_sig])
                ACT(lambda e: e.activation(out=logf[:n, :], in_=sig[:n, :], func=AF.Ln), r=[B_sig], w=[B_logf])
                DVE(lambda e: e.tensor_scalar(out=kr[:n, :], in0=sig[:n, :], scalar1=-1.0, scalar2=1.0, op0=OP.mult, op1=OP.add),
                    r=[B_sig], w=[B_kr])
                DVE(lambda e: e.tensor_copy(out=kr_bf[:n, :], in_=kr[:n, :]), r=[B_kr], w=[B_krbf])
                bk = zgroup(4)
                ACT(lambda e, bk=bk: e.activation(out=qr_bf[:n, :], in_=pbank[bk][:n, :], func=AF.Silu), r=[B_pb[bk]], w=[B_qr])
                bk = zgroup(5)
                DVE(lambda e, bk=bk: e.tensor_copy(out=iv_bf[:n, :], in_=pbank[bk][:n, :]), r=[B_pb[bk]], w=[B_iv])
                bk = zgroup(6)
                ACT(lambda e, bk=bk: e.activation(out=sog[:n, :], in_=pbank[bk][:n, :], func=AF.Silu), r=[B_pb[bk]], w=[B_sog])
                for gi in range(2):
                    bk = zgroup(7 + gi)
                    ACT(lambda e, bk=bk, gi=gi: e.activation(out=sgA[:n, gi * 512:(gi + 1) * 512], in_=pbank[bk][:n, :], func=AF.Sigmoid),
                        r=[B_pb[bk]], w=[B_sgA])
                for gi in range(2):
                    bk = zgroup(9 + gi)
                    ACT(lambda e, bk=bk, gi=gi: e.activation(out=sgB[:n, gi * 512:(gi + 1) * 512], in_=pbank[bk][:n, :], func=AF.Sigmoid),
                        r=[B_pb[bk]], w=[B_sgB])

                if kind == "p":
                    ktiles = [(j, 128) for j in range(i + 1)]
                else:
                    ktiles = [(j, 128) for j in range(PAST // 128)] + [(PAST // 128, n)]
                groups = []
                g_ = []
                for (j, kn) in ktiles:
                    if kn < 128 and g_:
                        groups.append(g_)
                        g_ = []
                    g_.append((j, kn))
                    if len(g_) == 4:
                        groups.append(g_)
                        g_ = []
                if g_:
                    groups.append(g_)
                items = []
                for h in range(4):
                    for m in range(2):
                        for gi, grp in enumerate(groups):
                            items.append((h, m, gi, grp))

                def emit_scores(k):
                    h, m, gi, grp = items[k]
                    ps_ = slice(m * 64, (m + 1) * 64)
                    sbk = 4 + (k % 2)
                    pt = k % 2
                    for jj, (j, kn) in enumerate(grp):
                        PE(lambda e, jj=jj, j=j, kn=kn, sbk=sbk, ps_=ps_, h=h: e.matmul(
                            pbank[sbk][:kn, jj * 128:jj * 128 + n], lhsT=KT[ps_, h, j * 128:j * 128 + kn],
                            rhs=QT[ps_, h, :n], start=True, stop=True),
                           r=[B_KT[j], B_QT], w=[B_pb[sbk]])
                    knmax = max(kn for _, kn in grp)
                    nb_ = len(grp)
                    ACT(lambda e, sbk=sbk, pt=pt, knmax=knmax, nb_=nb_: e.activation(
                        out=PT[pt].rearrange("p (j t) -> p j t", t=128)[:knmax, :nb_, :n],
                        in_=pbank[sbk].rearrange("p (j t) -> p j t", t=128)[:knmax, :nb_, :n], func=AF.Exp, scale=0.125),
                        r=[B_pb[sbk]], w=[B_PT[pt]])
                    for jj, (j, kn) in enumerate(grp):
                        if kind == "p" and j == i:
                            DVE(lambda e, jj=jj, pt=pt: e.memset(PT[pt][64:128, jj * 128:jj * 128 + 64], 0.0),
                                r=[B_PT[pt]], w=[B_PT[pt]])

                def emit_pv(k):
                    h, m, gi, grp = items[k]
                    ob = 6 + (h % 2)
                    pt = k % 2
                    for jj, (j, kn) in enumerate(grp):
                        first = (gi == 0 and jj == 0)
                        last = (gi == len(groups) - 1 and jj == len(grp) - 1)
                        PE(lambda e, jj=jj, j=j, kn=kn, pt=pt, first=first, last=last, ob=ob, m=m, h=h: e.matmul(
                            pbank[ob][:n, m * 256:m * 256 + 129], lhsT=PT[pt][:kn, jj * 128:jj * 128 + n],
                            rhs=Vaug[:kn, j, h, 0:129], start=first, stop=last),
                           r=[B_PT[pt], B_V[j]], w=[B_pb[ob]])
                    if m == 1 and gi == len(groups) - 1:
                        ov = pbank[ob].rearrange("p (m c) -> p m c", c=256)
                        DVE(lambda e, ov=ov: e.reciprocal(out=rz[:n, :], in_=ov[:n, :, 128]), r=[B_pb[ob]], w=[B_rz])
                        DVE(lambda e: e.tensor_tensor(out=rz1n[:n, :], in0=rz[:n, 1:2], in1=neg_lam[:n, :], op=OP.mult), r=[B_rz, B_c], w=[B_rz])
                        DVE(lambda e, ob=ob: e.tensor_scalar(out=t1[:n, :], in0=pbank[ob][:n, 256:384], scalar1=rz1n[:n, :], scalar2=None,
                                                            op0=OP.mult), r=[B_pb[ob], B_rz], w=[B_t1])
                        DVE(lambda e, ob=ob, h=h: e.scalar_tensor_tensor(out=oa[:n, h, :], in0=pbank[ob][:n, 0:128], scalar=rz[:n, 0:1],
                                                                         in1=t1[:n, :], op0=OP.mult, op1=OP.add),
                            r=[B_pb[ob], B_rz, B_t1], w=[B_oa])


                def head_norm(src, Bsrc, gain_b, dst_bf, Bdst, extra=None, Bextra=None):
                    DVE(lambda e: e.tensor_tensor(out=sq[:n], in0=src[:n], in1=src[:n], op=OP.mult), r=[Bsrc], w=[B_sq])
                    DVE(lambda e: e.tensor_reduce(out=hss[:n, :], in_=sq[:n], axis=AX.X, op=OP.add), r=[B_sq], w=[B_hss])
                    ACT(lambda e: e.activation(out=hss[:n, :], in_=hss[:n, :], func=AF.Sqrt, scale=1.0 / 128, bias=EPS), r=[B_hss], w=[B_hss])
                    DVE(lambda e: e.reciprocal(out=hss[:n, :], in_=hss[:n, :]), r=[B_hss], w=[B_hss])
                    DVE(lambda e: e.tensor_tensor(out=sq[:n], in0=src[:n], in1=hss[:n, :].unsqueeze(2).to_broadcast([n, 4, 128]), op=OP.mult),
                        r=[Bsrc, B_hss], w=[B_sq])
                    if extra is None:
                        DVE(lambda e: e.tensor_tensor(out=dst_bf[:n, :].rearrange("p (h v) -> p h v", h=4), in0=sq[:n],
                                                      in1=gain_b[:n, :].unsqueeze(1).to_broadcast([n, 4, 128]), op=OP.mult),
                            r=[B_sq, B_c], w=[Bdst])
                    else:
                        DVE(lambda e: e.tensor_tensor(out=sq[:n], in0=sq[:n], in1=gain_b[:n, :].unsqueeze(1).to_broadcast([n, 4, 128]), op=OP.mult),
                            r=[B_sq, B_c], w=[B_sq])
                        DVE(lambda e: e.tensor_tensor(out=dst_bf[:n, :], in0=sq[:n].rearrange("p h v -> p (h v)"), in1=extra[:n, :], op=OP.mult),
                            r=[B_sq, Bextra], w=[Bdst])

                def hgrn_gen():
                    bk = next_bank()
                    PE(lambda e, bk=bk: e.matmul(pbank[bk][:n, :], lhsT=up_f[:n, :n], rhs=logf[:n, :], start=True, stop=True),
                       r=[B_c, B_logf], w=[B_pb[bk]])
                    ACT(lambda e, bk=bk: e.activation(out=ebmb[:n, :], in_=pbank[bk][:n, :], func=AF.Exp), r=[B_pb[bk]], w=[B_ebmb])
                    DVE(lambda e: e.tensor_tensor(out=k_out[:n, :], in0=kr[:n, :], in1=ebmb[:n, :], op=OP.mult), r=[B_kr, B_ebmb], w=[B_kout2])
                    yield
                    bk = next_bank()
                    for h in range(4):
                        PE(lambda e, h=h, bk=bk: e.matmul(pbank[bk][:, h * 128:h * 128 + n], lhsT=logf[:n, h * 128:(h + 1) * 128],
                                                          rhs=tri_f[:n, :n], start=True, stop=True), r=[B_logf, B_c], w=[B_pb[bk]])
                    bTv = pbank[bk].rearrange("p (h t) -> p h t", h=4)[:, :, :n]
                    ACT(lambda e: e.activation(out=ebT[:, :, :n], in_=bTv, func=AF.Exp), r=[B_pb[bk]], w=[B_ebT])
                    ACT(lambda e: e.activation(out=enbT[:, :, :n], in_=bTv, func=AF.Exp, scale=-1.0), r=[B_pb[bk]], w=[B_enbT])
                    yield
                    transpose4(qr_bf, B_qr, lambda: q_inT[:, :, :n], [B_qinT], mul=ebT[:, :, :n], Bmul=B_ebT)
                    yield
                    transpose4(kr_bf, B_krbf, lambda: k_inT[:, :, :n], [B_kinT], mul=enbT[:, :, :n], Bmul=B_enbT)
                    yield
                    for h in range(4):
                        bk = next_bank()
                        am = h % 2
                        hs_ = slice(h * 128, (h + 1) * 128)
                        PE(lambda e, h=h, bk=bk: e.matmul(pbank[bk][:n, 0:n], lhsT=k_inT[:, h, :n], rhs=q_inT[:, h, :n], start=True, stop=True),
                           r=[B_kinT, B_qinT], w=[B_pb[bk]])
                        DVE(lambda e, bk=bk, am=am: e.tensor_tensor(out=ATm[am][:n, :n], in0=pbank[bk][:n, 0:n], in1=tri_f[:n, :n], op=OP.mult),
                            r=[B_pb[bk], B_c], w=[B_ATm[am]])
                        yield
                        PE(lambda e, h=h, bk=bk, am=am: e.matmul(pbank[bk][:n, 128:256], lhsT=ATm[am][:n, :n], rhs=iv_bf[:n, hs_], start=True, stop=False),
                           r=[B_ATm[am], B_iv], w=[B_pb[bk]])
                        PE(lambda e, h=h, bk=bk: e.matmul(pbank[bk][:n, 128:256], lhsT=q_inT[:, h, :n], rhs=S_bf[:, h, :], start=False, stop=True),
                           r=[B_qinT, B_Sbf[h]], w=[B_pb[bk]])
                        PE(lambda e, h=h, bk=bk: e.matmul(pbank[bk][:, 256:384], lhsT=k_out[:n, hs_], rhs=iv_bf[:n, hs_], start=True, stop=True),
                           r=[B_kout2, B_iv], w=[B_pb[bk]])
                        ACT(lambda e, h=h, bk=bk: e.copy(out=orr[:n, h, :], in_=pbank[bk][:n, 128:256]), r=[B_pb[bk]], w=[B_orr])
                        DVE(lambda e, h=h, bk=bk: e.scalar_tensor_tensor(out=Sst[:, h, :], in0=Sst[:, h, :], scalar=ebT[:, h, n - 1:n],
                                                                         in1=pbank[bk][:, 256:384], op0=OP.mult, op1=OP.add),
                            r=[B_S[h], B_ebT, B_pb[bk]], w=[B_S[h]])
                        DVE(lambda e, h=h: e.tensor_copy(out=S_bf[:, h, :], in_=Sst[:, h, :]), r=[B_S[h]], w=[B_Sbf[h]])
                hg = hgrn_gen()
                emit_scores(0)
                for k in range(len(items)):
                    if k + 1 < len(items):
                        emit_scores(k + 1)
                    emit_pv(k)
                    next(hg, None)
                head_norm(oa, B_oa, asub_b, oa_bf, B_oabf)
                for _ in hg:
                    pass
                head_norm(orr, B_orr, gn_b, orr_bf, B_orrbf, extra=sog, Bextra=B_sog)
                last_of_seq = (kind == "s") or (i == TP // 128 - 1)
                if last_of_seq:
                    dsts = s_p[b] if kind == "p" else s_s[b]
                    S.dma("sp", dsts.rearrange("h d v -> d h v"), Sst[:], reads=B_S, sb=B_S[0], is_output=True)

                if cfg.stage == 2:
                    DVE(lambda e: e.tensor_copy(out=dbg[:n, 0:512], in_=oa_bf[:n, :]), r=[B_oabf], w=[B_dbg])
                    DVE(lambda e: e.tensor_copy(out=dbg[:n, 512:1024], in_=orr_bf[:n, :]), r=[B_orrbf], w=[B_dbg])
                    dst = y_p[b, i * 128:(i + 1) * 128, :] if kind == "p" else y_s[b, :, :]
                    S.dma("sp", dst, dbg[:n, :], reads=[B_dbg], sb=B_dbg, is_output=True)
                S.dma("sp", mid[tk0:tk0 + n, :], midb[:n, :], reads=[B_oabf, B_orrbf, B_sgA, B_sgB], writes=[B_mid], sb=B_oabf)

            for ti, t in enumerate(tiles):
                prepass(4)
                mixer_tile(ti, t)
            prepass(2 * NCH)
        S.barrier()

        if cfg.stage < 3:
            S.finish("sp")
            print("ninst", S.ninst, S.cnt)
            return nc

        btiles = []
        for b in range(NP):
            for i in range(TP // 128):
                r0 = (b * (TP // 128) + i) * 128
                btiles.append((r0, 128, x_p[b, i * 128:(i + 1) * 128, :], y_p[b, i * 128:(i + 1) * 128, :]))
        xs_flat = x_s.rearrange("b t d -> (b t) d")
        ys_flat = y_s.rearrange("b t d -> (b t) d")
        for r in range(0, NS * TS, 128):
            n_ = min(128, NS * TS - r)
            btiles.append((NP * TP + r, n_, xs_flat[r:r + n_, :], ys_flat[r:r + n_, :]))
        h2s = nc.dram_tensor("h2_scratch", [NTOK, D], F32, kind="Internal").ap()
        B_h2s = Buf("h2s")

        es_b = contextlib.ExitStack()

        def sbb(name, shape, dt=F32):
            return es_b.enter_context(nc.sbuf_tensor(name, list(shape), dt))

        with es_b:
            xt2 = [sbb("xt2_%d" % i, [128, D], F32) for i in range(2)]
            B_xt2 = [Buf("xt2_%d" % i) for i in range(2)]
            stage_ref["bufs"], stage_ref["B"] = xt2, B_xt2
            w_a_sb = sbb("w_a_sb", [128, 4, D], BF16)
            w_b_sb = sbb("w_b_sb", [128, 4, D], BF16)
            w_out_sb = sbb("w_out_sb", [128, 8, D], BF16)
            load_w(w_a_sb, w_a, 4, D)
            load_w(w_b_sb, w_b, 4, D)
            load_w(w_out_sb, w_out, 8, D)
            midt = [sbb("midt%d" % i, [128, 3072], BF16) for i in range(2)]
            B_midt = [Buf("midt%d" % i) for i in range(2)]
            oT = sbb("oT", [128, 8, 128], BF16)
            B_oT = Buf("oT")
            ua = sbb("ua", [128, D], F32)
            ub = sbb("ub", [128, D], F32)
            B_ua, B_ub = Buf("ua"), Buf("ub")
            u_bf = sbb("u_bf", [128, D], BF16)
            B_ubf = Buf("u_bf")
            uT = sbb("uT", [128, 8, 128], BF16)
            B_uT = Buf("uT")

            def a2_load(k):
                r0, n, xsrc, _ = btiles[k]
                c_ = k % 2
                S.dma("sp", midt[c_][:n, :], mid[r0:r0 + n, :], reads=[B_mid], writes=[B_midt[c_]])
                S.dma("sp", xt2[c_][:n, :], xsrc, writes=[B_xt2[c_]])

            a2_load(0)
            for k, (r0, n, xsrc, _) in enumerate(btiles):
                c_ = k % 2
                if k + 1 < len(btiles):
                    a2_load(k + 1)
                M, BM = midt[c_], B_midt[c_]
                tb = 4 + (k % 2)
                for c in range(8):
                    PE(lambda e, c=c: e.transpose(out=pb_bf(tb)[:, c * 128:c * 128 + n], in_=M[:n, c * 128:(c + 1) * 128],
                                                  identity=ident_b[:n, :n]), r=[BM, B_c], w=[B_pb[tb]])
                DVE(lambda e: e.tensor_copy(out=oT[:, :, :n], in_=pb_bf(tb).rearrange("p (c t) -> p c t", c=8)[:, :, :n]),
                    r=[B_pb[tb]], w=[B_oT])
                for half in range(2):
                    hs_ = slice(half * 512, (half + 1) * 512)
                    for c in range(4):
                        PE(lambda e, c=c, half=half, hs_=hs_: e.matmul(pbank[half][:n, :], lhsT=oT[:, c, :n], rhs=w_a_sb[:, c, hs_],
                                                                       start=(c == 0), stop=(c == 3)), r=[B_oT, B_w], w=[B_pb[half]])
                    for c in range(4):
                        PE(lambda e, c=c, half=half, hs_=hs_: e.matmul(pbank[2 + half][:n, :], lhsT=oT[:, 4 + c, :n], rhs=w_b_sb[:, c, hs_],
                                                                       start=(c == 0), stop=(c == 3)), r=[B_oT, B_w], w=[B_pb[2 + half]])
                for half in range(2):
                    hs_ = slice(half * 512, (half + 1) * 512)
                    DVE(lambda e, half=half, hs_=hs_: e.tensor_tensor(out=ua[:n, hs_], in0=pbank[half][:n, :],
                                                                      in1=M[:n, 1024 + half * 512:1024 + (half + 1) * 512], op=OP.mult),
                        r=[B_pb[half], BM], w=[B_ua])
                    DVE(lambda e, half=half, hs_=hs_: e.tensor_tensor(out=ub[:n, hs_], in0=pbank[2 + half][:n, :],
                                                                      in1=M[:n, 2048 + half * 512:2048 + (half + 1) * 512], op=OP.mult),
                        r=[B_pb[2 + half], BM], w=[B_ub])
                DVE(lambda e: e.tensor_tensor(out=u_bf[:n, :], in0=ua[:n, :], in1=ub[:n, :], op=OP.add), r=[B_ua, B_ub], w=[B_ubf])
                tb2 = 6 + (k % 2)
                for c in range(8):
                    PE(lambda e, c=c: e.transpose(out=pb_bf(tb2)[:, c * 128:c * 128 + n], in_=u_bf[:n, c * 128:(c + 1) * 128],
                                                  identity=ident_b[:n, :n]), r=[B_ubf, B_c], w=[B_pb[tb2]])
                ACT(lambda e: e.copy(out=uT[:, :, :n], in_=pb_bf(tb2).rearrange("p (c t) -> p c t", c=8)[:, :, :n]),
                    r=[B_pb[tb2]], w=[B_uT])
                for half in range(2):
                    hs_ = slice(half * 512, (half + 1) * 512)
                    for c in range(8):
                        PE(lambda e, c=c, half=half, hs_=hs_: e.matmul(pbank[half][:n, :], lhsT=uT[:, c, :n], rhs=w_out_sb[:, c, hs_],
                                                                       start=(c == 0), stop=(c == 7)), r=[B_uT, B_w], w=[B_pb[half]])
                    DVE(lambda e, half=half, hs_=hs_: e.tensor_tensor(out=xt2[c_][:n, hs_], in0=pbank[half][:n, :], in1=xt2[c_][:n, hs_], op=OP.add),
                        r=[B_pb[half], B_xt2[c_]], w=[B_xt2[c_]])
                S.dma("sp", h2s[r0:r0 + n, :], xt2[c_][:n, :], reads=[B_xt2[c_]], writes=[B_h2s], sb=B_xt2[c_])

        S.barrier()
        if cfg.stage < 4:
            S.finish("sp")
            print("ninst", S.ninst, S.cnt)
            return nc

        es_c = contextlib.ExitStack()

        def sbc(name, shape, dt=F32):
            return es_c.enter_context(nc.sbuf_tensor(name, list(shape), dt))

        TPB = 2
        TSUB = 8
        with es_c:
            n2b = bcast_load("n2b", norm2, D, alloc=sbc)
            fnb = bcast_load("fnb", final_norm, D, alloc=sbc)
            iota_i = sbc("iota_i", [128, 128], I32)
            iota_f = sbc("iota_f", [128, 128], F32)
            bmask = sbc("bmask", [128, 8], F32)
            POOL(lambda e: e.iota(out=iota_i[:], pattern=[[1, 128]], base=0, channel_multiplier=0), w=[B_c])
            POOL(lambda e: e.memset(bmask[:], 1.0), w=[B_c])
            POOL(lambda e: e.affine_select(out=bmask[:], in_=bmask[:], pattern=[[-16, 8]], compare_op=OP.is_ge, fill=0.0,
                                           base=0, channel_multiplier=1), r=[B_c], w=[B_c])
            POOL(lambda e: e.affine_select(out=bmask[:], in_=bmask[:], pattern=[[16, 8]], compare_op=OP.is_ge, fill=0.0,
                                           base=15, channel_multiplier=-1), r=[B_c], w=[B_c])
            DVE(lambda e: e.tensor_copy(out=iota_f[:], in_=iota_i[:]), r=[B_c], w=[B_c])

            GT_sb = sbc("GT_sb", [128, TPB * 128, 128], BF16)
            B_GT = [Buf("GT%d" % j) for j in range(TPB)]
            h2 = [sbc("h2_%d" % j, [128, D], F32) for j in range(TPB)]
            B_h2 = [Buf("h2_%d" % j) for j in range(TPB)]
            stage_ref["bufs"], stage_ref["B"] = h2, B_h2
            p_wq_sb = sbc("p_wq_sb", [128, 8, 2048], BF16)
            load_w(p_wq_sb, p_wq, 8, 2048)
            keysT = sbc("keysT", [128, 16, 128], BF16)
            kst = GT_sb[:, 0:32, :].bitcast(F32).rearrange("p (g k) d -> p g (k d)", g=16)
            S.dma("sp", kst, p_keys.rearrange("g k d -> k g d"), writes=[B_GT[0]])
            for g4 in range(4):
                bk = g4
                for gg in range(4):
                    g = g4 * 4 + gg
                    PE(lambda e, g=g, gg=gg, bk=bk: e.transpose(out=pbank[bk][:, gg * 128:(gg + 1) * 128], in_=kst[:, g, :],
                                                                 identity=ident_f[:]), r=[B_GT[0], B_c], w=[B_pb[bk]])
                DVE(lambda e, g4=g4, bk=bk: e.tensor_copy(out=keysT[:, g4 * 4:(g4 + 1) * 4, :],
                                                          in_=pbank[bk].rearrange("p (g k) -> p g k", g=4)), r=[B_pb[bk]], w=[B_w])
            xn2Ts = [sbc("xn2T_%d" % i, [128, 8, TPB * 128], BF16) for i in range(2)]
            B_xn2Ts = [[Buf("xn2T%d_%d" % (i, j)) for j in range(TPB)] for i in range(2)]
            xn2 = sbc("xn2", [128, D], BF16)
            B_xn2 = Buf("xn2")
            ss2 = sbc("ss2", [128, 1], F32)
            rstd2 = sbc("rstd2", [128, 1], F32)
            B_ss2, B_rstd2 = Buf("ss2"), Buf("rstd2")
            q_bf = sbc("q_bf", [128, 2048], BF16)
            B_q = Buf("q_bf")
            qT = sbc("qT", [128, 16, 128], BF16)
            B_qT = Buf("qT")
            s_sb = sbc("s_sb", [128, 2048], F32)
            B_s = Buf("s_sb")
            comb = sbc("comb", [128, 2048], F32)
            B_comb = Buf("comb")
            W_bf = sbc("W_bf", [128, 2048], BF16)
            B_W = Buf("W_bf")
            sv = sbc("sv", [128, 256], F32)
            si_u = sbc("si_u", [128, 256], U32)
            B_sv, B_siu = Buf("sv"), Buf("si_u")
            si01 = sbc("si01", [128, 2, 128], BF16)
            B_si01 = Buf("si01")
            work = sbc("work", [128, 128], F32)
            work2 = sbc("work2", [128, 256], F32)
            B_work, B_work2 = Buf("work"), Buf("work2")
            m8a = sbc("m8a", [128, 8, 8], F32)
            m8b = sbc("m8b", [128, 8, 8], F32)
            negm = sbc("negm", [128, 8], F32)
            Zs = sbc("Zs", [128, 8], F32)
            B_m8 = Buf("m8")
            B_Z = Buf("Z")
            WT2 = [sbc("WT_sb%d" % j, [128, 16, 128], BF16) for j in range(TPB)]
            B_WT2 = [Buf("WT%d" % j) for j in range(TPB)]
            WB4D = True
            bmask_b = sbc("bmask_b", [128, 8], BF16)
            DVE(lambda e: e.tensor_copy(out=bmask_b[:], in_=bmask[:]), r=[B_c], w=[B_c])
            siT2 = [sbc("siT%d" % j, [128, 2, 128], BF16) for j in range(TPB)]
            iota_b = sbc("iota_b", [128, 128], BF16)
            DVE(lambda e: e.tensor_copy(out=iota_b[:], in_=iota_f[:]), r=[B_c], w=[B_c])
            B_siT2 = [Buf("siT%d" % j) for j in range(TPB)]
            A0 = [sbc("A0_%d" % i, [128, TSUB, 128], BF16) for i in range(2)]
            B0 = [sbc("B0_%d" % i, [128, TSUB, 128], BF16) for i in range(2)]
            Wblk = [sbc("Wblk%d" % i, [128, TSUB, 128], BF16) for i in range(2)]
            B_A0 = [Buf("A0_%d" % i) for i in range(2)]
            B_B0 = [Buf("B0_%d" % i) for i in range(2)]
            B_Wblk = [Buf("Wblk%d" % i) for i in range(2)]
            R_sb = sbc("R_sb", [128, TSUB, 128], BF16)
            B_R = [Buf("R%d" % i) for i in range(TSUB // 4)]
            NRING = 4
            exb = [sbc("exb%d" % i, [128, 2 * D], BF16) for i in range(NRING)]
            dn = [t[:, 0:D].rearrange("p (a b) -> p a b", a=8) for t in exb]
            upc = [t[:, D:2 * D] for t in exb]
            B_dn = [Buf("ex%d" % i) for i in range(NRING)]
            B_up = B_dn
            gsb = [sbc("gsb%d" % i, [128, TPB * 128], F32) for i in range(3)]
            gh = [sbc("gh%d" % i, [128, TPB * 128], BF16) for i in range(3)]
            B_g = [Buf("g%d" % i) for i in range(3)]
            B_gh = [Buf("gh%d" % i) for i in range(3)]
            yt, B_yt = comb[:, 0:D], B_comb
            junk2, B_junk2 = q_bf[:, 0:D], B_q

            r8 = [0]

            def nb8():
                b_ = r8[0]
                r8[0] = (b_ + 1) % 8
                return b_

            rt = [0]

            def nbt():
                return 4

            ec = [0]
            sub_k = [0]

            def tok_major(j, r0, n, xs):
                xn2T, siT, WT_sb, B_siT, B_WT = xn2Ts[xs], siT2[j], WT2[j], B_siT2[j], B_WT2[j]
                H2, BH2 = h2[j], B_h2[j]
                S.dma("sp", H2[:n, :], h2s[r0:r0 + n, :], reads=[B_h2s], writes=[BH2])
                ACT(lambda e: e.activation(out=junk2[:n, :], in_=H2[:n, :], func=AF.Square, accum_out=ss2[:n, :]),
                    r=[BH2], w=[B_junk2, B_ss2])
                ACT(lambda e: e.activation(out=rstd2[:n, :], in_=ss2[:n, :], func=AF.Sqrt, scale=1.0 / D, bias=EPS),
                    r=[B_ss2], w=[B_rstd2])
                DVE(lambda e: e.reciprocal(out=rstd2[:n, :], in_=rstd2[:n, :]), r=[B_rstd2], w=[B_rstd2])
                DVE(lambda e: e.scalar_tensor_tensor(out=xn2[:n, :], in0=H2[:n, :], scalar=rstd2[:n, :], in1=n2b[:n, :],
                                                     op0=OP.mult, op1=OP.mult), r=[BH2, B_rstd2, B_c], w=[B_xn2])
                bk = nbt()
                for c in range(8):
                    PE(lambda e, c=c, bk=bk: e.transpose(out=pb_bf(bk)[:, c * 128:c * 128 + n], in_=xn2[:n, c * 128:(c + 1) * 128],
                                                         identity=ident_b[:n, :n]), r=[B_xn2, B_c], w=[B_pb[bk]])
                DVE(lambda e, bk=bk: e.tensor_copy(out=xn2T[:, :, j * 128:j * 128 + n],
                                                   in_=pb_bf(bk).rearrange("p (c t) -> p c t", c=8)[:, :, :n]),
                    r=[B_pb[bk]], w=[B_xn2Ts[xs][j]])
                yield
                for g in range(4):
                    bk = nbt()
                    for c in range(8):
                        PE(lambda e, c=c, bk=bk, g=g: e.matmul(pbank[bk][:n, :], lhsT=xn2T[:, c, j * 128:j * 128 + n],
                                                               rhs=p_wq_sb[:, c, g * 512:(g + 1) * 512], start=(c == 0), stop=(c == 7)),
                           r=[B_xn2Ts[xs][j], B_w], w=[B_pb[bk]])
                    if g % 2 == 0:
                        ACT(lambda e, bk=bk, g=g: e.copy(out=q_bf[:n, g * 512:(g + 1) * 512], in_=pbank[bk][:n, :]), r=[B_pb[bk]], w=[B_q])
                    else:
                        DVE(lambda e, bk=bk, g=g: e.tensor_copy(out=q_bf[:n, g * 512:(g + 1) * 512], in_=pbank[bk][:n, :]), r=[B_pb[bk]], w=[B_q])
                    yield
                for g2 in range(2):
                    bk = nbt()
                    for c in range(8):
                        hc = g2 * 8 + c
                        PE(lambda e, c=c, hc=hc, bk=bk: e.transpose(out=pb_bf(bk)[:, c * 128:c * 128 + n], in_=q_bf[:n, hc * 128:(hc + 1) * 128],
                                                                    identity=ident_b[:n, :n]), r=[B_q, B_c], w=[B_pb[bk]])
                    if g2 == 0:
                        ACT(lambda e, bk=bk, g2=g2: e.copy(out=qT[:, g2 * 8:(g2 + 1) * 8, :n],
                                                           in_=pb_bf(bk).rearrange("p (c t) -> p c t", c=8)[:, :, :n]), r=[B_pb[bk]], w=[B_qT])
                    else:
                        DVE(lambda e, bk=bk, g2=g2: e.tensor_copy(out=qT[:, g2 * 8:(g2 + 1) * 8, :n],
                                                                  in_=pb_bf(bk).rearrange("p (c t) -> p c t", c=8)[:, :, :n]), r=[B_pb[bk]], w=[B_qT])
                    yield
                for g4 in range(4):
                    bk = nbt()
                    for gg in range(4):
                        hc = g4 * 4 + gg
                        PE(lambda e, hc=hc, gg=gg, bk=bk: e.matmul(pbank[bk][:n, gg * 128:(gg + 1) * 128], lhsT=qT[:, hc, :n], rhs=keysT[:, hc, :],
                                                                   start=True, stop=True), r=[B_qT, B_w], w=[B_pb[bk]])
                    if g4 % 2 == 0:
                        ACT(lambda e, bk=bk, g4=g4: e.copy(out=s_sb[:n, g4 * 512:(g4 + 1) * 512], in_=pbank[bk][:n, :]), r=[B_pb[bk]], w=[B_s])
                    else:
                        DVE(lambda e, bk=bk, g4=g4: e.tensor_copy(out=s_sb[:n, g4 * 512:(g4 + 1) * 512], in_=pbank[bk][:n, :]), r=[B_pb[bk]], w=[B_s])
                    yield
                for hc in range(16):
                    grp = s_sb[:n, hc * 128:(hc + 1) * 128]
                    o8a = slice(hc * 16, hc * 16 + 8)
                    o8b = slice(hc * 16 + 8, hc * 16 + 16)
                    DVE(lambda e, grp=grp, o8a=o8a: e.max(out=sv[:n, o8a], in_=grp), r=[B_s], w=[B_sv])
                    DVE(lambda e, grp=grp, o8a=o8a: e.max_index(out=si_u[:n, o8a], in_max=sv[:n, o8a], in_values=grp), r=[B_s, B_sv], w=[B_siu])
                    DVE(lambda e, grp=grp, o8a=o8a: e.match_replace(out=work[:n, :], in_to_replace=sv[:n, o8a], in_values=grp, imm_value=-1e30),
                        r=[B_s, B_sv], w=[B_work])
                    DVE(lambda e, o8b=o8b: e.max(out=sv[:n, o8b], in_=work[:n, :]), r=[B_work], w=[B_sv])
                    DVE(lambda e, o8b=o8b: e.max_index(out=si_u[:n, o8b], in_max=sv[:n, o8b], in_values=work[:n, :]), r=[B_work, B_sv], w=[B_siu])
                    yield
                siv = si_u[:n, :].rearrange("p (h c a) -> p c h a", h=8, c=2)
                for c2 in range(2):
                    DVE(lambda e, c2=c2: e.tensor_copy(out=si01[:n, c2, :].rearrange("p (h a) -> p h a", h=8), in_=siv[:, c2]),
                        r=[B_siu], w=[B_si01])
                svv = sv[:n, :].rearrange("p (h c a) -> p h c a", h=8, c=2)
                DVE(lambda e: e.tensor_tensor(out=comb[:n, :].rearrange("p (h a b) -> p h a b", h=8, a=16),
                                              in0=svv[:, :, 0, :].unsqueeze(3).to_broadcast([n, 8, 16, 16]),
                                              in1=svv[:, :, 1, :].unsqueeze(2).to_broadcast([n, 8, 16, 16]), op=OP.add),
                    r=[B_sv], w=[B_comb])
                yield
                for h in range(8):
                    cg = comb[:n, h * 256:(h + 1) * 256]
                    DVE(lambda e, cg=cg, h=h: e.max(out=m8a[:n, h, :], in_=cg), r=[B_comb], w=[B_m8])
                    DVE(lambda e, cg=cg, h=h: e.match_replace(out=work2[:n, :], in_to_replace=m8a[:n, h, :], in_values=cg, imm_value=-1e30),
                        r=[B_comb, B_m8], w=[B_work2])
                    DVE(lambda e, h=h: e.max(out=m8b[:n, h, :], in_=work2[:n, :]), r=[B_work2], w=[B_m8])
                    yield
                DVE(lambda e: e.tensor_scalar(out=negm[:n, :], in0=m8a[:n, :, 0], scalar1=-1.0, scalar2=None, op0=OP.mult), r=[B_m8], w=[B_m8])
                for h in range(8):
                    cg = comb[:n, h * 256:(h + 1) * 256]
                    eg = s_sb[:n, h * 256:(h + 1) * 256]
                    ACT(lambda e, cg=cg, eg=eg, h=h: e.activation(out=eg, in_=cg, func=AF.Exp, bias=negm[:n, h:h + 1], scale=1.0),
                        r=[B_comb, B_m8], w=[B_s])
                    DVE(lambda e, cg=cg, eg=eg, h=h: e.scalar_tensor_tensor(out=eg, in0=cg, scalar=m8b[:n, h, 7:8], in1=eg,
                                                                            op0=OP.is_ge, op1=OP.mult), r=[B_comb, B_m8, B_s], w=[B_s])
                    yield
                DVE(lambda e: e.tensor_reduce(out=Zs[:n, :], in_=s_sb[:n, :].rearrange("p (h x) -> p h x", h=8), axis=AX.X, op=OP.add),
                    r=[B_s], w=[B_Z])
                DVE(lambda e: e.reciprocal(out=Zs[:n, :], in_=Zs[:n, :]), r=[B_Z], w=[B_Z])
                DVE(lambda e: e.tensor_tensor(out=W_bf[:n, :].rearrange("p (h x) -> p h x", h=8),
                                              in0=s_sb[:n, :].rearrange("p (h x) -> p h x", h=8),
                                              in1=Zs[:n, :].unsqueeze(2).to_broadcast([n, 8, 256]), op=OP.mult), r=[B_s, B_Z], w=[B_W])
                yield
                bk = nbt()
                for c2 in range(2):
                    PE(lambda e, c2=c2, bk=bk: e.transpose(out=pb_bf(bk)[:, c2 * 128:c2 * 128 + n], in_=si01[:n, c2, :], identity=ident_b[:n, :n]),
                       r=[B_si01, B_c], w=[B_pb[bk]])
                ACT(lambda e, bk=bk: e.copy(out=siT[:, :, :n], in_=pb_bf(bk)[:, 0:256].rearrange("p (c t) -> p c t", c=2)[:, :, :n]),
                    r=[B_pb[bk]], w=[B_siT])
                yield
                Wv = W_bf[:n, :].rearrange("p (ha b) -> p ha b", b=16)
                for g2 in range(2):
                    bk = nbt()
                    for bb in range(8):
                        b_ = g2 * 8 + bb
                        PE(lambda e, bb=bb, b_=b_, bk=bk: e.transpose(out=pb_bf(bk)[:, bb * 128:bb * 128 + n], in_=Wv[:, :, b_],
                                                                      identity=ident_b[:n, :n]), r=[B_W, B_c], w=[B_pb[bk]])
                    if g2 == 0:
                        ACT(lambda e, bk=bk, g2=g2: e.copy(out=WT_sb[:, g2 * 8:(g2 + 1) * 8, :n],
                                                           in_=pb_bf(bk).rearrange("p (c t) -> p c t", c=8)[:, :, :n]), r=[B_pb[bk]], w=[B_WT])
                    else:
                        DVE(lambda e, bk=bk, g2=g2: e.tensor_copy(out=WT_sb[:, g2 * 8:(g2 + 1) * 8, :n],
                                                                  in_=pb_bf(bk).rearrange("p (c t) -> p c t", c=8)[:, :, :n]), r=[B_pb[bk]], w=[B_WT])
                    yield
            def slot_phase(j, n):
                siT, WT_sb, B_siT, B_WT = siT2[j], WT2[j], B_siT2[j], B_WT2[j]
                for t0 in range(0, n, TSUB):
                    tn = min(TSUB, n - t0)
                    sk = sub_k[0] % 2
                    sub_k[0] += 1
                    DVE(lambda e, sk=sk, t0=t0, tn=tn: e.tensor_tensor(
                        out=A0[sk][:, :tn, :], in0=iota_b[:].unsqueeze(1).to_broadcast([128, tn, 128]),
                        in1=siT[:, 0, t0:t0 + tn].unsqueeze(2).to_broadcast([128, tn, 128]), op=OP.is_equal),
                        r=[B_siT, B_c], w=[B_A0[sk]])
                    DVE(lambda e, sk=sk, t0=t0, tn=tn: e.tensor_tensor(
                        out=B0[sk][:, :tn, :], in0=iota_b[:].unsqueeze(1).to_broadcast([128, tn, 128]),
                        in1=siT[:, 1, t0:t0 + tn].unsqueeze(2).to_broadcast([128, tn, 128]), op=OP.is_equal),
                        r=[B_siT, B_c], w=[B_B0[sk]])
                    if WB4D:
                        DVE(lambda e, sk=sk, t0=t0, tn=tn: e.tensor_tensor(
                            out=Wblk[sk][:, :tn, :].rearrange("p t (h b) -> p t h b", h=8),
                            in0=WT_sb[:, :, t0:t0 + tn].rearrange("p b t -> p t b").unsqueeze(2).to_broadcast([128, tn, 8, 16]),
                            in1=bmask_b[:, :].unsqueeze(1).unsqueeze(3).to_broadcast([128, tn, 8, 16]), op=OP.mult),
                            r=[B_WT, B_c], w=[B_Wblk[sk]])
                    else:
                        for hp in range(8):
                            if hp % 2 == 0:
                                DVE(lambda e, sk=sk, t0=t0, tn=tn, hp=hp: e.tensor_scalar(
                                    out=Wblk[sk][:, :tn, hp * 16:(hp + 1) * 16], in0=WT_sb[:, :, t0:t0 + tn].rearrange("p b t -> p t b"),
                                    scalar1=bmask[:, hp:hp + 1], scalar2=None, op0=OP.mult), r=[B_WT, B_c], w=[B_Wblk[sk]])
                            else:
                                ACT(lambda e, sk=sk, t0=t0, tn=tn, hp=hp: e.activation(
                                    out=Wblk[sk][:, :tn, hp * 16:(hp + 1) * 16], in_=WT_sb[:, :, t0:t0 + tn].rearrange("p b t -> p t b"),
                                    func=AF.Copy, scale=bmask[:, hp:hp + 1]), r=[B_WT, B_c], w=[B_Wblk[sk]])
                    def emit_R(q0, sk=sk, tn=tn):
                        w4 = min(4, tn - q0)
                        BR = B_R[q0 // 4]
                        bk = nb8()
                        for tt in range(q0, q0 + w4):
                            PE(lambda e, tt=tt, q0=q0, bk=bk, sk=sk: e.matmul(pbank[bk][:, (tt - q0) * 128:(tt - q0 + 1) * 128], lhsT=Wblk[sk][:, tt, :],
                                                                              rhs=A0[sk][:, tt, :], start=True, stop=True),
                               r=[B_Wblk[sk], B_A0[sk]], w=[B_pb[bk]])
                        ACT(lambda e, bk=bk, q0=q0, w4=w4: e.copy(out=R_sb[:, q0:q0 + w4, :],
                                                                   in_=pbank[bk][:, :w4 * 128].rearrange("p (t i) -> p t i", i=128)),
                            r=[B_pb[bk]], w=[BR])

                    def emit_GT(q0, sk=sk, tn=tn, t0=t0):
                        w4 = min(4, tn - q0)
                        BR = B_R[q0 // 4]
                        bk2 = nb8()
                        for tt in range(q0, q0 + w4):
                            PE(lambda e, tt=tt, q0=q0, bk2=bk2, sk=sk: e.matmul(pbank[bk2][:, (tt - q0) * 128:(tt - q0 + 1) * 128], lhsT=B0[sk][:, tt, :],
                                                                                rhs=R_sb[:, tt, :], start=True, stop=True),
                               r=[B_B0[sk], BR], w=[B_pb[bk2]])
                        g0 = j * 128 + t0 + q0
                        ACT(lambda e, bk2=bk2, g0=g0, w4=w4: e.copy(out=GT_sb[:, g0:g0 + w4, :],
                                                                     in_=pbank[bk2][:, :w4 * 128].rearrange("p (t i) -> p t i", i=128)),
                            r=[B_pb[bk2]], w=[B_GT[j]])

                    q0s = list(range(0, tn, 4))
                    for gi, q0 in enumerate(q0s):
                        emit_R(q0)
                        if gi > 0:
                            emit_GT(q0s[gi - 1])
                    emit_GT(q0s[-1])

            def drain(g):
                for _ in g:
                    pass

            def tok_chain(blk, xs):
                for j, (r0, n, _, _) in enumerate(blk):
                    yield from tok_major(j, r0, n, xs)

            blocks = [btiles[k0:k0 + TPB] for k0 in range(0, len(btiles), TPB)]
            drain(tok_chain(blocks[0], 0))
            for bi, blk in enumerate(blocks):
                xs = bi % 2
                xn2T = xn2Ts[xs]
                for j, (r0, n, _, _) in enumerate(blk):
                    slot_phase(j, n)
                span = (len(blk) - 1) * 128 + blk[-1][1]
                BXT = [B_xn2Ts[xs][j] for j in range(len(blk))]
                BGT = [B_GT[j] for j in range(len(blk))]
                nxt = tok_chain(blocks[bi + 1], 1 - xs) if bi + 1 < len(blocks) else iter(())

                def emit_hid(c, xn2T=xn2T, span=span, BXT=BXT, BGT=BGT):
                    k3 = ec[0] % NRING
                    e2 = ec[0] % 3
                    ec[0] += 1
                    S.dma("sp", exb[k3][:], ex_bf[c], reads=B_pre, writes=[B_dn[k3]])
                    hb = (6, 7, 5)[e2]
                    for dh in range(8):
                        PE(lambda e, dh=dh, hb=hb, k3=k3: e.matmul(pbank[hb][:, :span], lhsT=dn[k3][:, dh, :], rhs=xn2T[:, dh, :span],
                                                                   start=(dh == 0), stop=(dh == 7)), r=[B_dn[k3]] + BXT, w=[B_pb[hb]])
                    ACT(lambda e, hb=hb, e2=e2: e.activation(out=gsb[e2][:, :span], in_=pbank[hb][:, :span], func=AF.Gelu),
                        r=[B_pb[hb]], w=[B_g[e2]])
                    DVE(lambda e, e2=e2, c=c: e.tensor_tensor(out=gh[e2][:, :span], in0=gsb[e2][:, :span], in1=GT_sb[:, :span, c], op=OP.mult),
                        r=[B_g[e2]] + BGT, w=[B_gh[e2]])
                    return (c, k3, e2)

                def emit_out(st_, blk=blk):
                    c, k3, e2 = st_
                    for j, (r0, n, _, _) in enumerate(blk):
                        for half in range(2):
                            ob = 2 * j + half
                            PE(lambda e, j=j, n=n, half=half, ob=ob, e2=e2, k3=k3, c=c: e.matmul(
                                pbank[ob][:n, :], lhsT=gh[e2][:, j * 128:j * 128 + n], rhs=upc[k3][:, half * 512:(half + 1) * 512],
                                start=(c == 0), stop=(c == NCH - 1)), r=[B_gh[e2], B_up[k3]], w=[B_pb[ob]])

                pend = []
                for c in range(NCH):
                    pend.append(emit_hid(c))
                    next(nxt, None)
                    if len(pend) > 2:
                        emit_out(pend.pop(0))
                for st_ in pend:
                    emit_out(st_)
                drain(nxt)
                for j, (r0, n, _, ydst) in enumerate(blk):
                    H2, BH2 = h2[j], B_h2[j]
                    S.dma("sp", H2[:n, :], h2s[r0:r0 + n, :], reads=[B_h2s], writes=[BH2])
                    for half in range(2):
                        ob = 2 * j + half
                        hs_ = slice(half * 512, (half + 1) * 512)
                        DVE(lambda e, ob=ob, hs_=hs_, H2=H2, n=n: e.tensor_tensor(out=H2[:n, hs_], in0=pbank[ob][:n, :], in1=H2[:n, hs_], op=OP.add),
                            r=[B_pb[ob], BH2], w=[BH2])
                    ACT(lambda e, H2=H2, n=n: e.activation(out=junk2[:n, :], in_=H2[:n, :], func=AF.Square, accum_out=ss2[:n, :]),
                        r=[BH2], w=[B_junk2, B_ss2])
                    ACT(lambda e, n=n: e.activation(out=rstd2[:n, :], in_=ss2[:n, :], func=AF.Sqrt, scale=1.0 / D, bias=EPS),
                        r=[B_ss2], w=[B_rstd2])
                    DVE(lambda e, n=n: e.reciprocal(out=rstd2[:n, :], in_=rstd2[:n, :]), r=[B_rstd2], w=[B_rstd2])
                    DVE(lambda e, H2=H2, n=n: e.scalar_tensor_tensor(out=yt[:n, :], in0=H2[:n, :], scalar=rstd2[:n, :], in1=fnb[:n, :],
                                                                     op0=OP.mult, op1=OP.mult), r=[BH2, B_rstd2, B_c], w=[B_yt])
                    S.dma("sp", ydst, yt[:n, :], reads=[B_yt], sb=B_yt, is_output=True)
        S.finish("sp")
    print("ninst", S.ninst, S.cnt)
    return nc


_OUT_NAMES = ["y_prompt", "y_sample", "k_prompt", "v_prompt", "s_prompt", "k_sample", "v_sample", "s_sample"]


def make_in_maps(inputs, cfg, ncores):
    f = lambda a: np.ascontiguousarray(np.asarray(a, dtype=np.float32))
    shared = {"p_downT": f(np.asarray(inputs["p_down"], dtype=np.float32)[0].reshape(128, 128, 8, 128).transpose(0, 3, 2, 1)).reshape(128, 128, D)}
    maps = []
    for c in range(ncores):
        ps_ = slice(c * cfg.nseq_p, (c + 1) * cfg.nseq_p)
        ss_ = slice(c * cfg.nseq_s, (c + 1) * cfg.nseq_s)
        m = {
            "x_prompt": f(inputs["x_prompt"][ps_, :cfg.t_p]),
            "x_sample": f(inputs["x_sample"][ss_]),
            "cache_k": f(inputs["cache_k"][0, ss_]).reshape(cfg.nseq_s, cfg.past, 512),
            "cache_v": f(inputs["cache_v"][0, ss_]).reshape(cfg.nseq_s, cfg.past, 512),
            "state_hgrn": f(inputs["state_hgrn"][0, ss_]),
            "norm1": f(inputs["norm1"]).reshape(1, D),
            "w_in": f(inputs["w_in"][0]),
            "lam_params": f(inputs["lam_params"]).reshape(1, 256),
            "a_subln": f(inputs["a_subln"]).reshape(1, 128),
            "r_lb_logits": f(inputs["r_lb_logits"]),
            "r_gnorm": f(inputs["r_gnorm"]).reshape(1, 128),
            "w_a": f(inputs["w_a"][0]),
            "w_b": f(inputs["w_b"][0]),
            "w_out": f(inputs["w_out"][0]),
            "norm2": f(inputs["norm2"]).reshape(1, D),
            "p_wq": f(inputs["p_wq"][0]),
            "p_keys": f(inputs["p_keys"][0]).reshape(16, 128, 128),
            "p_downT": shared["p_downT"],
            "p_up": f(inputs["p_up"][0]),
            "final_norm": f(inputs["final_norm"]).reshape(1, D),
        }
        maps.append(m)
    return maps


def run(inputs, cfg, ncores):
    nc = build(cfg)
    maps = make_in_maps(inputs, cfg, ncores)
    res = run_bass_kernel_spmd(nc, maps, core_ids=list(range(ncores)))
    outs = []
    for name in _OUT_NAMES:
        parts = [np.asarray(r[name]) for r in res.results]
        outs.append(np.concatenate(parts, axis=0))
    return outs


def kernel(**inputs):
    cfg = Cfg()
    yp, ys, kp, vp, sp_, ks, vs, ss_ = run(inputs, cfg, NCORES)
    B, T = 32, cfg.t_p
    return (yp.reshape(B, T, D), ys.reshape(32, cfg.t_s, D),
            kp.reshape(1, B, T, 4, 128), vp.reshape(1, B, T, 4, 128), sp_.reshape(1, B, 4, 128, 128),
            ks.reshape(1, 32, cfg.t_s, 4, 128), vs.reshape(1, 32, cfg.t_s, 4, 128), ss_.reshape(1, 32, 4, 128, 128))
```

```python
import math
import numpy as np
import concourse.bass as bass
import concourse.mybir as mybir
from concourse.bass_utils import run_bass_kernel_spmd

F32 = mybir.dt.float32
BF16 = mybir.dt.bfloat16
I32 = mybir.dt.int32
U32 = mybir.dt.uint32
AF = mybir.ActivationFunctionType
OP = mybir.AluOpType
AX = mybir.AxisListType

D = 1024
EPS = 1e-6
NCORES = 8
IN_COLS = 5632


class Cfg:
    def __init__(self, nseq_p=4, t_p=2048, nseq_s=4, t_s=32, past=1024, stage=99, nchunk=128):
        self.nseq_p, self.t_p, self.nseq_s, self.t_s, self.past = nseq_p, t_p, nseq_s, t_s, past
        self.stage = stage
        self.nchunk = nchunk


class Buf:
    __slots__ = ("name", "w", "r", "dsem", "dcnt", "excl")

    def __init__(self, name="", excl=False):
        self.name = name
        self.excl = excl
        self.w = {}
        self.r = {}
        self.dsem = None
        self.dcnt = 0


class Sched:
    ENGS = ("pe", "act", "dve", "pool", "sp")

    def __init__(self, nc):
        self.nc = nc
        self.eng = dict(pe=nc.tensor, act=nc.scalar, dve=nc.vector, pool=nc.gpsimd, sp=nc.sync)
        self.sem = {e: nc.alloc_semaphore("c_" + e) for e in self.ENGS}
        self.cnt = {e: 0 for e in self.ENGS}
        self.seen = {e: {} for e in self.ENGS}
        self.dsems = {}
        self.ninst = 0
        self.out_tokens = {}

    def _semof(self, key):
        if key in self.sem:
            return self.sem[key]
        return self.dsems[key][0]

    def _cur(self, key, val):
        if key in self.dsems:
            return max(val, self.dsems[key][1].dcnt * 16)
        return val

    def _wait(self, eng, deps):
        e = self.eng[eng]
        for key, val in deps.items():
            if key == eng and eng == "pe":
                continue
            val = self._cur(key, val)
            if self.seen[eng].get(key, 0) >= val:
                continue
            e.wait_ge(self._semof(key), val)
            self.seen[eng][key] = val
            self.ninst += 1

    @staticmethod
    def _merge(deps, d):
        for k, v in d.items():
            if deps.get(k, 0) < v:
                deps[k] = v

    def _deps(self, reads, writes, eng=None):
        deps = {}
        for b in reads:
            self._merge(deps, b.w)
            if b.excl:
                self._merge(deps, {k: v for k, v in b.r.items() if k != eng})
        for b in writes:
            self._merge(deps, b.w)
            self._merge(deps, b.r)
        return deps

    def op(self, eng, fn, reads=(), writes=()):
        self._wait(eng, self._deps(reads, writes, eng))
        ins = fn(self.eng[eng])
        self.cnt[eng] += 1
        n = self.cnt[eng]
        ins.then_inc(self.sem[eng], 1)
        self.ninst += 1
        for b in reads:
            if b.r.get(eng, 0) < n:
                b.r[eng] = n
        for b in writes:
            b.w = {eng: n}
            b.r = {}
        return ins

    def dma(self, eng, out, in_, reads=(), writes=(), sb=None, fn=None, is_output=False):
        if sb is None:
            sb = writes[0] if writes else reads[0]
        if sb.dsem is None:
            sb.dsem = self.nc.alloc_semaphore("d_%d" % len(self.dsems))
            self.dsems[id(sb)] = (sb.dsem, sb)
        key = id(sb)
        self._wait(eng, self._deps(reads, writes))
        if fn is None:
            ins = self.eng[eng].dma_start(out=out, in_=in_)
        else:
            ins = fn(self.eng[eng])
        sb.dcnt += 1
        val = sb.dcnt * 16
        ins.then_inc(sb.dsem, 16)
        self.ninst += 1
        for b in reads:
            b.r[key] = val
        for b in writes:
            b.w = {key: val}
            b.r = {}
        if is_output:
            self.out_tokens[key] = val
        return ins

    def barrier(self):
        deps = {key: b.dcnt * 16 for key, (sem, b) in self.dsems.items()}
        for k in self.ENGS:
            if self.cnt[k]:
                deps[k] = self.cnt[k]
        for eng in self.ENGS:
            self._wait(eng, {k: v for k, v in deps.items() if k != eng})

    def finish(self, eng="sp"):
        deps = {key: b.dcnt * 16 for key, (sem, b) in self.dsems.items()}
        for k in ("pe", "act", "dve", "pool"):
            if self.cnt[k]:
                deps[k] = self.cnt[k]
        self._wait(eng, deps)


def build(cfg):
    nc = bass.Bass("TRN2", target_bir_lowering=False)
    NP, TP, NS, TS, PAST = cfg.nseq_p, cfg.t_p, cfg.nseq_s, cfg.t_s, cfg.past
    S = Sched(nc)

    def din(name, shape, dt=F32):
        return nc.dram_tensor(name, list(shape), dt, kind="ExternalInput").ap()

    def dout(name, shape, dt=F32):
        return nc.dram_tensor(name, list(shape), dt, kind="ExternalOutput").ap()

    x_p = din("x_prompt", [NP, TP, D])
    x_s = din("x_sample", [NS, TS, D])
    ck = din("cache_k", [NS, PAST, 512])
    cv = din("cache_v", [NS, PAST, 512])
    st = din("state_hgrn", [NS, 4, 128, 128])
    norm1 = din("norm1", [1, D])
    w_in = din("w_in", [D, IN_COLS])
    lam_params = din("lam_params", [1, 256])
    a_subln = din("a_subln", [1, 128])
    r_lb = din("r_lb_logits", [2, 512])
    r_gnorm = din("r_gnorm", [1, 128])
    w_a = din("w_a", [512, D])
    w_b = din("w_b", [512, D])
    w_out = din("w_out", [D, D])
    norm2 = din("norm2", [1, D])
    p_wq = din("p_wq", [D, 2048])
    p_keys = din("p_keys", [16, 128, 128])
    p_downT = din("p_downT", [128, 128, D])
    p_up = din("p_up", [16384, D])
    final_norm = din("final_norm", [1, D])

    y_p = dout("y_prompt", [NP, TP, D])
    y_s = dout("y_sample", [NS, TS, D])
    k_p = dout("k_prompt", [NP, TP, 512])
    v_p = dout("v_prompt", [NP, TP, 512])
    s_p = dout("s_prompt", [NP, 4, 128, 128])
    k_s = dout("k_sample", [NS, TS, 512])
    v_s = dout("v_sample", [NS, TS, 512])
    s_s = dout("s_sample", [NS, 4, 128, 128])

    import contextlib
    es = contextlib.ExitStack()

    def sb(name, shape, dt=F32):
        return es.enter_context(nc.sbuf_tensor(name, list(shape), dt))

    def ps(name, shape, dt=F32):
        return es.enter_context(nc.psum_tensor(name, list(shape), dt))

    with es:
        NKT = max(TP // 128, PAST // 128 + 1)
        NTOK = NP * TP + NS * TS

        def PE(fn, r=(), w=()):
            return S.op("pe", fn, reads=r, writes=w)

        def ACT(fn, r=(), w=()):
            return S.op("act", fn, reads=r, writes=w)

        def DVE(fn, r=(), w=()):
            return S.op("dve", fn, reads=r, writes=w)

        def POOL(fn, r=(), w=()):
            return S.op("pool", fn, reads=r, writes=w)

        es_a = contextlib.ExitStack()

        def sba(name, shape, dt=F32):
            return es_a.enter_context(nc.sbuf_tensor(name, list(shape), dt))

        ident_f = sb("ident_f", [128, 128], F32)
        ident_b = sb("ident_b", [128, 128], BF16)
        tri_f = sba("tri_f", [128, 128], F32)
        up_f = sba("up_f", [128, 128], F32)
        B_c = Buf("consts")
        POOL(lambda e: e.memset(ident_f[:], 0.0), w=[B_c])
        POOL(lambda e: e.affine_select(out=ident_f[:], in_=ident_f[:], pattern=[[-1, 128]], compare_op=OP.not_equal,
                                       fill=1.0, base=0, channel_multiplier=1), r=[B_c], w=[B_c])
        POOL(lambda e: e.memset(tri_f[:], 1.0), w=[B_c])
        POOL(lambda e: e.affine_select(out=tri_f[:], in_=tri_f[:], pattern=[[1, 128]], compare_op=OP.is_ge,
                                       fill=0.0, base=0, channel_multiplier=-1), r=[B_c], w=[B_c])
        POOL(lambda e: e.memset(up_f[:], 1.0), w=[B_c])
        POOL(lambda e: e.affine_select(out=up_f[:], in_=up_f[:], pattern=[[-1, 128]], compare_op=OP.is_gt,
                                       fill=0.0, base=0, channel_multiplier=1), r=[B_c], w=[B_c])
        DVE(lambda e: e.tensor_copy(out=ident_b[:], in_=ident_f[:]), r=[B_c], w=[B_c])

        def bcast_load(name, src, width, alloc=None):
            t = (alloc or sba)(name, [128, width], F32)
            S.dma("sp", t[:], src.partition_broadcast(128), writes=[B_c])
            return t

        n1b = bcast_load("n1b", norm1, D)
        lpb = bcast_load("lpb", lam_params, 256)
        asub_b = bcast_load("asub_b", a_subln, 128)
        gn_b = bcast_load("gn_b", r_gnorm, 128)
        lb0 = bcast_load("lb0", r_lb[0:1, :], 512)
        lb1 = bcast_load("lb1", r_lb[1:2, :], 512)
        oml_b = sba("oml_b", [128, 512], F32)
        DVE(lambda e: e.tensor_tensor(out=lb0[:], in0=lb0[:], in1=lb1[:], op=OP.subtract), r=[B_c], w=[B_c])
        ACT(lambda e: e.activation(out=lb0[:], in_=lb0[:], func=AF.Sigmoid), r=[B_c], w=[B_c])
        DVE(lambda e: e.tensor_scalar(out=oml_b[:], in0=lb0[:], scalar1=-1.0, scalar2=1.0, op0=OP.mult, op1=OP.add),
            r=[B_c], w=[B_c])
        lb_b = lb0
        lam_s = sba("lam_s", [128, 4], F32)
        lam_j = sba("lam_j", [128, 64], F32)
        neg_lam = sba("neg_lam", [128, 1], F32)
        DVE(lambda e: e.scalar_tensor_tensor(out=lam_j[:], in0=lpb[:, 0:64], scalar=1.0, in1=lpb[:, 64:128],
                                             op0=OP.mult, op1=OP.mult, accum_out=lam_s[:, 0:1]), r=[B_c], w=[B_c])
        DVE(lambda e: e.scalar_tensor_tensor(out=lam_j[:], in0=lpb[:, 128:192], scalar=1.0, in1=lpb[:, 192:256],
                                             op0=OP.mult, op1=OP.mult, accum_out=lam_s[:, 1:2]), r=[B_c], w=[B_c])
        ACT(lambda e: e.activation(out=lam_s[:, 2:4], in_=lam_s[:, 0:2], func=AF.Exp), r=[B_c], w=[B_c])
        DVE(lambda e: e.tensor_tensor(out=neg_lam[:], in0=lam_s[:, 3:4], in1=lam_s[:, 2:3], op=OP.subtract), r=[B_c], w=[B_c])
        DVE(lambda e: e.tensor_scalar(out=neg_lam[:], in0=neg_lam[:], scalar1=-0.2, scalar2=None, op0=OP.add), r=[B_c], w=[B_c])
        DVE(lambda e: e.tensor_scalar(out=asub_b[:], in0=asub_b[:], scalar1=0.8, scalar2=None, op0=OP.mult), r=[B_c], w=[B_c])

        KSTOP = ""
        stage_ref = {}
        B_w = Buf("weights")
        kk = [0]

        def load_w(dst3, src2, nchunk, ncols):
            v = src2.rearrange("(c p) n -> p c n", p=128)
            for c in range(nchunk):
                for c0 in range(0, ncols, 1024):
                    w_ = min(1024, ncols - c0)
                    bi = kk[0] % 2
                    stage_bufs, B_stage = stage_ref["bufs"], stage_ref["B"]
                    S.dma("sp", stage_bufs[bi][:, :w_], v[:, c, c0:c0 + w_], writes=[B_stage[bi]])
                    if kk[0] % 2 == 0:
                        DVE(lambda e, bi=bi, c=c, c0=c0, w_=w_: e.tensor_copy(out=dst3[:, c, c0:c0 + w_], in_=stage_bufs[bi][:, :w_]),
                            r=[B_stage[bi]], w=[B_w])
                    else:
                        ACT(lambda e, bi=bi, c=c, c0=c0, w_=w_: e.copy(out=dst3[:, c, c0:c0 + w_], in_=stage_bufs[bi][:, :w_]),
                            r=[B_stage[bi]], w=[B_w])
                    kk[0] += 1

        pbank = [ps("pb%d" % i, [128, 512], F32) for i in range(8)]
        B_pb = [Buf("pb%d" % i, excl=True) for i in range(8)]

        def pb_bf(bk):
            return pbank[bk].bitcast(BF16)

        rr = [0]

        def next_bank():
            b_ = rr[0]
            rr[0] = (b_ + 1) % 4
            return b_

        NCH = cfg.nchunk
        dnT_bf = nc.dram_tensor("dnT_bf", [128, 128, D], BF16, kind="Internal").ap()
        up_bf = nc.dram_tensor("up_bf", [16384, D], BF16, kind="Internal").ap()
        B_pre = [Buf("pre%d" % i) for i in range(4)]
        pre_k = [0]

        def prepass(cnt):
            for _ in range(cnt):
                i_ = pre_k[0]
                if i_ >= 2 * NCH:
                    return
                pre_k[0] += 1
                c = i_ // 2
                if i_ % 2 == 0:
                    S.dma("pool", dnT_bf[c], p_downT[c], writes=[B_pre[i_ % 4]])
                else:
                    S.dma("pool", up_bf[c * 128:(c + 1) * 128, :], p_up[c * 128:(c + 1) * 128, :], writes=[B_pre[i_ % 4]])

        tiles = []
        tok0 = 0
        for b in range(NP):
            for i in range(TP // 128):
                tiles.append(("p", b, i, 128, tok0))
                tok0 += 128
        for b in range(NS):
            tiles.append(("s", b, 0, TS, tok0))
            tok0 += TS

        def x_src(t):
            kind, b, i, n, _ = t
            if kind == "p":
                return x_p[b, i * 128:(i + 1) * 128, :]
            return x_s[b, :, :]

        with es_a:
            w_in_sb = sba("w_in_sb", [128, 8, IN_COLS], BF16)

            xt = [sba("xt%d" % i, [128, D], F32) for i in range(2)]
            B_xt = [Buf("xt%d" % i) for i in range(2)]
            stage_ref["bufs"], stage_ref["B"] = xt, B_xt
            load_w(w_in_sb, w_in, 8, IN_COLS)
            ss = sba("ss", [128, 1], F32)
            rstd = sba("rstd", [128, 1], F32)
            B_ss, B_rstd = Buf("ss"), Buf("rstd")
            xn = sba("xn", [128, D], BF16)
            B_xn = Buf("xn")
            junk, B_junk = xn, B_xn
            xnT = sba("xnT", [128, 8, 128], BF16)
            B_xnT = Buf("xnT")
            kout = [sba("kout0", [128, 512], F32)] * 2
            vout = [sba("vout0", [128, 512], F32)] * 2
            B_kout = [Buf("kout0")] * 2
            B_vout = [Buf("vout0")] * 2
            qa_bf = sba("qa_bf", [128, 512], BF16)
            ka_bf = sba("ka_bf", [128, 512], BF16)
            B_qa, B_ka = Buf("qa_bf"), Buf("ka_bf")
            QT = sba("QT", [128, 4, 128], BF16)
            B_QT = Buf("QT")
            KT = sba("KT", [128, 4, NKT * 128], BF16)
            B_KT = [Buf("KT%d" % j) for j in range(NKT)]
            Vaug = sba("Vaug", [128, NKT, 4, 130], BF16)
            B_V = [Buf("V%d" % j) for j in range(NKT)]
            PT = [sba("PT%d" % i, [128, 512], BF16) for i in range(2)]
            B_PT = [Buf("PT%d" % i) for i in range(2)]
            rz = sba("rz", [128, 2], F32)
            rz1n = sba("rz1n", [128, 1], F32)
            B_rz = Buf("rz")
            t1 = sba("t1", [128, 128], F32)
            B_t1 = Buf("t1")
            oa = sba("oa", [128, 4, 128], F32)
            B_oa = Buf("oa")
            midb = sba("midb", [128, 3072], BF16)
            hss = sba("hss", [128, 4], F32)
            B_hss = Buf("hss")
            oa_bf = midb[:, 0:512]
            B_oabf = Buf("oa_bf")
            sig = sba("sig", [128, 512], F32)
            logf = sba("logf", [128, 512], F32)
            kr = sba("kr", [128, 512], F32)
            kr_bf = sba("kr_bf", [128, 512], BF16)
            qr_bf = sba("qr_bf", [128, 512], BF16)
            iv_bf = sba("iv_bf", [128, 512], BF16)
            sog = sba("sog", [128, 512], BF16)
            sq = sig[:].rearrange("p (h v) -> p h v", h=4)
            B_sig, B_logf, B_kr, B_krbf, B_qr, B_iv, B_sog = (Buf(x) for x in ("sig", "logf", "kr", "krbf", "qr", "iv", "sog"))
            B_sq = B_sig
            ebmb = sba("ebmb", [128, 512], F32)
            k_out = sba("k_out", [128, 512], BF16)
            B_ebmb, B_kout2 = Buf("ebmb"), Buf("k_out")
            ebT = sba("ebT", [128, 4, 128], F32)
            enbT = sba("enbT", [128, 4, 128], F32)
            B_ebT, B_enbT = Buf("ebT"), Buf("enbT")
            q_inT = sba("q_inT", [128, 4, 128], BF16)
            k_inT = sba("k_inT", [128, 4, 128], BF16)
            B_qinT, B_kinT = Buf("q_inT"), Buf("k_inT")
            ATm = [sba("ATm%d" % i, [128, 128], BF16) for i in range(2)]
            B_ATm = [Buf("ATm%d" % i) for i in range(2)]
            Sst = sba("Sst", [128, 4, 128], F32)
            S_bf = sba("S_bf", [128, 4, 128], BF16)
            B_S = [Buf("S%d" % h) for h in range(4)]
            B_Sbf = [Buf("Sbf%d" % h) for h in range(4)]
            orr = sba("orr", [128, 4, 128], F32)
            B_orr = Buf("orr")
            orr_bf = midb[:, 512:1024]
            B_orrbf = Buf("orr_bf")
            sgA = midb[:, 1024:2048]
            sgB = midb[:, 2048:3072]
            B_sgA, B_sgB = Buf("sgA"), Buf("sgB")
            cst = [midb[:, 1024:2048].bitcast(F32), midb[:, 2048:3072].bitcast(F32)]
            B_cst = [B_sgA, B_sgB]
            dbg = sba("dbg", [128, D], F32) if cfg.stage == 2 else None
            B_dbg = Buf("dbg")
            mid = nc.dram_tensor("mid_scratch", [NTOK, 3072], BF16, kind="Internal").ap()
            B_mid = Buf("mid")

            if KSTOP == "weights":
                S.finish("sp"); S.eng["sp"].wait_ge(S.sem["dve"], S.cnt["dve"]); S.eng["sp"].wait_ge(S.sem["act"], S.cnt["act"]); S.eng["sp"].wait_ge(S.sem["pool"], S.cnt["pool"])
                return nc
            POOL(lambda e: e.memset(Vaug[:, :, :, 128:130], 1.0), w=B_V)

            if KSTOP == "vaug":
                S.finish("sp"); S.eng["sp"].wait_ge(S.sem["dve"], S.cnt["dve"]); S.eng["sp"].wait_ge(S.sem["act"], S.cnt["act"]); S.eng["sp"].wait_ge(S.sem["pool"], S.cnt["pool"])
                return nc
            S.dma("sp", xt[0][:tiles[0][3], :], x_src(tiles[0]), writes=[B_xt[0]])
            cstk = [0]

            def seq_start(kind, b):
                if kind == "p":
                    for h in range(4):
                        POOL(lambda e, h=h: e.memset(Sst[:, h, :], 0.0), w=[B_S[h]])
                        POOL(lambda e, h=h: e.memset(S_bf[:, h, :], 0.0), w=[B_Sbf[h]])
                    return
                S.dma("sp", Sst[:], st[b].rearrange("h d v -> d h v"), writes=B_S)
                for h in range(4):
                    ACT(lambda e, h=h: e.copy(out=S_bf[:, h, :], in_=Sst[:, h, :]), r=[B_S[h]], w=[B_Sbf[h]])
                for j in range(PAST // 128):
                    ci = cstk[0] % 2
                    cstk[0] += 1
                    S.dma("sp", cst[ci][:], ck[b, j * 128:(j + 1) * 128, :], writes=[B_cst[ci]])
                    bk = next_bank()
                    for h in range(4):
                        PE(lambda e, h=h, bk=bk, ci=ci: e.transpose(out=pbank[bk][:, h * 128:(h + 1) * 128],
                                                                    in_=cst[ci][:, h * 128:(h + 1) * 128], identity=ident_f[:]),
                           r=[B_cst[ci], B_c], w=[B_pb[bk]])
                    DVE(lambda e, bk=bk, j=j: e.tensor_copy(out=KT[:, :, j * 128:(j + 1) * 128],
                                                           in_=pbank[bk].rearrange("p (h t) -> p h t", h=4)),
                        r=[B_pb[bk]], w=[B_KT[j]])
                    ci = cstk[0] % 2
                    cstk[0] += 1
                    S.dma("sp", cst[ci][:], cv[b, j * 128:(j + 1) * 128, :], writes=[B_cst[ci]])
                    ACT(lambda e, j=j, ci=ci: e.copy(out=Vaug[:, j, :, 0:128], in_=cst[ci].rearrange("p (h v) -> p h v", h=4)),
                        r=[B_cst[ci]], w=[B_V[j]])

            def mixer_tile(ti, t):
                kind, b, i, n, tk0 = t
                cur = ti % 2
                if i == 0 and KSTOP != "noseq":
                    seq_start(kind, b)
                if ti + 1 < len(tiles):
                    nt = tiles[ti + 1]
                    S.dma("sp", xt[1 - cur][:nt[3], :], x_src(nt), writes=[B_xt[1 - cur]])
                X, BX = xt[cur], B_xt[cur]
                jn = i if kind == "p" else PAST // 128
                ACT(lambda e: e.activation(out=junk[:n, :], in_=X[:n, :], func=AF.Square, accum_out=ss[:n, :]),
                    r=[BX], w=[B_junk, B_ss])
                ACT(lambda e: e.activation(out=rstd[:n, :], in_=ss[:n, :], func=AF.Sqrt, scale=1.0 / D, bias=EPS),
                    r=[B_ss], w=[B_rstd])
                DVE(lambda e: e.reciprocal(out=rstd[:n, :], in_=rstd[:n, :]), r=[B_rstd], w=[B_rstd])
                DVE(lambda e: e.scalar_tensor_tensor(out=xn[:n, :], in0=X[:n, :], scalar=rstd[:n, :], in1=n1b[:n, :],
                                                     op0=OP.mult, op1=OP.mult), r=[BX, B_rstd, B_c], w=[B_xn])
                bk = next_bank()
                for c in range(8):
                    PE(lambda e, c=c, bk=bk: e.transpose(out=pb_bf(bk)[:, c * 128:c * 128 + n], in_=xn[:n, c * 128:(c + 1) * 128],
                                                         identity=ident_b[:n, :n]), r=[B_xn, B_c], w=[B_pb[bk]])
                DVE(lambda e, bk=bk: e.tensor_copy(out=xnT[:, :, :n], in_=pb_bf(bk).rearrange("p (c t) -> p c t", c=8)[:, :, :n]),
                    r=[B_pb[bk]], w=[B_xnT])

                def zgroup(g):
                    bk = next_bank()
                    for c in range(8):
                        PE(lambda e, c=c, bk=bk: e.matmul(pbank[bk][:n, :], lhsT=xnT[:, c, :n], rhs=w_in_sb[:, c, g * 512:(g + 1) * 512],
                                                          start=(c == 0), stop=(c == 7)), r=[B_xnT, B_w], w=[B_pb[bk]])
                    return bk

                def transpose4(src_bf, Bsrc, dst_fn, Bdst_list, evac_eng="dve", mul=None, Bmul=None):
                    bk = next_bank()
                    for h in range(4):
                        PE(lambda e, h=h, bk=bk: e.transpose(out=pb_bf(bk)[:, h * 128:h * 128 + n], in_=src_bf[:n, h * 128:(h + 1) * 128],
                                                             identity=ident_b[:n, :n]), r=[Bsrc, B_c], w=[B_pb[bk]])
                    src = pb_bf(bk)[:, 0:512].rearrange("p (h t) -> p h t", h=4)[:, :, :n]
                    if mul is not None:
                        DVE(lambda e: e.tensor_tensor(out=dst_fn(), in0=src, in1=mul, op=OP.mult), r=[B_pb[bk], Bmul], w=Bdst_list)
                    elif evac_eng == "dve":
                        DVE(lambda e: e.tensor_copy(out=dst_fn(), in_=src), r=[B_pb[bk]], w=Bdst_list)
                    else:
                        ACT(lambda e: e.copy(out=dst_fn(), in_=src), r=[B_pb[bk]], w=Bdst_list)

                if KSTOP == "t_xnT":
                    return
                bk = zgroup(0)
                ACT(lambda e, bk=bk: e.copy(out=qa_bf[:n, :], in_=pbank[bk][:n, :]), r=[B_pb[bk]], w=[B_qa])
                transpose4(qa_bf, B_qa, lambda: QT[:, :, :n], [B_QT])
                if KSTOP == "t_g0":
                    return
                bk = zgroup(1)
                ACT(lambda e, bk=bk: e.copy(out=kout[cur][:n, :], in_=pbank[bk][:n, :]), r=[B_pb[bk]], w=[B_kout[cur]])
                DVE(lambda e, bk=bk: e.tensor_copy(out=ka_bf[:n, :], in_=kout[cur][:n, :]), r=[B_kout[cur]], w=[B_ka])
                dst = k_p[b, i * 128:(i + 1) * 128, :] if kind == "p" else k_s[b, :, :]
                S.dma("sp", dst, kout[cur][:n, :], reads=[B_kout[cur]], sb=B_kout[cur], is_output=True)
                transpose4(ka_bf, B_ka, lambda: KT[:, :, jn * 128:jn * 128 + n], [B_KT[jn]])
                if KSTOP == "t_g1":
                    return
                bk = zgroup(2)
                DVE(lambda e, bk=bk: e.tensor_copy(out=vout[cur][:n, :], in_=pbank[bk][:n, :]), r=[B_pb[bk]], w=[B_vout[cur]])
                ACT(lambda e, bk=bk: e.copy(out=Vaug[:n, jn, :, 0:128], in_=pbank[bk].rearrange("p (h v) -> p h v", h=4)[:n]),
                    r=[B_pb[bk]], w=[B_V[jn]])
                dst = v_p[b, i * 128:(i + 1) * 128, :] if kind == "p" else v_s[b, :, :]
                S.dma("sp", dst, vout[cur][:n, :], reads=[B_vout[cur]], sb=B_vout[cur], is_output=True)
                if cfg.stage <= 1:
                    return
                bk = zgroup(3)
                ACT(lambda e, bk=bk: e.activation(out=sig[:n, :], in_=pbank[bk][:n, :], func=AF.Sigmoid), r=[B_pb[bk]], w=[B_sig])
                DVE(lambda e: e.tensor_tensor(out=sig[:n, :], in0=sig[:n, :], in1=oml_b[:n, :], op=OP.mult), r=[B_sig, B_c], w=[B_sig])
                DVE(lambda e: e.tensor_tensor(out=sig[:n, :], in0=sig[:n, :], in1=lb_b[:n, :], op=OP.add), r=[B_sig, B_c], w=[B_sig])
                ACT(lambda e: e.activation(out=logf[:n, :], in_=sig[:n, :], func=AF.Ln), r=[B_sig], w=[B_logf])
                DVE(lambda e: e.tensor_scalar(out=kr[:n, :], in0=sig[:n, :], scalar1=-1.0, scalar2=1.0, op0=OP.mult, op1=OP.add),
                    r=[B_sig], w=[B_kr])
                DVE(lambda e: e.tensor_copy(out=kr_bf[:n, :], in_=kr[:n, :]), r=[B_kr], w=[B_krbf])
                bk = zgroup(4)
                ACT(lambda e, bk=bk: e.activation(out=qr_bf[:n, :], in_=pbank[bk][:n, :], func=AF.Silu), r=[B_pb[bk]], w=[B_qr])
                bk = zgroup(5)
                DVE(lambda e, bk=bk: e.tensor_copy(out=iv_bf[:n, :], in_=pbank[bk][:n, :]), r=[B_pb[bk]], w=[B_iv])
                bk = zgroup(6)
                ACT(lambda e, bk=bk: e.activation(out=sog[:n, :], in_=pbank[bk][:n, :], func=AF.Silu), r=[B_pb[bk]], w=[B_sog])
                for gi in range(2):
                    bk = zgroup(7 + gi)
                    ACT(lambda e, bk=bk, gi=gi: e.activation(out=sgA[:n, gi * 512:(gi + 1) * 512], in_=pbank[bk][:n, :], func=AF.Sigmoid),
                        r=[B_pb[bk]], w=[B_sgA])
                for gi in range(2):
                    bk = zgroup(9 + gi)
                    ACT(lambda e, bk=bk, gi=gi: e.activation(out=sgB[:n, gi * 512:(gi + 1) * 512], in_=pbank[bk][:n, :], func=AF.Sigmoid),
                        r=[B_pb[bk]], w=[B_sgB])

                if kind == "p":
                    ktiles = [(j, 128) for j in range(i + 1)]
                else:
                    ktiles = [(j, 128) for j in range(PAST // 128)] + [(PAST // 128, n)]
                groups = []
                g_ = []
                for (j, kn) in ktiles:
                    if kn < 128 and g_:
                        groups.append(g_)
                        g_ = []
                    g_.append((j, kn))
                    if len(g_) == 4:
                        groups.append(g_)
                        g_ = []
                if g_:
                    groups.append(g_)
                items = []
                for h in range(4):
                    for m in range(2):
                        for gi, grp in enumerate(groups):
                            items.append((h, m, gi, grp))

                def emit_scores(k):
                    h, m, gi, grp = items[k]
                    ps_ = slice(m * 64, (m + 1) * 64)
                    sbk = 4 + (k % 2)
                    pt = k % 2
                    for jj, (j, kn) in enumerate(grp):
                        PE(lambda e, jj=jj, j=j, kn=kn, sbk=sbk, ps_=ps_, h=h: e.matmul(
                            pbank[sbk][:kn, jj * 128:jj * 128 + n], lhsT=KT[ps_, h, j * 128:j * 128 + kn],
                            rhs=QT[ps_, h, :n], start=True, stop=True),
                           r=[B_KT[j], B_QT], w=[B_pb[sbk]])
                    knmax = max(kn for _, kn in grp)
                    nb_ = len(grp)
                    ACT(lambda e, sbk=sbk, pt=pt, knmax=knmax, nb_=nb_: e.activation(
                        out=PT[pt].rearrange("p (j t) -> p j t", t=128)[:knmax, :nb_, :n],
                        in_=pbank[sbk].rearrange("p (j t) -> p j t", t=128)[:knmax, :nb_, :n], func=AF.Exp, scale=0.125),
                        r=[B_pb[sbk]], w=[B_PT[pt]])
                    for jj, (j, kn) in enumerate(grp):
                        if kind == "p" and j == i:
                            DVE(lambda e, jj=jj, pt=pt: e.memset(PT[pt][64:128, jj * 128:jj * 128 + 64], 0.0),
                                r=[B_PT[pt]], w=[B_PT[pt]])

                def emit_pv(k):
                    h, m, gi, grp = items[k]
                    ob = 6 + (h % 2)
                    pt = k % 2
                    for jj, (j, kn) in enumerate(grp):
                        first = (gi == 0 and jj == 0)
                        last = (gi == len(groups) - 1 and jj == len(grp) - 1)
                        PE(lambda e, jj=jj, j=j, kn=kn, pt=pt, first=first, last=last, ob=ob, m=m, h=h: e.matmul(
                            pbank[ob][:n, m * 256:m * 256 + 129], lhsT=PT[pt][:kn, jj * 128:jj * 128 + n],
                            rhs=Vaug[:kn, j, h, 0:129], start=first, stop=last),
                           r=[B_PT[pt], B_V[j]], w=[B_pb[ob]])
                    if m == 1 and gi == len(groups) - 1:
                        ov = pbank[ob].rearrange("p (m c) -> p m c", c=256)
                        DVE(lambda e, ov=ov: e.reciprocal(out=rz[:n, :], in_=ov[:n, :, 128]), r=[B_pb[ob]], w=[B_rz])
                        DVE(lambda e: e.tensor_tensor(out=rz1n[:n, :], in0=rz[:n, 1:2], in1=neg_lam[:n, :], op=OP.mult), r=[B_rz, B_c], w=[B_rz])
                        DVE(lambda e, ob=ob: e.tensor_scalar(out=t1[:n, :], in0=pbank[ob][:n, 256:384], scalar1=rz1n[:n, :], scalar2=None,
                                                            op0=OP.mult), r=[B_pb[ob], B_rz], w=[B_t1])
                        DVE(lambda e, ob=ob, h=h: e.scalar_tensor_tensor(out=oa[:n, h, :], in0=pbank[ob][:n, 0:128], scalar=rz[:n, 0:1],
                                                                         in1=t1[:n, :], op0=OP.mult, op1=OP.add),
                            r=[B_pb[ob], B_rz, B_t1], w=[B_oa])


                def head_norm(src, Bsrc, gain_b, dst_bf, Bdst, extra=None, Bextra=None):
                    DVE(lambda e: e.tensor_tensor(out=sq[:n], in0=src[:n], in1=src[:n], op=OP.mult), r=[Bsrc], w=[B_sq])
                    DVE(lambda e: e.tensor_reduce(out=hss[:n, :], in_=sq[:n], axis=AX.X, op=OP.add), r=[B_sq], w=[B_hss])
                    ACT(lambda e: e.activation(out=hss[:n, :], in_=hss[:n, :], func=AF.Sqrt, scale=1.0 / 128, bias=EPS), r=[B_hss], w=[B_hss])
                    DVE(lambda e: e.reciprocal(out=hss[:n, :], in_=hss[:n, :]), r=[B_hss], w=[B_hss])
                    DVE(lambda e: e.tensor_tensor(out=sq[:n], in0=src[:n], in1=hss[:n, :].unsqueeze(2).to_broadcast([n, 4, 128]), op=OP.mult),
                        r=[Bsrc, B_hss], w=[B_sq])
                    if extra is None:
                        DVE(lambda e: e.tensor_tensor(out=dst_bf[:n, :].rearrange("p (h v) -> p h v", h=4), in0=sq[:n],
                                                      in1=gain_b[:n, :].unsqueeze(1).to_broadcast([n, 4, 128]), op=OP.mult),
                            r=[B_sq, B_c], w=[Bdst])
                    else:
                        DVE(lambda e: e.tensor_tensor(out=sq[:n], in0=sq[:n], in1=gain_b[:n, :].unsqueeze(1).to_broadcast([n, 4, 128]), op=OP.mult),
                            r=[B_sq, B_c], w=[B_sq])
                        DVE(lambda e: e.tensor_tensor(out=dst_bf[:n, :], in0=sq[:n].rearrange("p h v -> p (h v)"), in1=extra[:n, :], op=OP.mult),
                            r=[B_sq, Bextra], w=[Bdst])

                def hgrn_gen():
                    bk = next_bank()
                    PE(lambda e, bk=bk: e.matmul(pbank[bk][:n, :], lhsT=up_f[:n, :n], rhs=logf[:n, :], start=True, stop=True),
                       r=[B_c, B_logf], w=[B_pb[bk]])
                    ACT(lambda e, bk=bk: e.activation(out=ebmb[:n, :], in_=pbank[bk][:n, :], func=AF.Exp), r=[B_pb[bk]], w=[B_ebmb])
                    DVE(lambda e: e.tensor_tensor(out=k_out[:n, :], in0=kr[:n, :], in1=ebmb[:n, :], op=OP.mult), r=[B_kr, B_ebmb], w=[B_kout2])
                    yield
                    bk = next_bank()
                    for h in range(4):
                        PE(lambda e, h=h, bk=bk: e.matmul(pbank[bk][:, h * 128:h * 128 + n], lhsT=logf[:n, h * 128:(h + 1) * 128],
                                                          rhs=tri_f[:n, :n], start=True, stop=True), r=[B_logf, B_c], w=[B_pb[bk]])
                    bTv = pbank[bk].rearrange("p (h t) -> p h t", h=4)[:, :, :n]
                    ACT(lambda e: e.activation(out=ebT[:, :, :n], in_=bTv, func=AF.Exp), r=[B_pb[bk]], w=[B_ebT])
                    ACT(lambda e: e.activation(out=enbT[:, :, :n], in_=bTv, func=AF.Exp, scale=-1.0), r=[B_pb[bk]], w=[B_enbT])
                    yield
                    transpose4(qr_bf, B_qr, lambda: q_inT[:, :, :n], [B_qinT], mul=ebT[:, :, :n], Bmul=B_ebT)
                    yield
                    transpose4(kr_bf, B_krbf, lambda: k_inT[:, :, :n], [B_kinT], mul=enbT[:, :, :n], Bmul=B_enbT)
                    yield
                    for h in range(4):
                        bk = next_bank()
                        am = h % 2
                        hs_ = slice(h * 128, (h + 1) * 128)
                        PE(lambda e, h=h, bk=bk: e.matmul(pbank[bk][:n, 0:n], lhsT=k_inT[:, h, :n], rhs=q_inT[:, h, :n], start=True, stop=True),
                           r=[B_kinT, B_qinT], w=[B_pb[bk]])
                        DVE(lambda e, bk=bk, am=am: e.tensor_tensor(out=ATm[am][:n, :n], in0=pbank[bk][:n, 0:n], in1=tri_f[:n, :n], op=OP.mult),
                            r=[B_pb[bk], B_c], w=[B_ATm[am]])
                        yield
                        PE(lambda e, h=h, bk=bk, am=am: e.matmul(pbank[bk][:n, 128:256], lhsT=ATm[am][:n, :n], rhs=iv_bf[:n, hs_], start=True, stop=False),
                           r=[B_ATm[am], B_iv], w=[B_pb[bk]])
                        PE(lambda e, h=h, bk=bk: e.matmul(pbank[bk][:n, 128:256], lhsT=q_inT[:, h, :n], rhs=S_bf[:, h, :], start=False, stop=True),
                           r=[B_qinT, B_Sbf[h]], w=[B_pb[bk]])
                        PE(lambda e, h=h, bk=bk: e.matmul(pbank[bk][:, 256:384], lhsT=k_out[:n, hs_], rhs=iv_bf[:n, hs_], start=True, stop=True),
                           r=[B_kout2, B_iv], w=[B_pb[bk]])
                        ACT(lambda e, h=h, bk=bk: e.copy(out=orr[:n, h, :], in_=pbank[bk][:n, 128:256]), r=[B_pb[bk]], w=[B_orr])
                        DVE(lambda e, h=h, bk=bk: e.scalar_tensor_tensor(out=Sst[:, h, :], in0=Sst[:, h, :], scalar=ebT[:, h, n - 1:n],
                                                                         in1=pbank[bk][:, 256:384], op0=OP.mult, op1=OP.add),
                            r=[B_S[h], B_ebT, B_pb[bk]], w=[B_S[h]])
                        DVE(lambda e, h=h: e.tensor_copy(out=S_bf[:, h, :], in_=Sst[:, h, :]), r=[B_S[h]], w=[B_Sbf[h]])
                hg = hgrn_gen()
                emit_scores(0)
                for k in range(len(items)):
                    if k + 1 < len(items):
                        emit_scores(k + 1)
                    emit_pv(k)
                    next(hg, None)
                head_norm(oa, B_oa, asub_b, oa_bf, B_oabf)
                for _ in hg:
                    pass
                head_norm(orr, B_orr, gn_b, orr_bf, B_orrbf, extra=sog, Bextra=B_sog)
                last_of_seq = (kind == "s") or (i == TP // 128 - 1)
                if last_of_seq:
                    dsts = s_p[b] if kind == "p" else s_s[b]
                    S.dma("sp", dsts.rearrange("h d v -> d h v"), Sst[:], reads=B_S, sb=B_S[0], is_output=True)

                if cfg.stage == 2:
                    DVE(lambda e: e.tensor_copy(out=dbg[:n, 0:512], in_=oa_bf[:n, :]), r=[B_oabf], w=[B_dbg])
                    DVE(lambda e: e.tensor_copy(out=dbg[:n, 512:1024], in_=orr_bf[:n, :]), r=[B_orrbf], w=[B_dbg])
                    dst = y_p[b, i * 128:(i + 1) * 128, :] if kind == "p" else y_s[b, :, :]
                    S.dma("sp", dst, dbg[:n, :], reads=[B_dbg], sb=B_dbg, is_output=True)
                S.dma("sp", mid[tk0:tk0 + n, :], midb[:n, :], reads=[B_oabf, B_orrbf, B_sgA, B_sgB], writes=[B_mid], sb=B_oabf)

            for ti, t in enumerate(tiles):
                prepass(4)
                mixer_tile(ti, t)
            prepass(2 * NCH)
        S.barrier()

        if cfg.stage < 3:
            S.finish("sp")
            print("ninst", S.ninst, S.cnt)
            return nc

        btiles = []
        for b in range(NP):
            for i in range(TP // 128):
                r0 = (b * (TP // 128) + i) * 128
                btiles.append((r0, 128, x_p[b, i * 128:(i + 1) * 128, :], y_p[b, i * 128:(i + 1) * 128, :]))
        xs_flat = x_s.rearrange("b t d -> (b t) d")
        ys_flat = y_s.rearrange("b t d -> (b t) d")
        for r in range(0, NS * TS, 128):
            n_ = min(128, NS * TS - r)
            btiles.append((NP * TP + r, n_, xs_flat[r:r + n_, :], ys_flat[r:r + n_, :]))
        h2s = nc.dram_tensor("h2_scratch", [NTOK, D], F32, kind="Internal").ap()
        B_h2s = Buf("h2s")

        es_b = contextlib.ExitStack()

        def sbb(name, shape, dt=F32):
            return es_b.enter_context(nc.sbuf_tensor(name, list(shape), dt))

        with es_b:
            xt2 = [sbb("xt2_%d" % i, [128, D], F32) for i in range(2)]
            B_xt2 = [Buf("xt2_%d" % i) for i in range(2)]
            stage_ref["bufs"], stage_ref["B"] = xt2, B_xt2
            w_a_sb = sbb("w_a_sb", [128, 4, D], BF16)
            w_b_sb = sbb("w_b_sb", [128, 4, D], BF16)
            w_out_sb = sbb("w_out_sb", [128, 8, D], BF16)
            load_w(w_a_sb, w_a, 4, D)
            load_w(w_b_sb, w_b, 4, D)
            load_w(w_out_sb, w_out, 8, D)
            midt = [sbb("midt%d" % i, [128, 3072], BF16) for i in range(2)]
            B_midt = [Buf("midt%d" % i) for i in range(2)]
            oT = sbb("oT", [128, 8, 128], BF16)
            B_oT = Buf("oT")
            ua = sbb("ua", [128, D], F32)
            ub = sbb("ub", [128, D], F32)
            B_ua, B_ub = Buf("ua"), Buf("ub")
            u_bf = sbb("u_bf", [128, D], BF16)
            B_ubf = Buf("u_bf")
            uT = sbb("uT", [128, 8, 128], BF16)
            B_uT = Buf("uT")

            def a2_load(k):
                r0, n, xsrc, _ = btiles[k]
                c_ = k % 2
                S.dma("sp", midt[c_][:n, :], mid[r0:r0 + n, :], reads=[B_mid], writes=[B_midt[c_]])
                S.dma("sp", xt2[c_][:n, :], xsrc, writes=[B_xt2[c_]])

            a2_load(0)
            for k, (r0, n, xsrc, _) in enumerate(btiles):
                c_ = k % 2
                if k + 1 < len(btiles):
                    a2_load(k + 1)
                M, BM = midt[c_], B_midt[c_]
                tb = 4 + (k % 2)
                for c in range(8):
                    PE(lambda e, c=c: e.transpose(out=pb_bf(tb)[:, c * 128:c * 128 + n], in_=M[:n, c * 128:(c + 1) * 128],
                                                  identity=ident_b[:n, :n]), r=[BM, B_c], w=[B_pb[tb]])
                DVE(lambda e: e.tensor_copy(out=oT[:, :, :n], in_=pb_bf(tb).rearrange("p (c t) -> p c t", c=8)[:, :, :n]),
                    r=[B_pb[tb]], w=[B_oT])
                for half in range(2):
                    hs_ = slice(half * 512, (half + 1) * 512)
                    for c in range(4):
                        PE(lambda e, c=c, half=half, hs_=hs_: e.matmul(pbank[half][:n, :], lhsT=oT[:, c, :n], rhs=w_a_sb[:, c, hs_],
                                                                       start=(c == 0), stop=(c == 3)), r=[B_oT, B_w], w=[B_pb[half]])
                    for c in range(4):
                        PE(lambda e, c=c, half=half, hs_=hs_: e.matmul(pbank[2 + half][:n, :], lhsT=oT[:, 4 + c, :n], rhs=w_b_sb[:, c, hs_],
                                                                       start=(c == 0), stop=(c == 3)), r=[B_oT, B_w], w=[B_pb[2 + half]])
                for half in range(2):
                    hs_ = slice(half * 512, (half + 1) * 512)
                    DVE(lambda e, half=half, hs_=hs_: e.tensor_tensor(out=ua[:n, hs_], in0=pbank[half][:n, :],
                                                                      in1=M[:n, 1024 + half * 512:1024 + (half + 1) * 512], op=OP.mult),
                        r=[B_pb[half], BM], w=[B_ua])
                    DVE(lambda e, half=half, hs_=hs_: e.tensor_tensor(out=ub[:n, hs_], in0=pbank[2 + half][:n, :],
                                                                      in1=M[:n, 2048 + half * 512:2048 + (half + 1) * 512], op=OP.mult),
                        r=[B_pb[2 + half], BM], w=[B_ub])
                DVE(lambda e: e.tensor_tensor(out=u_bf[:n, :], in0=ua[:n, :], in1=ub[:n, :], op=OP.add), r=[B_ua, B_ub], w=[B_ubf])
                tb2 = 6 + (k % 2)
                for c in range(8):
                    PE(lambda e, c=c: e.transpose(out=pb_bf(tb2)[:, c * 128:c * 128 + n], in_=u_bf[:n, c * 128:(c + 1) * 128],
                                                  identity=ident_b[:n, :n]), r=[B_ubf, B_c], w=[B_pb[tb2]])
                ACT(lambda e: e.copy(out=uT[:, :, :n], in_=pb_bf(tb2).rearrange("p (c t) -> p c t", c=8)[:, :, :n]),
                    r=[B_pb[tb2]], w=[B_uT])
                for half in range(2):
                    hs_ = slice(half * 512, (half + 1) * 512)
                    for c in range(8):
                        PE(lambda e, c=c, half=half, hs_=hs_: e.matmul(pbank[half][:n, :], lhsT=uT[:, c, :n], rhs=w_out_sb[:, c, hs_],
                                                                       start=(c == 0), stop=(c == 7)), r=[B_uT, B_w], w=[B_pb[half]])
                    DVE(lambda e, half=half, hs_=hs_: e.tensor_tensor(out=xt2[c_][:n, hs_], in0=pbank[half][:n, :], in1=xt2[c_][:n, hs_], op=OP.add),
                        r=[B_pb[half], B_xt2[c_]], w=[B_xt2[c_]])
                S.dma("sp", h2s[r0:r0 + n, :], xt2[c_][:n, :], reads=[B_xt2[c_]], writes=[B_h2s], sb=B_xt2[c_])

        S.barrier()
        if cfg.stage < 4:
            S.finish("sp")
            print("ninst", S.ninst, S.cnt)
            return nc

        es_c = contextlib.ExitStack()

        def sbc(name, shape, dt=F32):
            return es_c.enter_context(nc.sbuf_tensor(name, list(shape), dt))

        TPB = 2
        TSUB = 8
        with es_c:
            n2b = bcast_load("n2b", norm2, D, alloc=sbc)
            fnb = bcast_load("fnb", final_norm, D, alloc=sbc)
            iota_i = sbc("iota_i", [128, 128], I32)
            iota_f = sbc("iota_f", [128, 128], F32)
            bmask = sbc("bmask", [128, 8], F32)
            POOL(lambda e: e.iota(out=iota_i[:], pattern=[[1, 128]], base=0, channel_multiplier=0), w=[B_c])
            POOL(lambda e: e.memset(bmask[:], 1.0), w=[B_c])
            POOL(lambda e: e.affine_select(out=bmask[:], in_=bmask[:], pattern=[[-16, 8]], compare_op=OP.is_ge, fill=0.0,
                                           base=0, channel_multiplier=1), r=[B_c], w=[B_c])
            POOL(lambda e: e.affine_select(out=bmask[:], in_=bmask[:], pattern=[[16, 8]], compare_op=OP.is_ge, fill=0.0,
                                           base=15, channel_multiplier=-1), r=[B_c], w=[B_c])
            DVE(lambda e: e.tensor_copy(out=iota_f[:], in_=iota_i[:]), r=[B_c], w=[B_c])

            GT_sb = sbc("GT_sb", [128, TPB * 128, 128], BF16)
            B_GT = [Buf("GT%d" % j) for j in range(TPB)]
            h2 = [sbc("h2_%d" % j, [128, D], F32) for j in range(TPB)]
            B_h2 = [Buf("h2_%d" % j) for j in range(TPB)]
            stage_ref["bufs"], stage_ref["B"] = h2, B_h2
            p_wq_sb = sbc("p_wq_sb", [128, 8, 2048], BF16)
            load_w(p_wq_sb, p_wq, 8, 2048)
            keysT = sbc("keysT", [128, 16, 128], BF16)
            kst = GT_sb[:, 0:32, :].bitcast(F32).rearrange("p (g k) d -> p g (k d)", g=16)
            S.dma("sp", kst, p_keys.rearrange("g k d -> k g d"), writes=[B_GT[0]])
            for g4 in range(4):
                bk = g4
                for gg in range(4):
                    g = g4 * 4 + gg
                    PE(lambda e, g=g, gg=gg, bk=bk: e.transpose(out=pbank[bk][:, gg * 128:(gg + 1) * 128], in_=kst[:, g, :],
                                                                 identity=ident_f[:]), r=[B_GT[0], B_c], w=[B_pb[bk]])
                DVE(lambda e, g4=g4, bk=bk: e.tensor_copy(out=keysT[:, g4 * 4:(g4 + 1) * 4, :],
                                                          in_=pbank[bk].rearrange("p (g k) -> p g k", g=4)), r=[B_pb[bk]], w=[B_w])
            xn2Ts = [sbc("xn2T_%d" % i, [128, 8, TPB * 128], BF16) for i in range(2)]
            B_xn2Ts = [[Buf("xn2T%d_%d" % (i, j)) for j in range(TPB)] for i in range(2)]
            xn2 = sbc("xn2", [128, D], BF16)
            B_xn2 = Buf("xn2")
            ss2 = sbc("ss2", [128, 1], F32)
            rstd2 = sbc("rstd2", [128, 1], F32)
            B_ss2, B_rstd2 = Buf("ss2"), Buf("rstd2")
            q_bf = sbc("q_bf", [128, 2048], BF16)
            B_q = Buf("q_bf")
            qT = sbc("qT", [128, 16, 128], BF16)
            B_qT = Buf("qT")
            s_sb = sbc("s_sb", [128, 2048], F32)
            B_s = Buf("s_sb")
            comb = sbc("comb", [128, 2048], F32)
            B_comb = Buf("comb")
            W_bf = sbc("W_bf", [128, 2048], BF16)
            B_W = Buf("W_bf")
            sv = sbc("sv", [128, 256], F32)
            si_u = sbc("si_u", [128, 256], U32)
            B_sv, B_siu = Buf("sv"), Buf("si_u")
            si01 = sbc("si01", [128, 2, 128], BF16)
            B_si01 = Buf("si01")
            work = sbc("work", [128, 128], F32)
            work2 = sbc("work2", [128, 256], F32)
            B_work, B_work2 = Buf("work"), Buf("work2")
            m8a = sbc("m8a", [128, 8, 8], F32)
            m8b = sbc("m8b", [128, 8, 8], F32)
            negm = sbc("negm", [128, 8], F32)
            Zs = sbc("Zs", [128, 8], F32)
            B_m8 = Buf("m8")
            B_Z = Buf("Z")
            WT2 = [sbc("WT_sb%d" % j, [128, 16, 128], BF16) for j in range(TPB)]
            B_WT2 = [Buf("WT%d" % j) for j in range(TPB)]
            WB4D = True
            bmask_b = sbc("bmask_b", [128, 8], BF16)
            DVE(lambda e: e.tensor_copy(out=bmask_b[:], in_=bmask[:]), r=[B_c], w=[B_c])
            siT2 = [sbc("siT%d" % j, [128, 2, 128], BF16) for j in range(TPB)]
            iota_b = sbc("iota_b", [128, 128], BF16)
            DVE(lambda e: e.tensor_copy(out=iota_b[:], in_=iota_f[:]), r=[B_c], w=[B_c])
            B_siT2 = [Buf("siT%d" % j) for j in range(TPB)]
            A0 = [sbc("A0_%d" % i, [128, TSUB, 128], BF16) for i in range(2)]
            B0 = [sbc("B0_%d" % i, [128, TSUB, 128], BF16) for i in range(2)]
            Wblk = [sbc("Wblk%d" % i, [128, TSUB, 128], BF16) for i in range(2)]
            B_A0 = [Buf("A0_%d" % i) for i in range(2)]
            B_B0 = [Buf("B0_%d" % i) for i in range(2)]
            B_Wblk = [Buf("Wblk%d" % i) for i in range(2)]
            R_sb = sbc("R_sb", [128, TSUB, 128], BF16)
            B_R = [Buf("R%d" % i) for i in range(TSUB // 4)]
            NRING = 4
            dn = [sbc("dn%d" % i, [128, 8, 128], BF16) for i in range(NRING)]
            upc = [sbc("upc%d" % i, [128, D], BF16) for i in range(NRING)]
            B_dn = [Buf("dn%d" % i) for i in range(NRING)]
            B_up = [Buf("upc%d" % i) for i in range(NRING)]
            gsb = [sbc("gsb%d" % i, [128, TPB * 128], F32) for i in range(2)]
            gh = [sbc("gh%d" % i, [128, TPB * 128], BF16) for i in range(2)]
            B_g = [Buf("g%d" % i) for i in range(2)]
            B_gh = [Buf("gh%d" % i) for i in range(2)]
            yt, B_yt = comb[:, 0:D], B_comb
            junk2, B_junk2 = q_bf[:, 0:D], B_q

            r8 = [0]

            def nb8():
                b_ = r8[0]
                r8[0] = (b_ + 1) % 8
                return b_

            rt = [0]

            def nbt():
                rt[0] ^= 1
                return 4 + rt[0]

            ec = [0]
            sub_k = [0]

            def tok_major(j, r0, n, xs):
                xn2T, siT, WT_sb, B_siT, B_WT = xn2Ts[xs], siT2[j], WT2[j], B_siT2[j], B_WT2[j]
                H2, BH2 = h2[j], B_h2[j]
                S.dma("sp", H2[:n, :], h2s[r0:r0 + n, :], reads=[B_h2s], writes=[BH2])
                ACT(lambda e: e.activation(out=junk2[:n, :], in_=H2[:n, :], func=AF.Square, accum_out=ss2[:n, :]),
                    r=[BH2], w=[B_junk2, B_ss2])
                ACT(lambda e: e.activation(out=rstd2[:n, :], in_=ss2[:n, :], func=AF.Sqrt, scale=1.0 / D, bias=EPS),
                    r=[B_ss2], w=[B_rstd2])
                DVE(lambda e: e.reciprocal(out=rstd2[:n, :], in_=rstd2[:n, :]), r=[B_rstd2], w=[B_rstd2])
                DVE(lambda e: e.scalar_tensor_tensor(out=xn2[:n, :], in0=H2[:n, :], scalar=rstd2[:n, :], in1=n2b[:n, :],
                                                     op0=OP.mult, op1=OP.mult), r=[BH2, B_rstd2, B_c], w=[B_xn2])
                bk = nbt()
                for c in range(8):
                    PE(lambda e, c=c, bk=bk: e.transpose(out=pb_bf(bk)[:, c * 128:c * 128 + n], in_=xn2[:n, c * 128:(c + 1) * 128],
                                                         identity=ident_b[:n, :n]), r=[B_xn2, B_c], w=[B_pb[bk]])
                DVE(lambda e, bk=bk: e.tensor_copy(out=xn2T[:, :, j * 128:j * 128 + n],
                                                   in_=pb_bf(bk).rearrange("p (c t) -> p c t", c=8)[:, :, :n]),
                    r=[B_pb[bk]], w=[B_xn2Ts[xs][j]])
                yield
                for g in range(4):
                    bk = nbt()
                    for c in range(8):
                        PE(lambda e, c=c, bk=bk, g=g: e.matmul(pbank[bk][:n, :], lhsT=xn2T[:, c, j * 128:j * 128 + n],
                                                               rhs=p_wq_sb[:, c, g * 512:(g + 1) * 512], start=(c == 0), stop=(c == 7)),
                           r=[B_xn2Ts[xs][j], B_w], w=[B_pb[bk]])
                    if g % 2 == 0:
                        ACT(lambda e, bk=bk, g=g: e.copy(out=q_bf[:n, g * 512:(g + 1) * 512], in_=pbank[bk][:n, :]), r=[B_pb[bk]], w=[B_q])
                    else:
                        DVE(lambda e, bk=bk, g=g: e.tensor_copy(out=q_bf[:n, g * 512:(g + 1) * 512], in_=pbank[bk][:n, :]), r=[B_pb[bk]], w=[B_q])
                    yield
                for g2 in range(2):
                    bk = nbt()
                    for c in range(8):
                        hc = g2 * 8 + c
                        PE(lambda e, c=c, hc=hc, bk=bk: e.transpose(out=pb_bf(bk)[:, c * 128:c * 128 + n], in_=q_bf[:n, hc * 128:(hc + 1) * 128],
                                                                    identity=ident_b[:n, :n]), r=[B_q, B_c], w=[B_pb[bk]])
                    if g2 == 0:
                        ACT(lambda e, bk=bk, g2=g2: e.copy(out=qT[:, g2 * 8:(g2 + 1) * 8, :n],
                                                           in_=pb_bf(bk).rearrange("p (c t) -> p c t", c=8)[:, :, :n]), r=[B_pb[bk]], w=[B_qT])
                    else:
                        DVE(lambda e, bk=bk, g2=g2: e.tensor_copy(out=qT[:, g2 * 8:(g2 + 1) * 8, :n],
                                                                  in_=pb_bf(bk).rearrange("p (c t) -> p c t", c=8)[:, :, :n]), r=[B_pb[bk]], w=[B_qT])
                    yield
                for g4 in range(4):
                    bk = nbt()
                    for gg in range(4):
                        hc = g4 * 4 + gg
                        PE(lambda e, hc=hc, gg=gg, bk=bk: e.matmul(pbank[bk][:n, gg * 128:(gg + 1) * 128], lhsT=qT[:, hc, :n], rhs=keysT[:, hc, :],
                                                                   start=True, stop=True), r=[B_qT, B_w], w=[B_pb[bk]])
                    if g4 % 2 == 0:
                        ACT(lambda e, bk=bk, g4=g4: e.copy(out=s_sb[:n, g4 * 512:(g4 + 1) * 512], in_=pbank[bk][:n, :]), r=[B_pb[bk]], w=[B_s])
                    else:
                        DVE(lambda e, bk=bk, g4=g4: e.tensor_copy(out=s_sb[:n, g4 * 512:(g4 + 1) * 512], in_=pbank[bk][:n, :]), r=[B_pb[bk]], w=[B_s])
                    yield
                for hc in range(16):
                    grp = s_sb[:n, hc * 128:(hc + 1) * 128]
                    o8a = slice(hc * 16, hc * 16 + 8)
                    o8b = slice(hc * 16 + 8, hc * 16 + 16)
                    DVE(lambda e, grp=grp, o8a=o8a: e.max(out=sv[:n, o8a], in_=grp), r=[B_s], w=[B_sv])
                    DVE(lambda e, grp=grp, o8a=o8a: e.max_index(out=si_u[:n, o8a], in_max=sv[:n, o8a], in_values=grp), r=[B_s, B_sv], w=[B_siu])
                    DVE(lambda e, grp=grp, o8a=o8a: e.match_replace(out=work[:n, :], in_to_replace=sv[:n, o8a], in_values=grp, imm_value=-1e30),
                        r=[B_s, B_sv], w=[B_work])
                    DVE(lambda e, o8b=o8b: e.max(out=sv[:n, o8b], in_=work[:n, :]), r=[B_work], w=[B_sv])
                    DVE(lambda e, o8b=o8b: e.max_index(out=si_u[:n, o8b], in_max=sv[:n, o8b], in_values=work[:n, :]), r=[B_work, B_sv], w=[B_siu])
                    yield
                siv = si_u[:n, :].rearrange("p (h c a) -> p c h a", h=8, c=2)
                for c2 in range(2):
                    DVE(lambda e, c2=c2: e.tensor_copy(out=si01[:n, c2, :].rearrange("p (h a) -> p h a", h=8), in_=siv[:, c2]),
                        r=[B_siu], w=[B_si01])
                svv = sv[:n, :].rearrange("p (h c a) -> p h c a", h=8, c=2)
                DVE(lambda e: e.tensor_tensor(out=comb[:n, :].rearrange("p (h a b) -> p h a b", h=8, a=16),
                                              in0=svv[:, :, 0, :].unsqueeze(3).to_broadcast([n, 8, 16, 16]),
                                              in1=svv[:, :, 1, :].unsqueeze(2).to_broadcast([n, 8, 16, 16]), op=OP.add),
                    r=[B_sv], w=[B_comb])
                yield
                for h in range(8):
                    cg = comb[:n, h * 256:(h + 1) * 256]
                    DVE(lambda e, cg=cg, h=h: e.max(out=m8a[:n, h, :], in_=cg), r=[B_comb], w=[B_m8])
                    DVE(lambda e, cg=cg, h=h: e.match_replace(out=work2[:n, :], in_to_replace=m8a[:n, h, :], in_values=cg, imm_value=-1e30),
                        r=[B_comb, B_m8], w=[B_work2])
                    DVE(lambda e, h=h: e.max(out=m8b[:n, h, :], in_=work2[:n, :]), r=[B_work2], w=[B_m8])
                    yield
                DVE(lambda e: e.tensor_scalar(out=negm[:n, :], in0=m8a[:n, :, 0], scalar1=-1.0, scalar2=None, op0=OP.mult), r=[B_m8], w=[B_m8])
                for h in range(8):
                    cg = comb[:n, h * 256:(h + 1) * 256]
                    eg = s_sb[:n, h * 256:(h + 1) * 256]
                    ACT(lambda e, cg=cg, eg=eg, h=h: e.activation(out=eg, in_=cg, func=AF.Exp, bias=negm[:n, h:h + 1], scale=1.0),
                        r=[B_comb, B_m8], w=[B_s])
                yield
                for h in range(8):
                    cg = comb[:n, h * 256:(h + 1) * 256]
                    eg = s_sb[:n, h * 256:(h + 1) * 256]
                    DVE(lambda e, cg=cg, eg=eg, h=h: e.scalar_tensor_tensor(out=eg, in0=cg, scalar=m8b[:n, h, 7:8], in1=eg,
                                                                            op0=OP.is_ge, op1=OP.mult), r=[B_comb, B_m8, B_s], w=[B_s])
                    if h % 2 == 1:
                        yield
                DVE(lambda e: e.tensor_reduce(out=Zs[:n, :], in_=s_sb[:n, :].rearrange("p (h x) -> p h x", h=8), axis=AX.X, op=OP.add),
                    r=[B_s], w=[B_Z])
                DVE(lambda e: e.reciprocal(out=Zs[:n, :], in_=Zs[:n, :]), r=[B_Z], w=[B_Z])
                DVE(lambda e: e.tensor_tensor(out=W_bf[:n, :].rearrange("p (h x) -> p h x", h=8),
                                              in0=s_sb[:n, :].rearrange("p (h x) -> p h x", h=8),
                                              in1=Zs[:n, :].unsqueeze(2).to_broadcast([n, 8, 256]), op=OP.mult), r=[B_s, B_Z], w=[B_W])
                yield
                bk = nbt()
                for c2 in range(2):
                    PE(lambda e, c2=c2, bk=bk: e.transpose(out=pb_bf(bk)[:, c2 * 128:c2 * 128 + n], in_=si01[:n, c2, :], identity=ident_b[:n, :n]),
                       r=[B_si01, B_c], w=[B_pb[bk]])
                ACT(lambda e, bk=bk: e.copy(out=siT[:, :, :n], in_=pb_bf(bk)[:, 0:256].rearrange("p (c t) -> p c t", c=2)[:, :, :n]),
                    r=[B_pb[bk]], w=[B_siT])
                yield
                Wv = W_bf[:n, :].rearrange("p (ha b) -> p ha b", b=16)
                for g2 in range(2):
                    bk = nbt()
                    for bb in range(8):
                        b_ = g2 * 8 + bb
                        PE(lambda e, bb=bb, b_=b_, bk=bk: e.transpose(out=pb_bf(bk)[:, bb * 128:bb * 128 + n], in_=Wv[:, :, b_],
                                                                      identity=ident_b[:n, :n]), r=[B_W, B_c], w=[B_pb[bk]])
                    if g2 == 0:
                        ACT(lambda e, bk=bk, g2=g2: e.copy(out=WT_sb[:, g2 * 8:(g2 + 1) * 8, :n],
                                                           in_=pb_bf(bk).rearrange("p (c t) -> p c t", c=8)[:, :, :n]), r=[B_pb[bk]], w=[B_WT])
                    else:
                        DVE(lambda e, bk=bk, g2=g2: e.tensor_copy(out=WT_sb[:, g2 * 8:(g2 + 1) * 8, :n],
                                                                  in_=pb_bf(bk).rearrange("p (c t) -> p c t", c=8)[:, :, :n]), r=[B_pb[bk]], w=[B_WT])
                    yield
            def slot_phase(j, n):
                siT, WT_sb, B_siT, B_WT = siT2[j], WT2[j], B_siT2[j], B_WT2[j]
                for t0 in range(0, n, TSUB):
                    tn = min(TSUB, n - t0)
                    sk = sub_k[0] % 2
                    sub_k[0] += 1
                    DVE(lambda e, sk=sk, t0=t0, tn=tn: e.tensor_tensor(
                        out=A0[sk][:, :tn, :], in0=iota_b[:].unsqueeze(1).to_broadcast([128, tn, 128]),
                        in1=siT[:, 0, t0:t0 + tn].unsqueeze(2).to_broadcast([128, tn, 128]), op=OP.is_equal),
                        r=[B_siT, B_c], w=[B_A0[sk]])
                    DVE(lambda e, sk=sk, t0=t0, tn=tn: e.tensor_tensor(
                        out=B0[sk][:, :tn, :], in0=iota_b[:].unsqueeze(1).to_broadcast([128, tn, 128]),
                        in1=siT[:, 1, t0:t0 + tn].unsqueeze(2).to_broadcast([128, tn, 128]), op=OP.is_equal),
                        r=[B_siT, B_c], w=[B_B0[sk]])
                    if WB4D:
                        DVE(lambda e, sk=sk, t0=t0, tn=tn: e.tensor_tensor(
                            out=Wblk[sk][:, :tn, :].rearrange("p t (h b) -> p t h b", h=8),
                            in0=WT_sb[:, :, t0:t0 + tn].rearrange("p b t -> p t b").unsqueeze(2).to_broadcast([128, tn, 8, 16]),
                            in1=bmask_b[:, :].unsqueeze(1).unsqueeze(3).to_broadcast([128, tn, 8, 16]), op=OP.mult),
                            r=[B_WT, B_c], w=[B_Wblk[sk]])
                    else:
                        for hp in range(8):
                            if hp % 2 == 0:
                                DVE(lambda e, sk=sk, t0=t0, tn=tn, hp=hp: e.tensor_scalar(
                                    out=Wblk[sk][:, :tn, hp * 16:(hp + 1) * 16], in0=WT_sb[:, :, t0:t0 + tn].rearrange("p b t -> p t b"),
                                    scalar1=bmask[:, hp:hp + 1], scalar2=None, op0=OP.mult), r=[B_WT, B_c], w=[B_Wblk[sk]])
                            else:
                                ACT(lambda e, sk=sk, t0=t0, tn=tn, hp=hp: e.activation(
                                    out=Wblk[sk][:, :tn, hp * 16:(hp + 1) * 16], in_=WT_sb[:, :, t0:t0 + tn].rearrange("p b t -> p t b"),
                                    func=AF.Copy, scale=bmask[:, hp:hp + 1]), r=[B_WT, B_c], w=[B_Wblk[sk]])
                    def emit_R(q0, sk=sk, tn=tn):
                        w4 = min(4, tn - q0)
                        BR = B_R[q0 // 4]
                        bk = nb8()
                        for tt in range(q0, q0 + w4):
                            PE(lambda e, tt=tt, q0=q0, bk=bk, sk=sk: e.matmul(pbank[bk][:, (tt - q0) * 128:(tt - q0 + 1) * 128], lhsT=Wblk[sk][:, tt, :],
                                                                              rhs=A0[sk][:, tt, :], start=True, stop=True),
                               r=[B_Wblk[sk], B_A0[sk]], w=[B_pb[bk]])
                        ACT(lambda e, bk=bk, q0=q0, w4=w4: e.copy(out=R_sb[:, q0:q0 + w4, :],
                                                                   in_=pbank[bk][:, :w4 * 128].rearrange("p (t i) -> p t i", i=128)),
                            r=[B_pb[bk]], w=[BR])

                    def emit_GT(q0, sk=sk, tn=tn, t0=t0):
                        w4 = min(4, tn - q0)
                        BR = B_R[q0 // 4]
                        bk2 = nb8()
                        for tt in range(q0, q0 + w4):
                            PE(lambda e, tt=tt, q0=q0, bk2=bk2, sk=sk: e.matmul(pbank[bk2][:, (tt - q0) * 128:(tt - q0 + 1) * 128], lhsT=B0[sk][:, tt, :],
                                                                                rhs=R_sb[:, tt, :], start=True, stop=True),
                               r=[B_B0[sk], BR], w=[B_pb[bk2]])
                        g0 = j * 128 + t0 + q0
                        ACT(lambda e, bk2=bk2, g0=g0, w4=w4: e.copy(out=GT_sb[:, g0:g0 + w4, :],
                                                                     in_=pbank[bk2][:, :w4 * 128].rearrange("p (t i) -> p t i", i=128)),
                            r=[B_pb[bk2]], w=[B_GT[j]])

                    q0s = list(range(0, tn, 4))
                    for gi, q0 in enumerate(q0s):
                        emit_R(q0)
                        if gi > 0:
                            emit_GT(q0s[gi - 1])
                    emit_GT(q0s[-1])

            def drain(g):
                for _ in g:
                    pass

            def tok_chain(blk, xs):
                for j, (r0, n, _, _) in enumerate(blk):
                    yield from tok_major(j, r0, n, xs)

            blocks = [btiles[k0:k0 + TPB] for k0 in range(0, len(btiles), TPB)]
            drain(tok_chain(blocks[0], 0))
            for bi, blk in enumerate(blocks):
                xs = bi % 2
                xn2T = xn2Ts[xs]
                for j, (r0, n, _, _) in enumerate(blk):
                    slot_phase(j, n)
                span = (len(blk) - 1) * 128 + blk[-1][1]
                BXT = [B_xn2Ts[xs][j] for j in range(len(blk))]
                BGT = [B_GT[j] for j in range(len(blk))]
                nxt = tok_chain(blocks[bi + 1], 1 - xs) if bi + 1 < len(blocks) else iter(())

                def emit_hid(c, xn2T=xn2T, span=span, BXT=BXT, BGT=BGT):
                    k3 = ec[0] % NRING
                    e2 = ec[0] % 2
                    ec[0] += 1
                    S.dma("sp", dn[k3][:].rearrange("p a b -> p (a b)"), dnT_bf[c], reads=B_pre, writes=[B_dn[k3]])
                    S.dma("sp", upc[k3][:], up_bf[c * 128:(c + 1) * 128, :], reads=B_pre, writes=[B_up[k3]])
                    hb = 6 + e2
                    for dh in range(8):
                        PE(lambda e, dh=dh, hb=hb, k3=k3: e.matmul(pbank[hb][:, :span], lhsT=dn[k3][:, dh, :], rhs=xn2T[:, dh, :span],
                                                                   start=(dh == 0), stop=(dh == 7)), r=[B_dn[k3]] + BXT, w=[B_pb[hb]])
                    ACT(lambda e, hb=hb, e2=e2: e.activation(out=gsb[e2][:, :span], in_=pbank[hb][:, :span], func=AF.Gelu),
                        r=[B_pb[hb]], w=[B_g[e2]])
                    DVE(lambda e, e2=e2, c=c: e.tensor_tensor(out=gh[e2][:, :span], in0=gsb[e2][:, :span], in1=GT_sb[:, :span, c], op=OP.mult),
                        r=[B_g[e2]] + BGT, w=[B_gh[e2]])
                    return (c, k3, e2)

                def emit_out(st_, blk=blk):
                    c, k3, e2 = st_
                    for j, (r0, n, _, _) in enumerate(blk):
                        for half in range(2):
                            ob = 2 * j + half
                            PE(lambda e, j=j, n=n, half=half, ob=ob, e2=e2, k3=k3, c=c: e.matmul(
                                pbank[ob][:n, :], lhsT=gh[e2][:, j * 128:j * 128 + n], rhs=upc[k3][:, half * 512:(half + 1) * 512],
                                start=(c == 0), stop=(c == NCH - 1)), r=[B_gh[e2], B_up[k3]], w=[B_pb[ob]])

                prev = None
                for c in range(NCH):
                    cur_ = emit_hid(c)
                    next(nxt, None)
                    if prev is not None:
                        emit_out(prev)
                    prev = cur_
                emit_out(prev)
                drain(nxt)
                for j, (r0, n, _, ydst) in enumerate(blk):
                    H2, BH2 = h2[j], B_h2[j]
                    S.dma("sp", H2[:n, :], h2s[r0:r0 + n, :], reads=[B_h2s], writes=[BH2])
                    for half in range(2):
                        ob = 2 * j + half
                        hs_ = slice(half * 512, (half + 1) * 512)
                        DVE(lambda e, ob=ob, hs_=hs_, H2=H2, n=n: e.tensor_tensor(out=H2[:n, hs_], in0=pbank[ob][:n, :], in1=H2[:n, hs_], op=OP.add),
                            r=[B_pb[ob], BH2], w=[BH2])
                    ACT(lambda e, H2=H2, n=n: e.activation(out=junk2[:n, :], in_=H2[:n, :], func=AF.Square, accum_out=ss2[:n, :]),
                        r=[BH2], w=[B_junk2, B_ss2])
                    ACT(lambda e, n=n: e.activation(out=rstd2[:n, :], in_=ss2[:n, :], func=AF.Sqrt, scale=1.0 / D, bias=EPS),
                        r=[B_ss2], w=[B_rstd2])
                    DVE(lambda e, n=n: e.reciprocal(out=rstd2[:n, :], in_=rstd2[:n, :]), r=[B_rstd2], w=[B_rstd2])
                    DVE(lambda e, H2=H2, n=n: e.scalar_tensor_tensor(out=yt[:n, :], in0=H2[:n, :], scalar=rstd2[:n, :], in1=fnb[:n, :],
                                                                     op0=OP.mult, op1=OP.mult), r=[BH2, B_rstd2, B_c], w=[B_yt])
                    S.dma("sp", ydst, yt[:n, :], reads=[B_yt], sb=B_yt, is_output=True)
        S.finish("sp")
    print("ninst", S.ninst, S.cnt)
    return nc


_OUT_NAMES = ["y_prompt", "y_sample", "k_prompt", "v_prompt", "s_prompt", "k_sample", "v_sample", "s_sample"]


def make_in_maps(inputs, cfg, ncores):
    f = lambda a: np.ascontiguousarray(np.asarray(a, dtype=np.float32))
    shared = {"p_downT": f(np.asarray(inputs["p_down"], dtype=np.float32)[0].reshape(128, 128, 8, 128).transpose(0, 3, 2, 1)).reshape(128, 128, D)}
    maps = []
    for c in range(ncores):
        ps_ = slice(c * cfg.nseq_p, (c + 1) * cfg.nseq_p)
        ss_ = slice(c * cfg.nseq_s, (c + 1) * cfg.nseq_s)
        m = {
            "x_prompt": f(inputs["x_prompt"][ps_, :cfg.t_p]),
            "x_sample": f(inputs["x_sample"][ss_]),
            "cache_k": f(inputs["cache_k"][0, ss_]).reshape(cfg.nseq_s, cfg.past, 512),
            "cache_v": f(inputs["cache_v"][0, ss_]).reshape(cfg.nseq_s, cfg.past, 512),
            "state_hgrn": f(inputs["state_hgrn"][0, ss_]),
            "norm1": f(inputs["norm1"]).reshape(1, D),
            "w_in": f(inputs["w_in"][0]),
            "lam_params": f(inputs["lam_params"]).reshape(1, 256),
            "a_subln": f(inputs["a_subln"]).reshape(1, 128),
            "r_lb_logits": f(inputs["r_lb_logits"]),
            "r_gnorm": f(inputs["r_gnorm"]).reshape(1, 128),
            "w_a": f(inputs["w_a"][0]),
            "w_b": f(inputs["w_b"][0]),
            "w_out": f(inputs["w_out"][0]),
            "norm2": f(inputs["norm2"]).reshape(1, D),
            "p_wq": f(inputs["p_wq"][0]),
            "p_keys": f(inputs["p_keys"][0]).reshape(16, 128, 128),
            "p_downT": shared["p_downT"],
            "p_up": f(inputs["p_up"][0]),
            "final_norm": f(inputs["final_norm"]).reshape(1, D),
        }
        maps.append(m)
    return maps


def run(inputs, cfg, ncores):
    nc = build(cfg)
    maps = make_in_maps(inputs, cfg, ncores)
    res = run_bass_kernel_spmd(nc, maps, core_ids=list(range(ncores)))
    outs = []
    for name in _OUT_NAMES:
        parts = [np.asarray(r[name]) for r in res.results]
        outs.append(np.concatenate(parts, axis=0))
    return outs


def kernel(**inputs):
    cfg = Cfg()
    yp, ys, kp, vp, sp_, ks, vs, ss_ = run(inputs, cfg, NCORES)
    B, T = 32, cfg.t_p
    return (yp.reshape(B, T, D), ys.reshape(32, cfg.t_s, D),
            kp.reshape(1, B, T, 4, 128), vp.reshape(1, B, T, 4, 128), sp_.reshape(1, B, 4, 128, 128),
            ks.reshape(1, 32, cfg.t_s, 4, 128), vs.reshape(1, 32, cfg.t_s, 4, 128), ss_.reshape(1, 32, 4, 128, 128))
```

```python
import math
import numpy as np
import concourse.bass as bass
import concourse.mybir as mybir
from concourse.bass_utils import run_bass_kernel_spmd

F32 = mybir.dt.float32
BF16 = mybir.dt.bfloat16
I32 = mybir.dt.int32
U32 = mybir.dt.uint32
AF = mybir.ActivationFunctionType
OP = mybir.AluOpType
AX = mybir.AxisListType

D = 1024
EPS = 1e-6
NCORES = 8
IN_COLS = 5632


class Cfg:
    def __init__(self, nseq_p=4, t_p=2048, nseq_s=4, t_s=32, past=1024, stage=99, nchunk=128):
        self.nseq_p, self.t_p, self.nseq_s, self.t_s, self.past = nseq_p, t_p, nseq_s, t_s, past
        self.stage = stage
        self.nchunk = nchunk


class Buf:
    __slots__ = ("name", "w", "r", "dsem", "dcnt", "excl")

    def __init__(self, name="", excl=False):
        self.name = name
        self.excl = excl
        self.w = {}
        self.r = {}
        self.dsem = None
        self.dcnt = 0


class Sched:
    ENGS = ("pe", "act", "dve", "pool", "sp")

    def __init__(self, nc):
        self.nc = nc
        self.eng = dict(pe=nc.tensor, act=nc.scalar, dve=nc.vector, pool=nc.gpsimd, sp=nc.sync)
        self.sem = {e: nc.alloc_semaphore("c_" + e) for e in self.ENGS}
        self.cnt = {e: 0 for e in self.ENGS}
        self.seen = {e: {} for e in self.ENGS}
        self.dsems = {}
        self.ninst = 0
        self.out_tokens = {}

    def _semof(self, key):
        if key in self.sem:
            return self.sem[key]
        return self.dsems[key][0]

    def _cur(self, key, val):
        if key in self.dsems:
            return max(val, self.dsems[key][1].dcnt * 16)
        return val

    def _wait(self, eng, deps):
        e = self.eng[eng]
        for key, val in deps.items():
            if key == eng and eng == "pe":
                continue
            val = self._cur(key, val)
            if self.seen[eng].get(key, 0) >= val:
                continue
            e.wait_ge(self._semof(key), val)
            self.seen[eng][key] = val
            self.ninst += 1

    @staticmethod
    def _merge(deps, d):
        for k, v in d.items():
            if deps.get(k, 0) < v:
                deps[k] = v

    def _deps(self, reads, writes, eng=None):
        deps = {}
        for b in reads:
            self._merge(deps, b.w)
            if b.excl:
                self._merge(deps, {k: v for k, v in b.r.items() if k != eng})
        for b in writes:
            self._merge(deps, b.w)
            self._merge(deps, b.r)
        return deps

    def op(self, eng, fn, reads=(), writes=()):
        self._wait(eng, self._deps(reads, writes, eng))
        ins = fn(self.eng[eng])
        self.cnt[eng] += 1
        n = self.cnt[eng]
        ins.then_inc(self.sem[eng], 1)
        self.ninst += 1
        for b in reads:
            if b.r.get(eng, 0) < n:
                b.r[eng] = n
        for b in writes:
            b.w = {eng: n}
            b.r = {}
        return ins

    def dma(self, eng, out, in_, reads=(), writes=(), sb=None, fn=None, is_output=False):
        if sb is None:
            sb = writes[0] if writes else reads[0]
        if sb.dsem is None:
            sb.dsem = self.nc.alloc_semaphore("d_%d" % len(self.dsems))
            self.dsems[id(sb)] = (sb.dsem, sb)
        key = id(sb)
        self._wait(eng, self._deps(reads, writes))
        if fn is None:
            ins = self.eng[eng].dma_start(out=out, in_=in_)
        else:
            ins = fn(self.eng[eng])
        sb.dcnt += 1
        val = sb.dcnt * 16
        ins.then_inc(sb.dsem, 16)
        self.ninst += 1
        for b in reads:
            b.r[key] = val
        for b in writes:
            b.w = {key: val}
            b.r = {}
        if is_output:
            self.out_tokens[key] = val
        return ins

    def barrier(self):
        deps = {key: b.dcnt * 16 for key, (sem, b) in self.dsems.items()}
        for k in self.ENGS:
            if self.cnt[k]:
                deps[k] = self.cnt[k]
        for eng in self.ENGS:
            self._wait(eng, {k: v for k, v in deps.items() if k != eng})

    def finish(self, eng="sp"):
        deps = {key: b.dcnt * 16 for key, (sem, b) in self.dsems.items()}
        for k in ("pe", "act", "dve", "pool"):
            if self.cnt[k]:
                deps[k] = self.cnt[k]
        self._wait(eng, deps)


def build(cfg):
    nc = bass.Bass("TRN2", target_bir_lowering=False)
    NP, TP, NS, TS, PAST = cfg.nseq_p, cfg.t_p, cfg.nseq_s, cfg.t_s, cfg.past
    S = Sched(nc)

    def din(name, shape, dt=F32):
        return nc.dram_tensor(name, list(shape), dt, kind="ExternalInput").ap()

    def dout(name, shape, dt=F32):
        return nc.dram_tensor(name, list(shape), dt, kind="ExternalOutput").ap()

    x_p = din("x_prompt", [NP, TP, D])
    x_s = din("x_sample", [NS, TS, D])
    ck = din("cache_k", [NS, PAST, 512])
    cv = din("cache_v", [NS, PAST, 512])
    st = din("state_hgrn", [NS, 4, 128, 128])
    norm1 = din("norm1", [1, D])
    w_in = din("w_in", [D, IN_COLS])
    lam_params = din("lam_params", [1, 256])
    a_subln = din("a_subln", [1, 128])
    r_lb = din("r_lb_logits", [2, 512])
    r_gnorm = din("r_gnorm", [1, 128])
    w_a = din("w_a", [512, D])
    w_b = din("w_b", [512, D])
    w_out = din("w_out", [D, D])
    norm2 = din("norm2", [1, D])
    p_wq = din("p_wq", [D, 2048])
    p_keys = din("p_keys", [16, 128, 128])
    p_downT = din("p_downT", [128, 128, D])
    p_up = din("p_up", [16384, D])
    final_norm = din("final_norm", [1, D])

    y_p = dout("y_prompt", [NP, TP, D])
    y_s = dout("y_sample", [NS, TS, D])
    k_p = dout("k_prompt", [NP, TP, 512])
    v_p = dout("v_prompt", [NP, TP, 512])
    s_p = dout("s_prompt", [NP, 4, 128, 128])
    k_s = dout("k_sample", [NS, TS, 512])
    v_s = dout("v_sample", [NS, TS, 512])
    s_s = dout("s_sample", [NS, 4, 128, 128])

    import contextlib
    es = contextlib.ExitStack()

    def sb(name, shape, dt=F32):
        return es.enter_context(nc.sbuf_tensor(name, list(shape), dt))

    def ps(name, shape, dt=F32):
        return es.enter_context(nc.psum_tensor(name, list(shape), dt))

    with es:
        NKT = max(TP // 128, PAST // 128 + 1)
        NTOK = NP * TP + NS * TS

        def PE(fn, r=(), w=()):
            return S.op("pe", fn, reads=r, writes=w)

        def ACT(fn, r=(), w=()):
            return S.op("act", fn, reads=r, writes=w)

        def DVE(fn, r=(), w=()):
            return S.op("dve", fn, reads=r, writes=w)

        def POOL(fn, r=(), w=()):
            return S.op("pool", fn, reads=r, writes=w)

        es_a = contextlib.ExitStack()

        def sba(name, shape, dt=F32):
            return es_a.enter_context(nc.sbuf_tensor(name, list(shape), dt))

        ident_f = sb("ident_f", [128, 128], F32)
        ident_b = sb("ident_b", [128, 128], BF16)
        tri_f = sba("tri_f", [128, 128], F32)
        up_f = sba("up_f", [128, 128], F32)
        B_c = Buf("consts")
        POOL(lambda e: e.memset(ident_f[:], 0.0), w=[B_c])
        POOL(lambda e: e.affine_select(out=ident_f[:], in_=ident_f[:], pattern=[[-1, 128]], compare_op=OP.not_equal,
                                       fill=1.0, base=0, channel_multiplier=1), r=[B_c], w=[B_c])
        POOL(lambda e: e.memset(tri_f[:], 1.0), w=[B_c])
        POOL(lambda e: e.affine_select(out=tri_f[:], in_=tri_f[:], pattern=[[1, 128]], compare_op=OP.is_ge,
                                       fill=0.0, base=0, channel_multiplier=-1), r=[B_c], w=[B_c])
        POOL(lambda e: e.memset(up_f[:], 1.0), w=[B_c])
        POOL(lambda e: e.affine_select(out=up_f[:], in_=up_f[:], pattern=[[-1, 128]], compare_op=OP.is_gt,
                                       fill=0.0, base=0, channel_multiplier=1), r=[B_c], w=[B_c])
        DVE(lambda e: e.tensor_copy(out=ident_b[:], in_=ident_f[:]), r=[B_c], w=[B_c])

        def bcast_load(name, src, width, alloc=None):
            t = (alloc or sba)(name, [128, width], F32)
            S.dma("sp", t[:], src.partition_broadcast(128), writes=[B_c])
            return t

        n1b = bcast_load("n1b", norm1, D)
        lpb = bcast_load("lpb", lam_params, 256)
        asub_b = bcast_load("asub_b", a_subln, 128)
        gn_b = bcast_load("gn_b", r_gnorm, 128)
        lb0 = bcast_load("lb0", r_lb[0:1, :], 512)
        lb1 = bcast_load("lb1", r_lb[1:2, :], 512)
        oml_b = sba("oml_b", [128, 512], F32)
        DVE(lambda e: e.tensor_tensor(out=lb0[:], in0=lb0[:], in1=lb1[:], op=OP.subtract), r=[B_c], w=[B_c])
        ACT(lambda e: e.activation(out=lb0[:], in_=lb0[:], func=AF.Sigmoid), r=[B_c], w=[B_c])
        DVE(lambda e: e.tensor_scalar(out=oml_b[:], in0=lb0[:], scalar1=-1.0, scalar2=1.0, op0=OP.mult, op1=OP.add),
            r=[B_c], w=[B_c])
        lb_b = lb0
        lam_s = sba("lam_s", [128, 4], F32)
        lam_j = sba("lam_j", [128, 64], F32)
        neg_lam = sba("neg_lam", [128, 1], F32)
        DVE(lambda e: e.scalar_tensor_tensor(out=lam_j[:], in0=lpb[:, 0:64], scalar=1.0, in1=lpb[:, 64:128],
                                             op0=OP.mult, op1=OP.mult, accum_out=lam_s[:, 0:1]), r=[B_c], w=[B_c])
        DVE(lambda e: e.scalar_tensor_tensor(out=lam_j[:], in0=lpb[:, 128:192], scalar=1.0, in1=lpb[:, 192:256],
                                             op0=OP.mult, op1=OP.mult, accum_out=lam_s[:, 1:2]), r=[B_c], w=[B_c])
        ACT(lambda e: e.activation(out=lam_s[:, 2:4], in_=lam_s[:, 0:2], func=AF.Exp), r=[B_c], w=[B_c])
        DVE(lambda e: e.tensor_tensor(out=neg_lam[:], in0=lam_s[:, 3:4], in1=lam_s[:, 2:3], op=OP.subtract), r=[B_c], w=[B_c])
        DVE(lambda e: e.tensor_scalar(out=neg_lam[:], in0=neg_lam[:], scalar1=-0.2, scalar2=None, op0=OP.add), r=[B_c], w=[B_c])
        DVE(lambda e: e.tensor_scalar(out=asub_b[:], in0=asub_b[:], scalar1=0.8, scalar2=None, op0=OP.mult), r=[B_c], w=[B_c])

        KSTOP = ""
        stage_ref = {}
        B_w = Buf("weights")
        kk = [0]

        def load_w(dst3, src2, nchunk, ncols):
            v = src2.rearrange("(c p) n -> p c n", p=128)
            for c in range(nchunk):
                for c0 in range(0, ncols, 1024):
                    w_ = min(1024, ncols - c0)
                    bi = kk[0] % 2
                    stage_bufs, B_stage = stage_ref["bufs"], stage_ref["B"]
                    S.dma("sp", stage_bufs[bi][:, :w_], v[:, c, c0:c0 + w_], writes=[B_stage[bi]])
                    if kk[0] % 2 == 0:
                        DVE(lambda e, bi=bi, c=c, c0=c0, w_=w_: e.tensor_copy(out=dst3[:, c, c0:c0 + w_], in_=stage_bufs[bi][:, :w_]),
                            r=[B_stage[bi]], w=[B_w])
                    else:
                        ACT(lambda e, bi=bi, c=c, c0=c0, w_=w_: e.copy(out=dst3[:, c, c0:c0 + w_], in_=stage_bufs[bi][:, :w_]),
                            r=[B_stage[bi]], w=[B_w])
                    kk[0] += 1

        pbank = [ps("pb%d" % i, [128, 512], F32) for i in range(8)]
        B_pb = [Buf("pb%d" % i, excl=True) for i in range(8)]

        def pb_bf(bk):
            return pbank[bk].bitcast(BF16)

        rr = [0]

        def next_bank():
            b_ = rr[0]
            rr[0] = (b_ + 1) % 4
            return b_

        NCH = cfg.nchunk
        dnT_bf = nc.dram_tensor("dnT_bf", [128, 128, D], BF16, kind="Internal").ap()
        up_bf = nc.dram_tensor("up_bf", [16384, D], BF16, kind="Internal").ap()
        B_pre = [Buf("pre%d" % i) for i in range(4)]
        pre_k = [0]

        def prepass(cnt):
            for _ in range(cnt):
                i_ = pre_k[0]
                if i_ >= 2 * NCH:
                    return
                pre_k[0] += 1
                c = i_ // 2
                if i_ % 2 == 0:
                    S.dma("pool", dnT_bf[c], p_downT[c], writes=[B_pre[i_ % 4]])
                else:
                    S.dma("pool", up_bf[c * 128:(c + 1) * 128, :], p_up[c * 128:(c + 1) * 128, :], writes=[B_pre[i_ % 4]])

        tiles = []
        tok0 = 0
        for b in range(NP):
            for i in range(TP // 128):
                tiles.append(("p", b, i, 128, tok0))
                tok0 += 128
        for b in range(NS):
            tiles.append(("s", b, 0, TS, tok0))
            tok0 += TS

        def x_src(t):
            kind, b, i, n, _ = t
            if kind == "p":
                return x_p[b, i * 128:(i + 1) * 128, :]
            return x_s[b, :, :]

        with es_a:
            w_in_sb = sba("w_in_sb", [128, 8, IN_COLS], BF16)

            xt = [sba("xt%d" % i, [128, D], F32) for i in range(2)]
            B_xt = [Buf("xt%d" % i) for i in range(2)]
            stage_ref["bufs"], stage_ref["B"] = xt, B_xt
            load_w(w_in_sb, w_in, 8, IN_COLS)
            ss = sba("ss", [128, 1], F32)
            rstd = sba("rstd", [128, 1], F32)
            B_ss, B_rstd = Buf("ss"), Buf("rstd")
            xn = sba("xn", [128, D], BF16)
            B_xn = Buf("xn")
            junk, B_junk = xn, B_xn
            xnT = sba("xnT", [128, 8, 128], BF16)
            B_xnT = Buf("xnT")
            kout = [sba("kout0", [128, 512], F32)] * 2
            vout = [sba("vout0", [128, 512], F32)] * 2
            B_kout = [Buf("kout0")] * 2
            B_vout = [Buf("vout0")] * 2
            qa_bf = sba("qa_bf", [128, 512], BF16)
            ka_bf = sba("ka_bf", [128, 512], BF16)
            B_qa, B_ka = Buf("qa_bf"), Buf("ka_bf")
            QT = sba("QT", [128, 4, 128], BF16)
            B_QT = Buf("QT")
            KT = sba("KT", [128, 4, NKT * 128], BF16)
            B_KT = [Buf("KT%d" % j) for j in range(NKT)]
            Vaug = sba("Vaug", [128, NKT, 4, 130], BF16)
            B_V = [Buf("V%d" % j) for j in range(NKT)]
            PT = [sba("PT%d" % i, [128, 512], BF16) for i in range(2)]
            B_PT = [Buf("PT%d" % i) for i in range(2)]
            rz = sba("rz", [128, 2], F32)
            rz1n = sba("rz1n", [128, 1], F32)
            B_rz = Buf("rz")
            t1 = sba("t1", [128, 128], F32)
            B_t1 = Buf("t1")
            oa = sba("oa", [128, 4, 128], F32)
            B_oa = Buf("oa")
            midb = sba("midb", [128, 3072], BF16)
            hss = sba("hss", [128, 4], F32)
            B_hss = Buf("hss")
            oa_bf = midb[:, 0:512]
            B_oabf = Buf("oa_bf")
            sig = sba("sig", [128, 512], F32)
            logf = sba("logf", [128, 512], F32)
            kr = sba("kr", [128, 512], F32)
            kr_bf = sba("kr_bf", [128, 512], BF16)
            qr_bf = sba("qr_bf", [128, 512], BF16)
            iv_bf = sba("iv_bf", [128, 512], BF16)
            sog = sba("sog", [128, 512], BF16)
            sq = sig[:].rearrange("p (h v) -> p h v", h=4)
            B_sig, B_logf, B_kr, B_krbf, B_qr, B_iv, B_sog = (Buf(x) for x in ("sig", "logf", "kr", "krbf", "qr", "iv", "sog"))
            B_sq = B_sig
            ebmb = sba("ebmb", [128, 512], F32)
            k_out = sba("k_out", [128, 512], BF16)
            B_ebmb, B_kout2 = Buf("ebmb"), Buf("k_out")
            ebT = sba("ebT", [128, 4, 128], F32)
            enbT = sba("enbT", [128, 4, 128], F32)
            B_ebT, B_enbT = Buf("ebT"), Buf("enbT")
            q_inT = sba("q_inT", [128, 4, 128], BF16)
            k_inT = sba("k_inT", [128, 4, 128], BF16)
            B_qinT, B_kinT = Buf("q_inT"), Buf("k_inT")
            ATm = [sba("ATm%d" % i, [128, 128], BF16) for i in range(2)]
            B_ATm = [Buf("ATm%d" % i) for i in range(2)]
            Sst = sba("Sst", [128, 4, 128], F32)
            S_bf = sba("S_bf", [128, 4, 128], BF16)
            B_S = [Buf("S%d" % h) for h in range(4)]
            B_Sbf = [Buf("Sbf%d" % h) for h in range(4)]
            orr = sba("orr", [128, 4, 128], F32)
            B_orr = Buf("orr")
            orr_bf = midb[:, 512:1024]
            B_orrbf = Buf("orr_bf")
            sgA = midb[:, 1024:2048]
            sgB = midb[:, 2048:3072]
            B_sgA, B_sgB = Buf("sgA"), Buf("sgB")
            cst = [midb[:, 1024:2048].bitcast(F32), midb[:, 2048:3072].bitcast(F32)]
            B_cst = [B_sgA, B_sgB]
            dbg = sba("dbg", [128, D], F32) if cfg.stage == 2 else None
            B_dbg = Buf("dbg")
            mid = nc.dram_tensor("mid_scratch", [NTOK, 3072], BF16, kind="Internal").ap()
            B_mid = Buf("mid")

            if KSTOP == "weights":
                S.finish("sp"); S.eng["sp"].wait_ge(S.sem["dve"], S.cnt["dve"]); S.eng["sp"].wait_ge(S.sem["act"], S.cnt["act"]); S.eng["sp"].wait_ge(S.sem["pool"], S.cnt["pool"])
                return nc
            POOL(lambda e: e.memset(Vaug[:, :, :, 128:130], 1.0), w=B_V)

            if KSTOP == "vaug":
                S.finish("sp"); S.eng["sp"].wait_ge(S.sem["dve"], S.cnt["dve"]); S.eng["sp"].wait_ge(S.sem["act"], S.cnt["act"]); S.eng["sp"].wait_ge(S.sem["pool"], S.cnt["pool"])
                return nc
            S.dma("sp", xt[0][:tiles[0][3], :], x_src(tiles[0]), writes=[B_xt[0]])
            cstk = [0]

            def seq_start(kind, b):
                if kind == "p":
                    for h in range(4):
                        POOL(lambda e, h=h: e.memset(Sst[:, h, :], 0.0), w=[B_S[h]])
                        POOL(lambda e, h=h: e.memset(S_bf[:, h, :], 0.0), w=[B_Sbf[h]])
                    return
                S.dma("sp", Sst[:], st[b].rearrange("h d v -> d h v"), writes=B_S)
                for h in range(4):
                    ACT(lambda e, h=h: e.copy(out=S_bf[:, h, :], in_=Sst[:, h, :]), r=[B_S[h]], w=[B_Sbf[h]])
                for j in range(PAST // 128):
                    ci = cstk[0] % 2
                    cstk[0] += 1
                    S.dma("sp", cst[ci][:], ck[b, j * 128:(j + 1) * 128, :], writes=[B_cst[ci]])
                    bk = next_bank()
                    for h in range(4):
                        PE(lambda e, h=h, bk=bk, ci=ci: e.transpose(out=pbank[bk][:, h * 128:(h + 1) * 128],
                                                                    in_=cst[ci][:, h * 128:(h + 1) * 128], identity=ident_f[:]),
                           r=[B_cst[ci], B_c], w=[B_pb[bk]])
                    DVE(lambda e, bk=bk, j=j: e.tensor_copy(out=KT[:, :, j * 128:(j + 1) * 128],
                                                           in_=pbank[bk].rearrange("p (h t) -> p h t", h=4)),
                        r=[B_pb[bk]], w=[B_KT[j]])
                    ci = cstk[0] % 2
                    cstk[0] += 1
                    S.dma("sp", cst[ci][:], cv[b, j * 128:(j + 1) * 128, :], writes=[B_cst[ci]])
                    ACT(lambda e, j=j, ci=ci: e.copy(out=Vaug[:, j, :, 0:128], in_=cst[ci].rearrange("p (h v) -> p h v", h=4)),
                        r=[B_cst[ci]], w=[B_V[j]])

            def mixer_tile(ti, t):
                kind, b, i, n, tk0 = t
                cur = ti % 2
                if i == 0 and KSTOP != "noseq":
                    seq_start(kind, b)
                if ti + 1 < len(tiles):
                    nt = tiles[ti + 1]
                    S.dma("sp", xt[1 - cur][:nt[3], :], x_src(nt), writes=[B_xt[1 - cur]])
                X, BX = xt[cur], B_xt[cur]
                jn = i if kind == "p" else PAST // 128
                ACT(lambda e: e.activation(out=junk[:n, :], in_=X[:n, :], func=AF.Square, accum_out=ss[:n, :]),
                    r=[BX], w=[B_junk, B_ss])
                ACT(lambda e: e.activation(out=rstd[:n, :], in_=ss[:n, :], func=AF.Sqrt, scale=1.0 / D, bias=EPS),
                    r=[B_ss], w=[B_rstd])
                DVE(lambda e: e.reciprocal(out=rstd[:n, :], in_=rstd[:n, :]), r=[B_rstd], w=[B_rstd])
                DVE(lambda e: e.scalar_tensor_tensor(out=xn[:n, :], in0=X[:n, :], scalar=rstd[:n, :], in1=n1b[:n, :],
                                                     op0=OP.mult, op1=OP.mult), r=[BX, B_rstd, B_c], w=[B_xn])
                bk = next_bank()
                for c in range(8):
                    PE(lambda e, c=c, bk=bk: e.transpose(out=pb_bf(bk)[:, c * 128:c * 128 + n], in_=xn[:n, c * 128:(c + 1) * 128],
                                                         identity=ident_b[:n, :n]), r=[B_xn, B_c], w=[B_pb[bk]])
                DVE(lambda e, bk=bk: e.tensor_copy(out=xnT[:, :, :n], in_=pb_bf(bk).rearrange("p (c t) -> p c t", c=8)[:, :, :n]),
                    r=[B_pb[bk]], w=[B_xnT])

                def zgroup(g):
                    bk = next_bank()
                    for c in range(8):
                        PE(lambda e, c=c, bk=bk: e.matmul(pbank[bk][:n, :], lhsT=xnT[:, c, :n], rhs=w_in_sb[:, c, g * 512:(g + 1) * 512],
                                                          start=(c == 0), stop=(c == 7)), r=[B_xnT, B_w], w=[B_pb[bk]])
                    return bk

                def transpose4(src_bf, Bsrc, dst_fn, Bdst_list, evac_eng="dve", mul=None, Bmul=None):
                    bk = next_bank()
                    for h in range(4):
                        PE(lambda e, h=h, bk=bk: e.transpose(out=pb_bf(bk)[:, h * 128:h * 128 + n], in_=src_bf[:n, h * 128:(h + 1) * 128],
                                                             identity=ident_b[:n, :n]), r=[Bsrc, B_c], w=[B_pb[bk]])
                    src = pb_bf(bk)[:, 0:512].rearrange("p (h t) -> p h t", h=4)[:, :, :n]
                    if mul is not None:
                        DVE(lambda e: e.tensor_tensor(out=dst_fn(), in0=src, in1=mul, op=OP.mult), r=[B_pb[bk], Bmul], w=Bdst_list)
                    elif evac_eng == "dve":
                        DVE(lambda e: e.tensor_copy(out=dst_fn(), in_=src), r=[B_pb[bk]], w=Bdst_list)
                    else:
                        ACT(lambda e: e.copy(out=dst_fn(), in_=src), r=[B_pb[bk]], w=Bdst_list)

                if KSTOP == "t_xnT":
                    return
                bk = zgroup(0)
                ACT(lambda e, bk=bk: e.copy(out=qa_bf[:n, :], in_=pbank[bk][:n, :]), r=[B_pb[bk]], w=[B_qa])
                transpose4(qa_bf, B_qa, lambda: QT[:, :, :n], [B_QT])
                if KSTOP == "t_g0":
                    return
                bk = zgroup(1)
                ACT(lambda e, bk=bk: e.copy(out=kout[cur][:n, :], in_=pbank[bk][:n, :]), r=[B_pb[bk]], w=[B_kout[cur]])
                DVE(lambda e, bk=bk: e.tensor_copy(out=ka_bf[:n, :], in_=kout[cur][:n, :]), r=[B_kout[cur]], w=[B_ka])
                dst = k_p[b, i * 128:(i + 1) * 128, :] if kind == "p" else k_s[b, :, :]
                S.dma("sp", dst, kout[cur][:n, :], reads=[B_kout[cur]], sb=B_kout[cur], is_output=True)
                transpose4(ka_bf, B_ka, lambda: KT[:, :, jn * 128:jn * 128 + n], [B_KT[jn]])
                if KSTOP == "t_g1":
                    return
                bk = zgroup(2)
                DVE(lambda e, bk=bk: e.tensor_copy(out=vout[cur][:n, :], in_=pbank[bk][:n, :]), r=[B_pb[bk]], w=[B_vout[cur]])
                ACT(lambda e, bk=bk: e.copy(out=Vaug[:n, jn, :, 0:128], in_=pbank[bk].rearrange("p (h v) -> p h v", h=4)[:n]),
                    r=[B_pb[bk]], w=[B_V[jn]])
                dst = v_p[b, i * 128:(i + 1) * 128, :] if kind == "p" else v_s[b, :, :]
                S.dma("sp", dst, vout[cur][:n, :], reads=[B_vout[cur]], sb=B_vout[cur], is_output=True)
                if cfg.stage <= 1:
                    return
                bk = zgroup(3)
                ACT(lambda e, bk=bk: e.activation(out=sig[:n, :], in_=pbank[bk][:n, :], func=AF.Sigmoid), r=[B_pb[bk]], w=[B_sig])
                DVE(lambda e: e.tensor_tensor(out=sig[:n, :], in0=sig[:n, :], in1=oml_b[:n, :], op=OP.mult), r=[B_sig, B_c], w=[B_sig])
                DVE(lambda e: e.tensor_tensor(out=sig[:n, :], in0=sig[:n, :], in1=lb_b[:n, :], op=OP.add), r=[B_sig, B_c], w=[B_sig])
                ACT(lambda e: e.activation(out=logf[:n, :], in_=sig[:n, :], func=AF.Ln), r=[B_sig], w=[B_logf])
                DVE(lambda e: e.tensor_scalar(out=kr[:n, :], in0=sig[:n, :], scalar1=-1.0, scalar2=1.0, op0=OP.mult, op1=OP.add),
                    r=[B_sig], w=[B_kr])
                DVE(lambda e: e.tensor_copy(out=kr_bf[:n, :], in_=kr[:n, :]), r=[B_kr], w=[B_krbf])
                bk = zgroup(4)
                ACT(lambda e, bk=bk: e.activation(out=qr_bf[:n, :], in_=pbank[bk][:n, :], func=AF.Silu), r=[B_pb[bk]], w=[B_qr])
                bk = zgroup(5)
                DVE(lambda e, bk=bk: e.tensor_copy(out=iv_bf[:n, :], in_=pbank[bk][:n, :]), r=[B_pb[bk]], w=[B_iv])
                bk = zgroup(6)
                ACT(lambda e, bk=bk: e.activation(out=sog[:n, :], in_=pbank[bk][:n, :], func=AF.Silu), r=[B_pb[bk]], w=[B_sog])
                for gi in range(2):
                    bk = zgroup(7 + gi)
                    ACT(lambda e, bk=bk, gi=gi: e.activation(out=sgA[:n, gi * 512:(gi + 1) * 512], in_=pbank[bk][:n, :], func=AF.Sigmoid),
                        r=[B_pb[bk]], w=[B_sgA])
                for gi in range(2):
                    bk = zgroup(9 + gi)
                    ACT(lambda e, bk=bk, gi=gi: e.activation(out=sgB[:n, gi * 512:(gi + 1) * 512], in_=pbank[bk][:n, :], func=AF.Sigmoid),
                        r=[B_pb[bk]], w=[B_sgB])

                if kind == "p":
                    ktiles = [(j, 128) for j in range(i + 1)]
                else:
                    ktiles = [(j, 128) for j in range(PAST // 128)] + [(PAST // 128, n)]
                groups = []
                g_ = []
                for (j, kn) in ktiles:
                    if kn < 128 and g_:
                        groups.append(g_)
                        g_ = []
                    g_.append((j, kn))
                    if len(g_) == 4:
                        groups.append(g_)
                        g_ = []
                if g_:
                    groups.append(g_)
                items = []
                for h in range(4):
                    for m in range(2):
                        for gi, grp in enumerate(groups):
                            items.append((h, m, gi, grp))

                def emit_scores(k):
                    h, m, gi, grp = items[k]
                    ps_ = slice(m * 64, (m + 1) * 64)
                    sbk = 4 + (k % 2)
                    pt = k % 2
                    for jj, (j, kn) in enumerate(grp):
                        PE(lambda e, jj=jj, j=j, kn=kn, sbk=sbk, ps_=ps_, h=h: e.matmul(
                            pbank[sbk][:kn, jj * 128:jj * 128 + n], lhsT=KT[ps_, h, j * 128:j * 128 + kn],
                            rhs=QT[ps_, h, :n], start=True, stop=True),
                           r=[B_KT[j], B_QT], w=[B_pb[sbk]])
                    knmax = max(kn for _, kn in grp)
                    nb_ = len(grp)
                    ACT(lambda e, sbk=sbk, pt=pt, knmax=knmax, nb_=nb_: e.activation(
                        out=PT[pt].rearrange("p (j t) -> p j t", t=128)[:knmax, :nb_, :n],
                        in_=pbank[sbk].rearrange("p (j t) -> p j t", t=128)[:knmax, :nb_, :n], func=AF.Exp, scale=0.125),
                        r=[B_pb[sbk]], w=[B_PT[pt]])
                    for jj, (j, kn) in enumerate(grp):
                        if kind == "p" and j == i:
                            DVE(lambda e, jj=jj, pt=pt: e.memset(PT[pt][64:128, jj * 128:jj * 128 + 64], 0.0),
                                r=[B_PT[pt]], w=[B_PT[pt]])

                def emit_pv(k):
                    h, m, gi, grp = items[k]
                    ob = 6 + (h % 2)
                    pt = k % 2
                    for jj, (j, kn) in enumerate(grp):
                        first = (gi == 0 and jj == 0)
                        last = (gi == len(groups) - 1 and jj == len(grp) - 1)
                        PE(lambda e, jj=jj, j=j, kn=kn, pt=pt, first=first, last=last, ob=ob, m=m, h=h: e.matmul(
                            pbank[ob][:n, m * 256:m * 256 + 129], lhsT=PT[pt][:kn, jj * 128:jj * 128 + n],
                            rhs=Vaug[:kn, j, h, 0:129], start=first, stop=last),
                           r=[B_PT[pt], B_V[j]], w=[B_pb[ob]])
                    if m == 1 and gi == len(groups) - 1:
                        ov = pbank[ob].rearrange("p (m c) -> p m c", c=256)
                        DVE(lambda e, ov=ov: e.reciprocal(out=rz[:n, :], in_=ov[:n, :, 128]), r=[B_pb[ob]], w=[B_rz])
                        DVE(lambda e: e.tensor_tensor(out=rz1n[:n, :], in0=rz[:n, 1:2], in1=neg_lam[:n, :], op=OP.mult), r=[B_rz, B_c], w=[B_rz])
                        DVE(lambda e, ob=ob: e.tensor_scalar(out=t1[:n, :], in0=pbank[ob][:n, 256:384], scalar1=rz1n[:n, :], scalar2=None,
                                                            op0=OP.mult), r=[B_pb[ob], B_rz], w=[B_t1])
                        DVE(lambda e, ob=ob, h=h: e.scalar_tensor_tensor(out=oa[:n, h, :], in0=pbank[ob][:n, 0:128], scalar=rz[:n, 0:1],
                                                                         in1=t1[:n, :], op0=OP.mult, op1=OP.add),
                            r=[B_pb[ob], B_rz, B_t1], w=[B_oa])


                def head_norm(src, Bsrc, gain_b, dst_bf, Bdst, extra=None, Bextra=None):
                    DVE(lambda e: e.tensor_tensor(out=sq[:n], in0=src[:n], in1=src[:n], op=OP.mult), r=[Bsrc], w=[B_sq])
                    DVE(lambda e: e.tensor_reduce(out=hss[:n, :], in_=sq[:n], axis=AX.X, op=OP.add), r=[B_sq], w=[B_hss])
                    ACT(lambda e: e.activation(out=hss[:n, :], in_=hss[:n, :], func=AF.Sqrt, scale=1.0 / 128, bias=EPS), r=[B_hss], w=[B_hss])
                    DVE(lambda e: e.reciprocal(out=hss[:n, :], in_=hss[:n, :]), r=[B_hss], w=[B_hss])
                    DVE(lambda e: e.tensor_tensor(out=sq[:n], in0=src[:n], in1=hss[:n, :].unsqueeze(2).to_broadcast([n, 4, 128]), op=OP.mult),
                        r=[Bsrc, B_hss], w=[B_sq])
                    if extra is None:
                        DVE(lambda e: e.tensor_tensor(out=dst_bf[:n, :].rearrange("p (h v) -> p h v", h=4), in0=sq[:n],
                                                      in1=gain_b[:n, :].unsqueeze(1).to_broadcast([n, 4, 128]), op=OP.mult),
                            r=[B_sq, B_c], w=[Bdst])
                    else:
                        DVE(lambda e: e.tensor_tensor(out=sq[:n], in0=sq[:n], in1=gain_b[:n, :].unsqueeze(1).to_broadcast([n, 4, 128]), op=OP.mult),
                            r=[B_sq, B_c], w=[B_sq])
                        DVE(lambda e: e.tensor_tensor(out=dst_bf[:n, :], in0=sq[:n].rearrange("p h v -> p (h v)"), in1=extra[:n, :], op=OP.mult),
                            r=[B_sq, Bextra], w=[Bdst])

                def hgrn_gen():
                    bk = next_bank()
                    PE(lambda e, bk=bk: e.matmul(pbank[bk][:n, :], lhsT=up_f[:n, :n], rhs=logf[:n, :], start=True, stop=True),
                       r=[B_c, B_logf], w=[B_pb[bk]])
                    ACT(lambda e, bk=bk: e.activation(out=ebmb[:n, :], in_=pbank[bk][:n, :], func=AF.Exp), r=[B_pb[bk]], w=[B_ebmb])
                    DVE(lambda e: e.tensor_tensor(out=k_out[:n, :], in0=kr[:n, :], in1=ebmb[:n, :], op=OP.mult), r=[B_kr, B_ebmb], w=[B_kout2])
                    yield
                    bk = next_bank()
                    for h in range(4):
                        PE(lambda e, h=h, bk=bk: e.matmul(pbank[bk][:, h * 128:h * 128 + n], lhsT=logf[:n, h * 128:(h + 1) * 128],
                                                          rhs=tri_f[:n, :n], start=True, stop=True), r=[B_logf, B_c], w=[B_pb[bk]])
                    bTv = pbank[bk].rearrange("p (h t) -> p h t", h=4)[:, :, :n]
                    ACT(lambda e: e.activation(out=ebT[:, :, :n], in_=bTv, func=AF.Exp), r=[B_pb[bk]], w=[B_ebT])
                    ACT(lambda e: e.activation(out=enbT[:, :, :n], in_=bTv, func=AF.Exp, scale=-1.0), r=[B_pb[bk]], w=[B_enbT])
                    yield
                    transpose4(qr_bf, B_qr, lambda: q_inT[:, :, :n], [B_qinT], mul=ebT[:, :, :n], Bmul=B_ebT)
                    yield
                    transpose4(kr_bf, B_krbf, lambda: k_inT[:, :, :n], [B_kinT], mul=enbT[:, :, :n], Bmul=B_enbT)
                    yield
                    for h in range(4):
                        bk = next_bank()
                        am = h % 2
                        hs_ = slice(h * 128, (h + 1) * 128)
                        PE(lambda e, h=h, bk=bk: e.matmul(pbank[bk][:n, 0:n], lhsT=k_inT[:, h, :n], rhs=q_inT[:, h, :n], start=True, stop=True),
                           r=[B_kinT, B_qinT], w=[B_pb[bk]])
                        DVE(lambda e, bk=bk, am=am: e.tensor_tensor(out=ATm[am][:n, :n], in0=pbank[bk][:n, 0:n], in1=tri_f[:n, :n], op=OP.mult),
                            r=[B_pb[bk], B_c], w=[B_ATm[am]])
                        yield
                        PE(lambda e, h=h, bk=bk, am=am: e.matmul(pbank[bk][:n, 128:256], lhsT=ATm[am][:n, :n], rhs=iv_bf[:n, hs_], start=True, stop=False),
                           r=[B_ATm[am], B_iv], w=[B_pb[bk]])
                        PE(lambda e, h=h, bk=bk: e.matmul(pbank[bk][:n, 128:256], lhsT=q_inT[:, h, :n], rhs=S_bf[:, h, :], start=False, stop=True),
                           r=[B_qinT, B_Sbf[h]], w=[B_pb[bk]])
                        PE(lambda e, h=h, bk=bk: e.matmul(pbank[bk][:, 256:384], lhsT=k_out[:n, hs_], rhs=iv_bf[:n, hs_], start=True, stop=True),
                           r=[B_kout2, B_iv], w=[B_pb[bk]])
                        ACT(lambda e, h=h, bk=bk: e.copy(out=orr[:n, h, :], in_=pbank[bk][:n, 128:256]), r=[B_pb[bk]], w=[B_orr])
                        DVE(lambda e, h=h, bk=bk: e.scalar_tensor_tensor(out=Sst[:, h, :], in0=Sst[:, h, :], scalar=ebT[:, h, n - 1:n],
                                                                         in1=pbank[bk][:, 256:384], op0=OP.mult, op1=OP.add),
                            r=[B_S[h], B_ebT, B_pb[bk]], w=[B_S[h]])
                        DVE(lambda e, h=h: e.tensor_copy(out=S_bf[:, h, :], in_=Sst[:, h, :]), r=[B_S[h]], w=[B_Sbf[h]])
                hg = hgrn_gen()
                emit_scores(0)
                for k in range(len(items)):
                    if k + 1 < len(items):
                        emit_scores(k + 1)
                    emit_pv(k)
                    next(hg, None)
                head_norm(oa, B_oa, asub_b, oa_bf, B_oabf)
                for _ in hg:
                    pass
                head_norm(orr, B_orr, gn_b, orr_bf, B_orrbf, extra=sog, Bextra=B_sog)
                last_of_seq = (kind == "s") or (i == TP // 128 - 1)
                if last_of_seq:
                    dsts = s_p[b] if kind == "p" else s_s[b]
                    S.dma("sp", dsts.rearrange("h d v -> d h v"), Sst[:], reads=B_S, sb=B_S[0], is_output=True)

                if cfg.stage == 2:
                    DVE(lambda e: e.tensor_copy(out=dbg[:n, 0:512], in_=oa_bf[:n, :]), r=[B_oabf], w=[B_dbg])
                    DVE(lambda e: e.tensor_copy(out=dbg[:n, 512:1024], in_=orr_bf[:n, :]), r=[B_orrbf], w=[B_dbg])
                    dst = y_p[b, i * 128:(i + 1) * 128, :] if kind == "p" else y_s[b, :, :]
                    S.dma("sp", dst, dbg[:n, :], reads=[B_dbg], sb=B_dbg, is_output=True)
                S.dma("sp", mid[tk0:tk0 + n, :], midb[:n, :], reads=[B_oabf, B_orrbf, B_sgA, B_sgB], writes=[B_mid], sb=B_oabf)

            for ti, t in enumerate(tiles):
                prepass(4)
                mixer_tile(ti, t)
            prepass(2 * NCH)
        S.barrier()

        if cfg.stage < 3:
            S.finish("sp")
            print("ninst", S.ninst, S.cnt)
            return nc

        btiles = []
        for b in range(NP):
            for i in range(TP // 128):
                r0 = (b * (TP // 128) + i) * 128
                btiles.append((r0, 128, x_p[b, i * 128:(i + 1) * 128, :], y_p[b, i * 128:(i + 1) * 128, :]))
        xs_flat = x_s.rearrange("b t d -> (b t) d")
        ys_flat = y_s.rearrange("b t d -> (b t) d")
        for r in range(0, NS * TS, 128):
            n_ = min(128, NS * TS - r)
            btiles.append((NP * TP + r, n_, xs_flat[r:r + n_, :], ys_flat[r:r + n_, :]))
        h2s = nc.dram_tensor("h2_scratch", [NTOK, D], F32, kind="Internal").ap()
        B_h2s = Buf("h2s")

        es_b = contextlib.ExitStack()

        def sbb(name, shape, dt=F32):
            return es_b.enter_context(nc.sbuf_tensor(name, list(shape), dt))

        with es_b:
            xt2 = [sbb("xt2_%d" % i, [128, D], F32) for i in range(2)]
            B_xt2 = [Buf("xt2_%d" % i) for i in range(2)]
            stage_ref["bufs"], stage_ref["B"] = xt2, B_xt2
            w_a_sb = sbb("w_a_sb", [128, 4, D], BF16)
            w_b_sb = sbb("w_b_sb", [128, 4, D], BF16)
            w_out_sb = sbb("w_out_sb", [128, 8, D], BF16)
            load_w(w_a_sb, w_a, 4, D)
            load_w(w_b_sb, w_b, 4, D)
            load_w(w_out_sb, w_out, 8, D)
            midt = [sbb("midt%d" % i, [128, 3072], BF16) for i in range(2)]
            B_midt = [Buf("midt%d" % i) for i in range(2)]
            oT = sbb("oT", [128, 8, 128], BF16)
            B_oT = Buf("oT")
            ua = sbb("ua", [128, D], F32)
            ub = sbb("ub", [128, D], F32)
            B_ua, B_ub = Buf("ua"), Buf("ub")
            u_bf = sbb("u_bf", [128, D], BF16)
            B_ubf = Buf("u_bf")
            uT = sbb("uT", [128, 8, 128], BF16)
            B_uT = Buf("uT")

            def a2_load(k):
                r0, n, xsrc, _ = btiles[k]
                c_ = k % 2
                S.dma("sp", midt[c_][:n, :], mid[r0:r0 + n, :], reads=[B_mid], writes=[B_midt[c_]])
                S.dma("sp", xt2[c_][:n, :], xsrc, writes=[B_xt2[c_]])

            a2_load(0)
            for k, (r0, n, xsrc, _) in enumerate(btiles):
                c_ = k % 2
                if k + 1 < len(btiles):
                    a2_load(k + 1)
                M, BM = midt[c_], B_midt[c_]
                tb = 4 + (k % 2)
                for c in range(8):
                    PE(lambda e, c=c: e.transpose(out=pb_bf(tb)[:, c * 128:c * 128 + n], in_=M[:n, c * 128:(c + 1) * 128],
                                                  identity=ident_b[:n, :n]), r=[BM, B_c], w=[B_pb[tb]])
                DVE(lambda e: e.tensor_copy(out=oT[:, :, :n], in_=pb_bf(tb).rearrange("p (c t) -> p c t", c=8)[:, :, :n]),
                    r=[B_pb[tb]], w=[B_oT])
                for half in range(2):
                    hs_ = slice(half * 512, (half + 1) * 512)
                    for c in range(4):
                        PE(lambda e, c=c, half=half, hs_=hs_: e.matmul(pbank[half][:n, :], lhsT=oT[:, c, :n], rhs=w_a_sb[:, c, hs_],
                                                                       start=(c == 0), stop=(c == 3)), r=[B_oT, B_w], w=[B_pb[half]])
                    for c in range(4):
                        PE(lambda e, c=c, half=half, hs_=hs_: e.matmul(pbank[2 + half][:n, :], lhsT=oT[:, 4 + c, :n], rhs=w_b_sb[:, c, hs_],
                                                                       start=(c == 0), stop=(c == 3)), r=[B_oT, B_w], w=[B_pb[2 + half]])
                for half in range(2):
                    hs_ = slice(half * 512, (half + 1) * 512)
                    DVE(lambda e, half=half, hs_=hs_: e.tensor_tensor(out=ua[:n, hs_], in0=pbank[half][:n, :],
                                                                      in1=M[:n, 1024 + half * 512:1024 + (half + 1) * 512], op=OP.mult),
                        r=[B_pb[half], BM], w=[B_ua])
                    DVE(lambda e, half=half, hs_=hs_: e.tensor_tensor(out=ub[:n, hs_], in0=pbank[2 + half][:n, :],
                                                                      in1=M[:n, 2048 + half * 512:2048 + (half + 1) * 512], op=OP.mult),
                        r=[B_pb[2 + half], BM], w=[B_ub])
                DVE(lambda e: e.tensor_tensor(out=u_bf[:n, :], in0=ua[:n, :], in1=ub[:n, :], op=OP.add), r=[B_ua, B_ub], w=[B_ubf])
                tb2 = 6 + (k % 2)
                for c in range(8):
                    PE(lambda e, c=c: e.transpose(out=pb_bf(tb2)[:, c * 128:c * 128 + n], in_=u_bf[:n, c * 128:(c + 1) * 128],
                                                  identity=ident_b[:n, :n]), r=[B_ubf, B_c], w=[B_pb[tb2]])
                ACT(lambda e: e.copy(out=uT[:, :, :n], in_=pb_bf(tb2).rearrange("p (c t) -> p c t", c=8)[:, :, :n]),
                    r=[B_pb[tb2]], w=[B_uT])
                for half in range(2):
                    hs_ = slice(half * 512, (half + 1) * 512)
                    for c in range(8):
                        PE(lambda e, c=c, half=half, hs_=hs_: e.matmul(pbank[half][:n, :], lhsT=uT[:, c, :n], rhs=w_out_sb[:, c, hs_],
                                                                       start=(c == 0), stop=(c == 7)), r=[B_uT, B_w], w=[B_pb[half]])
                    DVE(lambda e, half=half, hs_=hs_: e.tensor_tensor(out=xt2[c_][:n, hs_], in0=pbank[half][:n, :], in1=xt2[c_][:n, hs_], op=OP.add),
                        r=[B_pb[half], B_xt2[c_]], w=[B_xt2[c_]])
                S.dma("sp", h2s[r0:r0 + n, :], xt2[c_][:n, :], reads=[B_xt2[c_]], writes=[B_h2s], sb=B_xt2[c_])

        S.barrier()
        if cfg.stage < 4:
            S.finish("sp")
            print("ninst", S.ninst, S.cnt)
            return nc

        es_c = contextlib.ExitStack()

        def sbc(name, shape, dt=F32):
            return es_c.enter_context(nc.sbuf_tensor(name, list(shape), dt))

        TPB = 2
        TSUB = 8
        with es_c:
            n2b = bcast_load("n2b", norm2, D, alloc=sbc)
            fnb = bcast_load("fnb", final_norm, D, alloc=sbc)
            iota_i = sbc("iota_i", [128, 128], I32)
            iota_f = sbc("iota_f", [128, 128], F32)
            bmask = sbc("bmask", [128, 8], F32)
            POOL(lambda e: e.iota(out=iota_i[:], pattern=[[1, 128]], base=0, channel_multiplier=0), w=[B_c])
            POOL(lambda e: e.memset(bmask[:], 1.0), w=[B_c])
            POOL(lambda e: e.affine_select(out=bmask[:], in_=bmask[:], pattern=[[-16, 8]], compare_op=OP.is_ge, fill=0.0,
                                           base=0, channel_multiplier=1), r=[B_c], w=[B_c])
            POOL(lambda e: e.affine_select(out=bmask[:], in_=bmask[:], pattern=[[16, 8]], compare_op=OP.is_ge, fill=0.0,
                                           base=15, channel_multiplier=-1), r=[B_c], w=[B_c])
            DVE(lambda e: e.tensor_copy(out=iota_f[:], in_=iota_i[:]), r=[B_c], w=[B_c])

            GT_sb = sbc("GT_sb", [128, TPB * 128, 128], BF16)
            B_GT = [Buf("GT%d" % j) for j in range(TPB)]
            h2 = [sbc("h2_%d" % j, [128, D], F32) for j in range(TPB)]
            B_h2 = [Buf("h2_%d" % j) for j in range(TPB)]
            stage_ref["bufs"], stage_ref["B"] = h2, B_h2
            p_wq_sb = sbc("p_wq_sb", [128, 8, 2048], BF16)
            load_w(p_wq_sb, p_wq, 8, 2048)
            keysT = sbc("keysT", [128, 16, 128], BF16)
            kst = GT_sb[:, 0:32, :].bitcast(F32).rearrange("p (g k) d -> p g (k d)", g=16)
            S.dma("sp", kst, p_keys.rearrange("g k d -> k g d"), writes=[B_GT[0]])
            for g4 in range(4):
                bk = g4
                for gg in range(4):
                    g = g4 * 4 + gg
                    PE(lambda e, g=g, gg=gg, bk=bk: e.transpose(out=pbank[bk][:, gg * 128:(gg + 1) * 128], in_=kst[:, g, :],
                                                                 identity=ident_f[:]), r=[B_GT[0], B_c], w=[B_pb[bk]])
                DVE(lambda e, g4=g4, bk=bk: e.tensor_copy(out=keysT[:, g4 * 4:(g4 + 1) * 4, :],
                                                          in_=pbank[bk].rearrange("p (g k) -> p g k", g=4)), r=[B_pb[bk]], w=[B_w])
            xn2Ts = [sbc("xn2T_%d" % i, [128, 8, TPB * 128], BF16) for i in range(2)]
            B_xn2Ts = [[Buf("xn2T%d_%d" % (i, j)) for j in range(TPB)] for i in range(2)]
            xn2 = sbc("xn2", [128, D], BF16)
            B_xn2 = Buf("xn2")
            ss2 = sbc("ss2", [128, 1], F32)
            rstd2 = sbc("rstd2", [128, 1], F32)
            B_ss2, B_rstd2 = Buf("ss2"), Buf("rstd2")
            q_bf = sbc("q_bf", [128, 2048], BF16)
            B_q = Buf("q_bf")
            qT = sbc("qT", [128, 16, 128], BF16)
            B_qT = Buf("qT")
            s_sb = sbc("s_sb", [128, 2048], F32)
            B_s = Buf("s_sb")
            comb = sbc("comb", [128, 2048], F32)
            B_comb = Buf("comb")
            W_bf = sbc("W_bf", [128, 2048], BF16)
            B_W = Buf("W_bf")
            sv = sbc("sv", [128, 256], F32)
            si_u = sbc("si_u", [128, 256], U32)
            B_sv, B_siu = Buf("sv"), Buf("si_u")
            si01 = sbc("si01", [128, 2, 128], BF16)
            B_si01 = Buf("si01")
            work = sbc("work", [128, 128], F32)
            work2 = sbc("work2", [128, 256], F32)
            B_work, B_work2 = Buf("work"), Buf("work2")
            m8a = sbc("m8a", [128, 8, 8], F32)
            m8b = sbc("m8b", [128, 8, 8], F32)
            negm = sbc("negm", [128, 8], F32)
            Zs = sbc("Zs", [128, 8], F32)
            B_m8 = Buf("m8")
            B_Z = Buf("Z")
            WT2 = [sbc("WT_sb%d" % j, [128, 16, 128], BF16) for j in range(TPB)]
            B_WT2 = [Buf("WT%d" % j) for j in range(TPB)]
            WB4D = True
            bmask_b = sbc("bmask_b", [128, 8], BF16)
            DVE(lambda e: e.tensor_copy(out=bmask_b[:], in_=bmask[:]), r=[B_c], w=[B_c])
            siT2 = [sbc("siT%d" % j, [128, 2, 128], BF16) for j in range(TPB)]
            iota_b = sbc("iota_b", [128, 128], BF16)
            DVE(lambda e: e.tensor_copy(out=iota_b[:], in_=iota_f[:]), r=[B_c], w=[B_c])
            B_siT2 = [Buf("siT%d" % j) for j in range(TPB)]
            A0 = [sbc("A0_%d" % i, [128, TSUB, 128], BF16) for i in range(2)]
            B0 = [sbc("B0_%d" % i, [128, TSUB, 128], BF16) for i in range(2)]
            Wblk = [sbc("Wblk%d" % i, [128, TSUB, 128], BF16) for i in range(2)]
            B_A0 = [Buf("A0_%d" % i) for i in range(2)]
            B_B0 = [Buf("B0_%d" % i) for i in range(2)]
            B_Wblk = [Buf("Wblk%d" % i) for i in range(2)]
            R_sb = sbc("R_sb", [128, TSUB, 128], BF16)
            B_R = [Buf("R%d" % i) for i in range(TSUB // 4)]
            NRING = 5
            dn = [sbc("dn%d" % i, [128, 8, 128], BF16) for i in range(NRING)]
            upc = [sbc("upc%d" % i, [128, D], BF16) for i in range(NRING)]
            B_dn = [Buf("dn%d" % i) for i in range(NRING)]
            B_up = [Buf("upc%d" % i) for i in range(NRING)]
            gsb = [sbc("gsb%d" % i, [128, TPB * 128], F32) for i in range(2)]
            gh = [sbc("gh%d" % i, [128, TPB * 128], BF16) for i in range(2)]
            B_g = [Buf("g%d" % i) for i in range(2)]
            B_gh = [Buf("gh%d" % i) for i in range(2)]
            yt, B_yt = comb[:, 0:D], B_comb
            junk2, B_junk2 = q_bf[:, 0:D], B_q

            r8 = [0]

            def nb8():
                b_ = r8[0]
                r8[0] = (b_ + 1) % 8
                return b_

            rt = [0]

            def nbt():
                rt[0] ^= 1
                return 4 + rt[0]

            ec = [0]
            sub_k = [0]

            def tok_major(j, r0, n, xs):
                xn2T, siT, WT_sb, B_siT, B_WT = xn2Ts[xs], siT2[j], WT2[j], B_siT2[j], B_WT2[j]
                H2, BH2 = h2[j], B_h2[j]
                S.dma("sp", H2[:n, :], h2s[r0:r0 + n, :], reads=[B_h2s], writes=[BH2])
                ACT(lambda e: e.activation(out=junk2[:n, :], in_=H2[:n, :], func=AF.Square, accum_out=ss2[:n, :]),
                    r=[BH2], w=[B_junk2, B_ss2])
                ACT(lambda e: e.activation(out=rstd2[:n, :], in_=ss2[:n, :], func=AF.Sqrt, scale=1.0 / D, bias=EPS),
                    r=[B_ss2], w=[B_rstd2])
                DVE(lambda e: e.reciprocal(out=rstd2[:n, :], in_=rstd2[:n, :]), r=[B_rstd2], w=[B_rstd2])
                DVE(lambda e: e.scalar_tensor_tensor(out=xn2[:n, :], in0=H2[:n, :], scalar=rstd2[:n, :], in1=n2b[:n, :],
                                                     op0=OP.mult, op1=OP.mult), r=[BH2, B_rstd2, B_c], w=[B_xn2])
                bk = nbt()
                for c in range(8):
                    PE(lambda e, c=c, bk=bk: e.transpose(out=pb_bf(bk)[:, c * 128:c * 128 + n], in_=xn2[:n, c * 128:(c + 1) * 128],
                                                         identity=ident_b[:n, :n]), r=[B_xn2, B_c], w=[B_pb[bk]])
                DVE(lambda e, bk=bk: e.tensor_copy(out=xn2T[:, :, j * 128:j * 128 + n],
                                                   in_=pb_bf(bk).rearrange("p (c t) -> p c t", c=8)[:, :, :n]),
                    r=[B_pb[bk]], w=[B_xn2Ts[xs][j]])
                yield
                for g in range(4):
                    bk = nbt()
                    for c in range(8):
                        PE(lambda e, c=c, bk=bk, g=g: e.matmul(pbank[bk][:n, :], lhsT=xn2T[:, c, j * 128:j * 128 + n],
                                                               rhs=p_wq_sb[:, c, g * 512:(g + 1) * 512], start=(c == 0), stop=(c == 7)),
                           r=[B_xn2Ts[xs][j], B_w], w=[B_pb[bk]])
                    if g % 2 == 0:
                        ACT(lambda e, bk=bk, g=g: e.copy(out=q_bf[:n, g * 512:(g + 1) * 512], in_=pbank[bk][:n, :]), r=[B_pb[bk]], w=[B_q])
                    else:
                        DVE(lambda e, bk=bk, g=g: e.tensor_copy(out=q_bf[:n, g * 512:(g + 1) * 512], in_=pbank[bk][:n, :]), r=[B_pb[bk]], w=[B_q])
                    yield
                for g2 in range(2):
                    bk = nbt()
                    for c in range(8):
                        hc = g2 * 8 + c
                        PE(lambda e, c=c, hc=hc, bk=bk: e.transpose(out=pb_bf(bk)[:, c * 128:c * 128 + n], in_=q_bf[:n, hc * 128:(hc + 1) * 128],
                                                                    identity=ident_b[:n, :n]), r=[B_q, B_c], w=[B_pb[bk]])
                    if g2 == 0:
                        ACT(lambda e, bk=bk, g2=g2: e.copy(out=qT[:, g2 * 8:(g2 + 1) * 8, :n],
                                                           in_=pb_bf(bk).rearrange("p (c t) -> p c t", c=8)[:, :, :n]), r=[B_pb[bk]], w=[B_qT])
                    else:
                        DVE(lambda e, bk=bk, g2=g2: e.tensor_copy(out=qT[:, g2 * 8:(g2 + 1) * 8, :n],
                                                                  in_=pb_bf(bk).rearrange("p (c t) -> p c t", c=8)[:, :, :n]), r=[B_pb[bk]], w=[B_qT])
                    yield
                for g4 in range(4):
                    bk = nbt()
                    for gg in range(4):
                        hc = g4 * 4 + gg
                        PE(lambda e, hc=hc, gg=gg, bk=bk: e.matmul(pbank[bk][:n, gg * 128:(gg + 1) * 128], lhsT=qT[:, hc, :n], rhs=keysT[:, hc, :],
                                                                   start=True, stop=True), r=[B_qT, B_w], w=[B_pb[bk]])
                    if g4 % 2 == 0:
                        ACT(lambda e, bk=bk, g4=g4: e.copy(out=s_sb[:n, g4 * 512:(g4 + 1) * 512], in_=pbank[bk][:n, :]), r=[B_pb[bk]], w=[B_s])
                    else:
                        DVE(lambda e, bk=bk, g4=g4: e.tensor_copy(out=s_sb[:n, g4 * 512:(g4 + 1) * 512], in_=pbank[bk][:n, :]), r=[B_pb[bk]], w=[B_s])
                    yield
                for hc in range(16):
                    grp = s_sb[:n, hc * 128:(hc + 1) * 128]
                    o8a = slice(hc * 16, hc * 16 + 8)
                    o8b = slice(hc * 16 + 8, hc * 16 + 16)
                    DVE(lambda e, grp=grp, o8a=o8a: e.max(out=sv[:n, o8a], in_=grp), r=[B_s], w=[B_sv])
                    DVE(lambda e, grp=grp, o8a=o8a: e.max_index(out=si_u[:n, o8a], in_max=sv[:n, o8a], in_values=grp), r=[B_s, B_sv], w=[B_siu])
                    DVE(lambda e, grp=grp, o8a=o8a: e.match_replace(out=work[:n, :], in_to_replace=sv[:n, o8a], in_values=grp, imm_value=-1e30),
                        r=[B_s, B_sv], w=[B_work])
                    DVE(lambda e, o8b=o8b: e.max(out=sv[:n, o8b], in_=work[:n, :]), r=[B_work], w=[B_sv])
                    DVE(lambda e, o8b=o8b: e.max_index(out=si_u[:n, o8b], in_max=sv[:n, o8b], in_values=work[:n, :]), r=[B_work, B_sv], w=[B_siu])
                    yield
                siv = si_u[:n, :].rearrange("p (h c a) -> p c h a", h=8, c=2)
                for c2 in range(2):
                    DVE(lambda e, c2=c2: e.tensor_copy(out=si01[:n, c2, :].rearrange("p (h a) -> p h a", h=8), in_=siv[:, c2]),
                        r=[B_siu], w=[B_si01])
                svv = sv[:n, :].rearrange("p (h c a) -> p h c a", h=8, c=2)
                DVE(lambda e: e.tensor_tensor(out=comb[:n, :].rearrange("p (h a b) -> p h a b", h=8, a=16),
                                              in0=svv[:, :, 0, :].unsqueeze(3).to_broadcast([n, 8, 16, 16]),
                                              in1=svv[:, :, 1, :].unsqueeze(2).to_broadcast([n, 8, 16, 16]), op=OP.add),
                    r=[B_sv], w=[B_comb])
                yield
                for h in range(8):
                    cg = comb[:n, h * 256:(h + 1) * 256]
                    DVE(lambda e, cg=cg, h=h: e.max(out=m8a[:n, h, :], in_=cg), r=[B_comb], w=[B_m8])
                    DVE(lambda e, cg=cg, h=h: e.match_replace(out=work2[:n, :], in_to_replace=m8a[:n, h, :], in_values=cg, imm_value=-1e30),
                        r=[B_comb, B_m8], w=[B_work2])
                    DVE(lambda e, h=h: e.max(out=m8b[:n, h, :], in_=work2[:n, :]), r=[B_work2], w=[B_m8])
                    yield
                DVE(lambda e: e.tensor_scalar(out=negm[:n, :], in0=m8a[:n, :, 0], scalar1=-1.0, scalar2=None, op0=OP.mult), r=[B_m8], w=[B_m8])
                for h in range(8):
                    cg = comb[:n, h * 256:(h + 1) * 256]
                    eg = s_sb[:n, h * 256:(h + 1) * 256]
                    ACT(lambda e, cg=cg, eg=eg, h=h: e.activation(out=eg, in_=cg, func=AF.Exp, bias=negm[:n, h:h + 1], scale=1.0),
                        r=[B_comb, B_m8], w=[B_s])
                yield
                for h in range(8):
                    cg = comb[:n, h * 256:(h + 1) * 256]
                    eg = s_sb[:n, h * 256:(h + 1) * 256]
                    DVE(lambda e, cg=cg, eg=eg, h=h: e.scalar_tensor_tensor(out=eg, in0=cg, scalar=m8b[:n, h, 7:8], in1=eg,
                                                                            op0=OP.is_ge, op1=OP.mult), r=[B_comb, B_m8, B_s], w=[B_s])
                    if h % 2 == 1:
                        yield
                DVE(lambda e: e.tensor_reduce(out=Zs[:n, :], in_=s_sb[:n, :].rearrange("p (h x) -> p h x", h=8), axis=AX.X, op=OP.add),
                    r=[B_s], w=[B_Z])
                DVE(lambda e: e.reciprocal(out=Zs[:n, :], in_=Zs[:n, :]), r=[B_Z], w=[B_Z])
                DVE(lambda e: e.tensor_tensor(out=W_bf[:n, :].rearrange("p (h x) -> p h x", h=8),
                                              in0=s_sb[:n, :].rearrange("p (h x) -> p h x", h=8),
                                              in1=Zs[:n, :].unsqueeze(2).to_broadcast([n, 8, 256]), op=OP.mult), r=[B_s, B_Z], w=[B_W])
                yield
                bk = nbt()
                for c2 in range(2):
                    PE(lambda e, c2=c2, bk=bk: e.transpose(out=pb_bf(bk)[:, c2 * 128:c2 * 128 + n], in_=si01[:n, c2, :], identity=ident_b[:n, :n]),
                       r=[B_si01, B_c], w=[B_pb[bk]])
                ACT(lambda e, bk=bk: e.copy(out=siT[:, :, :n], in_=pb_bf(bk)[:, 0:256].rearrange("p (c t) -> p c t", c=2)[:, :, :n]),
                    r=[B_pb[bk]], w=[B_siT])
                yield
                Wv = W_bf[:n, :].rearrange("p (ha b) -> p ha b", b=16)
                for g2 in range(2):
                    bk = nbt()
                    for bb in range(8):
                        b_ = g2 * 8 + bb
                        PE(lambda e, bb=bb, b_=b_, bk=bk: e.transpose(out=pb_bf(bk)[:, bb * 128:bb * 128 + n], in_=Wv[:, :, b_],
                                                                      identity=ident_b[:n, :n]), r=[B_W, B_c], w=[B_pb[bk]])
                    if g2 == 0:
                        ACT(lambda e, bk=bk, g2=g2: e.copy(out=WT_sb[:, g2 * 8:(g2 + 1) * 8, :n],
                                                           in_=pb_bf(bk).rearrange("p (c t) -> p c t", c=8)[:, :, :n]), r=[B_pb[bk]], w=[B_WT])
                    else:
                        DVE(lambda e, bk=bk, g2=g2: e.tensor_copy(out=WT_sb[:, g2 * 8:(g2 + 1) * 8, :n],
                                                                  in_=pb_bf(bk).rearrange("p (c t) -> p c t", c=8)[:, :, :n]), r=[B_pb[bk]], w=[B_WT])
                    yield
            def slot_phase(j, n):
                siT, WT_sb, B_siT, B_WT = siT2[j], WT2[j], B_siT2[j], B_WT2[j]
                for t0 in range(0, n, TSUB):
                    tn = min(TSUB, n - t0)
                    sk = sub_k[0] % 2
                    sub_k[0] += 1
                    DVE(lambda e, sk=sk, t0=t0, tn=tn: e.tensor_tensor(
                        out=A0[sk][:, :tn, :], in0=iota_b[:].unsqueeze(1).to_broadcast([128, tn, 128]),
                        in1=siT[:, 0, t0:t0 + tn].unsqueeze(2).to_broadcast([128, tn, 128]), op=OP.is_equal),
                        r=[B_siT, B_c], w=[B_A0[sk]])
                    DVE(lambda e, sk=sk, t0=t0, tn=tn: e.tensor_tensor(
                        out=B0[sk][:, :tn, :], in0=iota_b[:].unsqueeze(1).to_broadcast([128, tn, 128]),
                        in1=siT[:, 1, t0:t0 + tn].unsqueeze(2).to_broadcast([128, tn, 128]), op=OP.is_equal),
                        r=[B_siT, B_c], w=[B_B0[sk]])
                    if WB4D:
                        DVE(lambda e, sk=sk, t0=t0, tn=tn: e.tensor_tensor(
                            out=Wblk[sk][:, :tn, :].rearrange("p t (h b) -> p t h b", h=8),
                            in0=WT_sb[:, :, t0:t0 + tn].rearrange("p b t -> p t b").unsqueeze(2).to_broadcast([128, tn, 8, 16]),
                            in1=bmask_b[:, :].unsqueeze(1).unsqueeze(3).to_broadcast([128, tn, 8, 16]), op=OP.mult),
                            r=[B_WT, B_c], w=[B_Wblk[sk]])
                    else:
                        for hp in range(8):
                            if hp % 2 == 0:
                                DVE(lambda e, sk=sk, t0=t0, tn=tn, hp=hp: e.tensor_scalar(
                                    out=Wblk[sk][:, :tn, hp * 16:(hp + 1) * 16], in0=WT_sb[:, :, t0:t0 + tn].rearrange("p b t -> p t b"),
                                    scalar1=bmask[:, hp:hp + 1], scalar2=None, op0=OP.mult), r=[B_WT, B_c], w=[B_Wblk[sk]])
                            else:
                                ACT(lambda e, sk=sk, t0=t0, tn=tn, hp=hp: e.activation(
                                    out=Wblk[sk][:, :tn, hp * 16:(hp + 1) * 16], in_=WT_sb[:, :, t0:t0 + tn].rearrange("p b t -> p t b"),
                                    func=AF.Copy, scale=bmask[:, hp:hp + 1]), r=[B_WT, B_c], w=[B_Wblk[sk]])
                    def emit_R(q0, sk=sk, tn=tn):
                        w4 = min(4, tn - q0)
                        BR = B_R[q0 // 4]
                        bk = nb8()
                        for tt in range(q0, q0 + w4):
                            PE(lambda e, tt=tt, q0=q0, bk=bk, sk=sk: e.matmul(pbank[bk][:, (tt - q0) * 128:(tt - q0 + 1) * 128], lhsT=Wblk[sk][:, tt, :],
                                                                              rhs=A0[sk][:, tt, :], start=True, stop=True),
                               r=[B_Wblk[sk], B_A0[sk]], w=[B_pb[bk]])
                        ACT(lambda e, bk=bk, q0=q0, w4=w4: e.copy(out=R_sb[:, q0:q0 + w4, :],
                                                                   in_=pbank[bk][:, :w4 * 128].rearrange("p (t i) -> p t i", i=128)),
                            r=[B_pb[bk]], w=[BR])

                    def emit_GT(q0, sk=sk, tn=tn, t0=t0):
                        w4 = min(4, tn - q0)
                        BR = B_R[q0 // 4]
                        bk2 = nb8()
                        for tt in range(q0, q0 + w4):
                            PE(lambda e, tt=tt, q0=q0, bk2=bk2, sk=sk: e.matmul(pbank[bk2][:, (tt - q0) * 128:(tt - q0 + 1) * 128], lhsT=B0[sk][:, tt, :],
                                                                                rhs=R_sb[:, tt, :], start=True, stop=True),
                               r=[B_B0[sk], BR], w=[B_pb[bk2]])
                        g0 = j * 128 + t0 + q0
                        ACT(lambda e, bk2=bk2, g0=g0, w4=w4: e.copy(out=GT_sb[:, g0:g0 + w4, :],
                                                                     in_=pbank[bk2][:, :w4 * 128].rearrange("p (t i) -> p t i", i=128)),
                            r=[B_pb[bk2]], w=[B_GT[j]])

                    q0s = list(range(0, tn, 4))
                    for gi, q0 in enumerate(q0s):
                        emit_R(q0)
                        if gi > 0:
                            emit_GT(q0s[gi - 1])
                    emit_GT(q0s[-1])

            def drain(g):
                for _ in g:
                    pass

            def tok_chain(blk, xs):
                for j, (r0, n, _, _) in enumerate(blk):
                    yield from tok_major(j, r0, n, xs)

            blocks = [btiles[k0:k0 + TPB] for k0 in range(0, len(btiles), TPB)]
            drain(tok_chain(blocks[0], 0))
            for bi, blk in enumerate(blocks):
                xs = bi % 2
                xn2T = xn2Ts[xs]
                for j, (r0, n, _, _) in enumerate(blk):
                    slot_phase(j, n)
                span = (len(blk) - 1) * 128 + blk[-1][1]
                BXT = [B_xn2Ts[xs][j] for j in range(len(blk))]
                BGT = [B_GT[j] for j in range(len(blk))]
                nxt = tok_chain(blocks[bi + 1], 1 - xs) if bi + 1 < len(blocks) else iter(())

                def emit_hid(c, xn2T=xn2T, span=span, BXT=BXT, BGT=BGT):
                    k3 = ec[0] % NRING
                    e2 = ec[0] % 2
                    ec[0] += 1
                    S.dma("sp", dn[k3][:].rearrange("p a b -> p (a b)"), dnT_bf[c], reads=B_pre, writes=[B_dn[k3]])
                    S.dma("sp", upc[k3][:], up_bf[c * 128:(c + 1) * 128, :], reads=B_pre, writes=[B_up[k3]])
                    hb = 6 + e2
                    for dh in range(8):
                        PE(lambda e, dh=dh, hb=hb, k3=k3: e.matmul(pbank[hb][:, :span], lhsT=dn[k3][:, dh, :], rhs=xn2T[:, dh, :span],
                                                                   start=(dh == 0), stop=(dh == 7)), r=[B_dn[k3]] + BXT, w=[B_pb[hb]])
                    ACT(lambda e, hb=hb, e2=e2: e.activation(out=gsb[e2][:, :span], in_=pbank[hb][:, :span], func=AF.Gelu),
                        r=[B_pb[hb]], w=[B_g[e2]])
                    DVE(lambda e, e2=e2, c=c: e.tensor_tensor(out=gh[e2][:, :span], in0=gsb[e2][:, :span], in1=GT_sb[:, :span, c], op=OP.mult),
                        r=[B_g[e2]] + BGT, w=[B_gh[e2]])
                    return (c, k3, e2)

                def emit_out(st_, blk=blk):
                    c, k3, e2 = st_
                    for j, (r0, n, _, _) in enumerate(blk):
                        for half in range(2):
                            ob = 2 * j + half
                            PE(lambda e, j=j, n=n, half=half, ob=ob, e2=e2, k3=k3, c=c: e.matmul(
                                pbank[ob][:n, :], lhsT=gh[e2][:, j * 128:j * 128 + n], rhs=upc[k3][:, half * 512:(half + 1) * 512],
                                start=(c == 0), stop=(c == NCH - 1)), r=[B_gh[e2], B_up[k3]], w=[B_pb[ob]])

                prev = None
                for c in range(NCH):
                    cur_ = emit_hid(c)
                    next(nxt, None)
                    if prev is not None:
                        emit_out(prev)
                    prev = cur_
                emit_out(prev)
                drain(nxt)
                for j, (r0, n, _, ydst) in enumerate(blk):
                    H2, BH2 = h2[j], B_h2[j]
                    S.dma("sp", H2[:n, :], h2s[r0:r0 + n, :], reads=[B_h2s], writes=[BH2])
                    for half in range(2):
                        ob = 2 * j + half
                        hs_ = slice(half * 512, (half + 1) * 512)
                        DVE(lambda e, ob=ob, hs_=hs_, H2=H2, n=n: e.tensor_tensor(out=H2[:n, hs_], in0=pbank[ob][:n, :], in1=H2[:n, hs_], op=OP.add),
                            r=[B_pb[ob], BH2], w=[BH2])
                    ACT(lambda e, H2=H2, n=n: e.activation(out=junk2[:n, :], in_=H2[:n, :], func=AF.Square, accum_out=ss2[:n, :]),
                        r=[BH2], w=[B_junk2, B_ss2])
                    ACT(lambda e, n=n: e.activation(out=rstd2[:n, :], in_=ss2[:n, :], func=AF.Sqrt, scale=1.0 / D, bias=EPS),
                        r=[B_ss2], w=[B_rstd2])
                    DVE(lambda e, n=n: e.reciprocal(out=rstd2[:n, :], in_=rstd2[:n, :]), r=[B_rstd2], w=[B_rstd2])
                    DVE(lambda e, H2=H2, n=n: e.scalar_tensor_tensor(out=yt[:n, :], in0=H2[:n, :], scalar=rstd2[:n, :], in1=fnb[:n, :],
                                                                     op0=OP.mult, op1=OP.mult), r=[BH2, B_rstd2, B_c], w=[B_yt])
                    S.dma("sp", ydst, yt[:n, :], reads=[B_yt], sb=B_yt, is_output=True)
        S.finish("sp")
    print("ninst", S.ninst, S.cnt)
    return nc


_OUT_NAMES = ["y_prompt", "y_sample", "k_prompt", "v_prompt", "s_prompt", "k_sample", "v_sample", "s_sample"]


def make_in_maps(inputs, cfg, ncores):
    f = lambda a: np.ascontiguousarray(np.asarray(a, dtype=np.float32))
    shared = {"p_downT": f(np.asarray(inputs["p_down"], dtype=np.float32)[0].reshape(128, 128, 8, 128).transpose(0, 3, 2, 1)).reshape(128, 128, D)}
    maps = []
    for c in range(ncores):
        ps_ = slice(c * cfg.nseq_p, (c + 1) * cfg.nseq_p)
        ss_ = slice(c * cfg.nseq_s, (c + 1) * cfg.nseq_s)
        m = {
            "x_prompt": f(inputs["x_prompt"][ps_, :cfg.t_p]),
            "x_sample": f(inputs["x_sample"][ss_]),
            "cache_k": f(inputs["cache_k"][0, ss_]).reshape(cfg.nseq_s, cfg.past, 512),
            "cache_v": f(inputs["cache_v"][0, ss_]).reshape(cfg.nseq_s, cfg.past, 512),
            "state_hgrn": f(inputs["state_hgrn"][0, ss_]),
            "norm1": f(inputs["norm1"]).reshape(1, D),
            "w_in": f(inputs["w_in"][0]),
            "lam_params": f(inputs["lam_params"]).reshape(1, 256),
            "a_subln": f(inputs["a_subln"]).reshape(1, 128),
            "r_lb_logits": f(inputs["r_lb_logits"]),
            "r_gnorm": f(inputs["r_gnorm"]).reshape(1, 128),
            "w_a": f(inputs["w_a"][0]),
            "w_b": f(inputs["w_b"][0]),
            "w_out": f(inputs["w_out"][0]),
            "norm2": f(inputs["norm2"]).reshape(1, D),
            "p_wq": f(inputs["p_wq"][0]),
            "p_keys": f(inputs["p_keys"][0]).reshape(16, 128, 128),
            "p_downT": shared["p_downT"],
            "p_up": f(inputs["p_up"][0]),
            "final_norm": f(inputs["final_norm"]).reshape(1, D),
        }
        maps.append(m)
    return maps


def run(inputs, cfg, ncores):
    nc = build(cfg)
    maps = make_in_maps(inputs, cfg, ncores)
    res = run_bass_kernel_spmd(nc, maps, core_ids=list(range(ncores)))
    outs = []
    for name in _OUT_NAMES:
        parts = [np.asarray(r[name]) for r in res.results]
        outs.append(np.concatenate(parts, axis=0))
    return outs


def kernel(**inputs):
    cfg = Cfg()
    yp, ys, kp, vp, sp_, ks, vs, ss_ = run(inputs, cfg, NCORES)
    B, T = 32, cfg.t_p
    return (yp.reshape(B, T, D), ys.reshape(32, cfg.t_s, D),
            kp.reshape(1, B, T, 4, 128), vp.reshape(1, B, T, 4, 128), sp_.reshape(1, B, 4, 128, 128),
            ks.reshape(1, 32, cfg.t_s, 4, 128), vs.reshape(1, 32, cfg.t_s, 4, 128), ss_.reshape(1, 32, 4, 128, 128))
```
